# Optimizing a Trainium2 kernel written in Bass

```python
import jax, jax.numpy as jnp
from jax import lax
import numpy as np

D_MODEL = 1024
BATCH = 16
SEQ = 4096
DEPTH = 4

GRID_W = 64
CTX_LEN = 256
D_CONV = D_MODEL // 4
CONV_K = 3
D_GMLP = D_MODEL // 4
GMLP_GROUPS = 4
GMLP_CHUNK = 128
HEAD_DIM = 64
N_HEADS = D_MODEL // 128
N_KV_HEADS = N_HEADS // 4
Q_PER_KV = N_HEADS // N_KV_HEADS
WINDOW = 128
ATT_BLOCK = 128
ROPE_BASE = 10000.0
N_BRANCH = 3
D_FF_DENSE = ((8 * D_MODEL // 3) + 255) // 256 * 256
N_EXPERTS = 8
TOP_K = 2
D_FF_EXPERT = 7 * D_MODEL // 2
N_DENSE = (DEPTH + 1) // 2
N_MOE = DEPTH // 2
EPS = 1e-6
NEG_INF = -1e30

OFF_CONV = 0
OFF_GMLP = OFF_CONV + 3 * D_CONV
OFF_Q = OFF_GMLP + 2 * D_GMLP
OFF_K = OFF_Q + N_HEADS * HEAD_DIM
OFF_V = OFF_K + N_KV_HEADS * HEAD_DIM
OFF_GATE = OFF_V + N_KV_HEADS * HEAD_DIM
D_IN = OFF_GATE + N_BRANCH * D_MODEL
KV_W = N_KV_HEADS * HEAD_DIM

kernel_name = "hybrid_parallel_conv_gmlp_swa_moe_dit"


def rms_norm(x, g):
    xf = x.astype(jnp.float32)
    y = xf * lax.rsqrt(jnp.mean(xf * xf, axis=-1, keepdims=True) + EPS)
    return (y * g.astype(jnp.float32)).astype(x.dtype)


def ada_params(cvec, w_mod, b_mod, n):
    m = jax.nn.silu(cvec) @ w_mod[:, :n * D_MODEL] + b_mod[:n * D_MODEL]
    return [p[..., None, :] for p in jnp.split(m, n, axis=-1)]


def modulate(h, shift, scale):
    return h * (1.0 + scale) + shift


def short_conv(z, w):
    L = z.shape[1]
    pad = CONV_K // 2
    zp = jnp.pad(z, ((0, 0), (pad, pad), (0, 0)))
    out = zp[:, 0:L] * w[0]
    for j in range(1, CONV_K):
        out = out + zp[:, j:j + L] * w[j]
    return out


def conv_branch(z, conv_w):
    b_g = z[..., OFF_CONV:OFF_CONV + D_CONV]
    c_g = z[..., OFF_CONV + D_CONV:OFF_CONV + 2 * D_CONV]
    h = z[..., OFF_CONV + 2 * D_CONV:OFF_GMLP]
    return b_g * short_conv(c_g * h, conv_w)


def gmlp_branch(z, ws, bs):
    uv = jax.nn.gelu(z[..., OFF_GMLP:OFF_Q])
    u, v = uv[..., :D_GMLP], uv[..., D_GMLP:]
    vf = v.astype(jnp.float32)
    mu = jnp.mean(vf, axis=-1, keepdims=True)
    var = jnp.mean(jnp.square(vf - mu), axis=-1, keepdims=True)
    vn = ((vf - mu) * lax.rsqrt(var + EPS)).astype(v.dtype)
    b_, L, _ = v.shape
    nc = L // GMLP_CHUNK
    vn = vn.reshape(b_, nc, GMLP_CHUNK, GMLP_GROUPS, D_GMLP // GMLP_GROUPS)
    sp = jnp.einsum("gpq,bnqgc->bnpgc", ws, vn) + bs.T[None, None, :, :, None]
    return u * sp.reshape(b_, L, D_GMLP)


def rope_tables(L):
    rows = L // GRID_W
    row = jnp.repeat(jnp.arange(rows), GRID_W).astype(jnp.float32)
    col = jnp.tile(jnp.arange(GRID_W), rows).astype(jnp.float32)
    half = HEAD_DIM // 2
    inv = 1.0 / (ROPE_BASE ** (jnp.arange(0, half, 2, dtype=jnp.float32) / half))
    ang_r = row[:, None] * inv[None, :]
    ang_c = col[:, None] * inv[None, :]
    ang = jnp.concatenate([ang_r, ang_r, ang_c, ang_c], axis=-1)
    return jnp.cos(ang), jnp.sin(ang)


def apply_rope_2d(x, cos, sin):
    half = HEAD_DIM // 2
    q = half // 2

    def rot(xh):
        return jnp.concatenate([-xh[..., q:], xh[..., :q]], axis=-1)

    xr = jnp.concatenate([rot(x[..., :half]), rot(x[..., half:])], axis=-1)
    c = cos.astype(x.dtype)[None, :, None, :]
    s = sin.astype(x.dtype)[None, :, None, :]
    return x * c + xr * s


def joint_softmax(sink, parts):
    m = sink
    for p in parts:
        m = jnp.maximum(m, jnp.max(p, axis=-1, keepdims=True))
    es = [jnp.exp(p - m) for p in parts]
    den = jnp.exp(sink - m)
    for e in es:
        den = den + jnp.sum(e, axis=-1, keepdims=True)
    inv = 1.0 / den
    return [e * inv for e in es]


def latent_attention(q, k, v, kc, vc, sink):
    b_, S = q.shape[0], q.shape[1]
    nb = S // ATT_BLOCK
    scale = HEAD_DIM ** -0.5
    qb = q.reshape(b_, nb, ATT_BLOCK, N_KV_HEADS, Q_PER_KV, HEAD_DIM)

    def band(t):
        tp = jnp.pad(t, ((0, 0), (ATT_BLOCK, ATT_BLOCK), (0, 0), (0, 0)))
        tp = tp.reshape(b_, nb + 2, ATT_BLOCK, N_KV_HEADS, HEAD_DIM)
        return jnp.concatenate([tp[:, :-2], tp[:, 1:-1], tp[:, 2:]], axis=2)

    kb, vb = band(k), band(v)
    s_loc = jnp.einsum("bnqkgd,bnjkd->bnkgqj", qb, kb, preferred_element_type=jnp.float32) * scale
    blk = jnp.arange(nb)[:, None, None]
    qpos = blk * ATT_BLOCK + jnp.arange(ATT_BLOCK)[None, :, None]
    kpos = (blk - 1) * ATT_BLOCK + jnp.arange(3 * ATT_BLOCK)[None, None, :]
    valid = (kpos >= 0) & (kpos < S) & (jnp.abs(qpos - kpos) <= WINDOW)
    s_loc = jnp.where(valid[None, :, None, None], s_loc, NEG_INF)
    s_ctx = jnp.einsum("bnqkgd,bckd->bnkgqc", qb, kc, preferred_element_type=jnp.float32) * scale
    sink_l = sink.astype(jnp.float32).reshape(N_KV_HEADS, Q_PER_KV)[:, :, None, None]
    p_loc, p_ctx = joint_softmax(sink_l, [s_loc, s_ctx])
    o = (jnp.einsum("bnkgqj,bnjkd->bnqkgd", p_loc.astype(v.dtype), vb)
         + jnp.einsum("bnkgqc,bckd->bnqkgd", p_ctx.astype(vc.dtype), vc))
    return o.reshape(b_, S, N_HEADS * HEAD_DIM)


def context_attention(qc, kc, vc, sink):
    b_, Lc = qc.shape[0], qc.shape[1]
    qg = qc.reshape(b_, Lc, N_KV_HEADS, Q_PER_KV, HEAD_DIM)
    s = jnp.einsum("bqkgd,bckd->bkgqc", qg, kc, preferred_element_type=jnp.float32) * (HEAD_DIM ** -0.5)
    sink_l = sink.astype(jnp.float32).reshape(N_KV_HEADS, Q_PER_KV)[:, :, None, None]
    (p,) = joint_softmax(sink_l, [s])
    o = jnp.einsum("bkgqc,bckd->bqkgd", p.astype(vc.dtype), vc)
    return o.reshape(b_, Lc, N_HEADS * HEAD_DIM)


def merge_branches(z, y_conv, y_gmlp, y_attn, w_bc, w_bg, w_ba, w_o):
    g = jax.nn.sigmoid(z[..., OFF_GATE:])
    mix = (g[..., :D_MODEL] * (y_conv @ w_bc)
           + g[..., D_MODEL:2 * D_MODEL] * (y_gmlp @ w_bg)
           + g[..., 2 * D_MODEL:] * (y_attn @ w_ba))
    return mix @ w_o


def token_mixers(hx, hc, w_in, conv_w, gm_ws, gm_b, sink, w_bc, w_bg, w_ba, w_o, cos, sin, ctx_out):
    b_, S = hx.shape[0], hx.shape[1]
    Lc = hc.shape[1]
    zx = hx @ w_in
    if ctx_out:
        zc = hc @ w_in
        zkv = zc[..., OFF_K:OFF_GATE]
    else:
        zkv = hc @ w_in[:, OFF_K:OFF_GATE]
    kc = zkv[..., :KV_W].reshape(b_, Lc, N_KV_HEADS, HEAD_DIM)
    vc = zkv[..., KV_W:].reshape(b_, Lc, N_KV_HEADS, HEAD_DIM)
    q = apply_rope_2d(zx[..., OFF_Q:OFF_K].reshape(b_, S, N_HEADS, HEAD_DIM), cos, sin)
    k = apply_rope_2d(zx[..., OFF_K:OFF_V].reshape(b_, S, N_KV_HEADS, HEAD_DIM), cos, sin)
    v = zx[..., OFF_V:OFF_GATE].reshape(b_, S, N_KV_HEADS, HEAD_DIM)
    ya = latent_attention(q, k, v, kc, vc, sink)
    out_x = merge_branches(zx, conv_branch(zx, conv_w), gmlp_branch(zx, gm_ws, gm_b), ya,
                           w_bc, w_bg, w_ba, w_o)
    out_c = None
    if ctx_out:
        qc = zc[..., OFF_Q:OFF_K].reshape(b_, Lc, N_HEADS, HEAD_DIM)
        yac = context_attention(qc, kc, vc, sink)
        out_c = merge_branches(zc, conv_branch(zc, conv_w), gmlp_branch(zc, gm_ws, gm_b), yac,
                               w_bc, w_bg, w_ba, w_o)
    return out_x, out_c


def swiglu(h, w_gu, w_d):
    gu = h @ w_gu
    g, u = jnp.split(gu, 2, axis=-1)
    return (jax.nn.silu(g) * u) @ w_d


def moe_swiglu(h, router, w_gu, w_d):
    logits = jnp.einsum("bld,de->ble", h, router, preferred_element_type=jnp.float32)
    top_v, top_i = lax.top_k(logits, TOP_K)
    top_w = jax.nn.softmax(top_v, axis=-1)
    gates = jnp.sum(jax.nn.one_hot(top_i, N_EXPERTS, dtype=jnp.float32) * top_w[..., None], axis=-2)
    gates = gates.astype(h.dtype)
    y = gates[..., 0:1] * swiglu(h, w_gu[0], w_d[0])
    for e in range(1, N_EXPERTS):
        y = y + gates[..., e:e + 1] * swiglu(h, w_gu[e], w_d[e])
    return y


def channel_mixer(h, i, ffn_w_gu, ffn_w_d, moe_router, moe_w_gu, moe_w_d):
    if i % 2 == 0:
        return swiglu(h, ffn_w_gu[i // 2], ffn_w_d[i // 2])
    j = i // 2
    return moe_swiglu(h, moe_router[j], moe_w_gu[j], moe_w_d[j])


def setup_inputs(seed: int = 0) -> dict:
    key = jax.random.key(seed)
    ks = jax.random.split(key, 24)
    f32 = jnp.float32

    def nrm(k, shape, scale):
        return jax.random.normal(k, shape, f32) * scale

    D = D_MODEL
    return {
        "x": nrm(ks[0], (BATCH, SEQ, D), 1.0),
        "c": nrm(ks[1], (BATCH, D), 1.0),
        "ctx": nrm(ks[2], (BATCH, CTX_LEN, D), 1.0),
        "c_ctx": nrm(ks[3], (D,), 1.0),
        "w_mod": nrm(ks[4], (DEPTH, D, 6 * D), 0.5 * D ** -0.5),
        "b_mod": nrm(ks[5], (DEPTH, 6 * D), 0.01),
        "norm1_g": 1.0 + nrm(ks[6], (DEPTH, D), 0.05),
        "norm2_g": 1.0 + nrm(ks[7], (DEPTH, D), 0.05),
        "w_in": nrm(ks[8], (DEPTH, D, D_IN), D ** -0.5),
        "conv_w": nrm(ks[9], (DEPTH, CONV_K, D_CONV), CONV_K ** -0.5),
        "gmlp_ws": nrm(ks[10], (DEPTH, GMLP_GROUPS, GMLP_CHUNK, GMLP_CHUNK), GMLP_CHUNK ** -0.5),
        "gmlp_b": 1.0 + nrm(ks[11], (DEPTH, GMLP_GROUPS, GMLP_CHUNK), 0.05),
        "attn_sink": nrm(ks[12], (DEPTH, N_HEADS), 0.5),
        "w_br_conv": nrm(ks[13], (DEPTH, D_CONV, D), D_CONV ** -0.5),
        "w_br_gmlp": nrm(ks[14], (DEPTH, D_GMLP, D), D_GMLP ** -0.5),
        "w_br_attn": nrm(ks[15], (DEPTH, N_HEADS * HEAD_DIM, D), (N_HEADS * HEAD_DIM) ** -0.5),
        "w_out": nrm(ks[16], (DEPTH, D, D), D ** -0.5),
        "ffn_w_gu": nrm(ks[17], (N_DENSE, D, 2 * D_FF_DENSE), D ** -0.5),
        "ffn_w_d": nrm(ks[18], (N_DENSE, D_FF_DENSE, D), D_FF_DENSE ** -0.5),
        "moe_router": nrm(ks[19], (N_MOE, D, N_EXPERTS), D ** -0.5),
        "moe_w_gu": nrm(ks[20], (N_MOE, N_EXPERTS, D, 2 * D_FF_EXPERT), D ** -0.5),
        "moe_w_d": nrm(ks[21], (N_MOE, N_EXPERTS, D_FF_EXPERT, D), D_FF_EXPERT ** -0.5),
        "final_norm_g": 1.0 + nrm(ks[22], (D,), 0.05),
    }


def reference(x, c, ctx, c_ctx, w_mod, b_mod, norm1_g, norm2_g, w_in, conv_w, gmlp_ws, gmlp_b,
              attn_sink, w_br_conv, w_br_gmlp, w_br_attn, w_out, ffn_w_gu, ffn_w_d,
              moe_router, moe_w_gu, moe_w_d, final_norm_g):
    cos, sin = rope_tables(x.shape[1])
    cx = ctx
    for i in range(DEPTH):
        last = i == DEPTH - 1
        sh1, sc1, g1, sh2, sc2, g2 = ada_params(c, w_mod[i], b_mod[i], 6)
        if last:
            csh1, csc1 = ada_params(c_ctx, w_mod[i], b_mod[i], 2)
        else:
            csh1, csc1, cg1, csh2, csc2, cg2 = ada_params(c_ctx, w_mod[i], b_mod[i], 6)
        hx = modulate(rms_norm(x, norm1_g[i]), sh1, sc1)
        hc = modulate(rms_norm(cx, norm1_g[i]), csh1, csc1)
        ax, ac = token_mixers(hx, hc, w_in[i], conv_w[i], gmlp_ws[i], gmlp_b[i], attn_sink[i],
                              w_br_conv[i], w_br_gmlp[i], w_br_attn[i], w_out[i], cos, sin,
                              not last)
        x = x + g1 * ax
        hx = modulate(rms_norm(x, norm2_g[i]), sh2, sc2)
        x = x + g2 * channel_mixer(hx, i, ffn_w_gu, ffn_w_d, moe_router, moe_w_gu, moe_w_d)
        if not last:
            cx = cx + cg1 * ac
            hc = modulate(rms_norm(cx, norm2_g[i]), csh2, csc2)
            cx = cx + cg2 * channel_mixer(hc, i, ffn_w_gu, ffn_w_d, moe_router, moe_w_gu, moe_w_d)
    return rms_norm(x, final_norm_g)
```

```python
import numpy as np
import concourse.bass as bass
import concourse.mybir as mybir
from contextlib import ExitStack

F32 = mybir.dt.float32
BF16 = mybir.dt.bfloat16
AF = mybir.ActivationFunctionType
ALU = mybir.AluOpType
AX = mybir.AxisListType


class Buf:
    __slots__ = ("name", "t", "writers", "readers", "dsem")

    def __init__(self, name, t=None):
        self.name = name
        self.t = t
        self.writers = {}
        self.readers = {}
        self.dsem = None

    def __getitem__(self, k):
        return self.t[k]


class MK:
    ENG = ("pe", "act", "dve", "pool", "sp")

    def __init__(self, nc, es):
        self.nc = nc
        self.es = es
        self.h = {"pe": nc.tensor, "act": nc.scalar, "dve": nc.vector,
                  "pool": nc.gpsimd, "sp": nc.sync}
        self.sems = {}
        self.issued = {}
        self.seen = {e: {} for e in self.ENG}
        for e in self.ENG:
            self.sems[e] = nc.alloc_semaphore(name="s_" + e)
            self.issued[e] = 0
        self.ndsem = 0
        self.ninstr = 0
        self.stage_bufs = []
        self.free_dsems = []
        self.dkeys = set()

    def sb(self, name, shape, dt):
        t = self.es.enter_context(self.nc.sbuf_tensor(name, list(shape), dt))
        return Buf(name, t)

    def uname(self, name):
        self.uid = getattr(self, "uid", 0) + 1
        return "%s_u%d" % (name, self.uid)

    def track(self, b):
        self.stage_bufs.append(b)
        return b

    def end_stage(self):
        for b in self.stage_bufs:
            if b.dsem is not None:
                self.free_dsems.append(b.dsem)
                b.dsem = None
        self.stage_bufs = []

    def ps(self, name, shape, dt):
        t = self.es.enter_context(self.nc.psum_tensor(name, list(shape), dt))
        return Buf(name, t)

    def dram(self, name, shape, dt, kind="Internal"):
        t = self.nc.dram_tensor(name, list(shape), dt, kind=kind)
        return Buf(name, t.ap())

    def _dsem(self, b):
        if b.dsem is None:
            if self.free_dsems:
                b.dsem = self.free_dsems.pop()
                return b.dsem
            k = "q%d" % self.ndsem
            self.ndsem += 1
            self.sems[k] = self.nc.alloc_semaphore(name="s_" + k)
            self.issued[k] = 0
            self.dkeys.add(k)
            b.dsem = k
        return b.dsem

    def _need(self, eng, reads, writes):
        need = {}

        def add(k, c, kind):
            if k == eng:
                if eng == "pe":
                    return
                if kind == "war":
                    return
            if c > need.get(k, 0):
                need[k] = c

        for b in reads:
            for k, c in b.writers.items():
                add(k, c, "raw")
        for b in writes:
            for k, c in b.writers.items():
                add(k, c, "waw")
            for k, c in b.readers.items():
                add(k, c, "war")
        seen = self.seen[eng]
        hnd = self.h[eng]
        for k, c in need.items():
            if seen.get(k, 0) >= c:
                continue
            if k in self.dkeys:
                c = max(c, self.issued[k])
            hnd.wait_ge(self.sems[k], c)
            seen[k] = c

    def _mark(self, key, cnt, reads, writes):
        for b in writes:
            b.writers = {key: cnt}
            b.readers = {}
        for b in reads:
            if b not in writes:
                b.readers[key] = cnt

    def op(self, eng, fn, reads=(), writes=()):
        self._need(eng, reads, writes)
        ins = fn(self.h[eng])
        self.issued[eng] += 1
        ins.then_inc(self.sems[eng], 1)
        self._mark(eng, self.issued[eng], reads, writes)
        self.ninstr += 1
        return ins

    def dma(self, q, out, in_, reads=(), writes=(), sembuf=None, **kw):
        self._need(q, reads, writes)
        k = self._dsem(sembuf)
        ins = self.h[q].dma_start(out=out, in_=in_, **kw)
        self.issued[k] += 16
        ins.then_inc(self.sems[k], 16)
        self._mark(k, self.issued[k], reads, writes)
        self.ninstr += 1
        return ins

    def wait_all(self, eng, bufs):
        self._need(eng, bufs, ())

from concourse.bass_utils import run_bass_kernel_spmd

D = 1024
KC = 8
LC = 256
NH = 8
E = 8
FD = 2816
FE = 3584
EPS = 1e-6
SCALE = 0.125
NEG = -1e30


class Cfg:
    def __init__(self, NB=2, S=4096, L=4, dbg=False, stop=None):
        self.NB, self.S, self.L, self.dbg, self.stop = NB, S, L, dbg, stop
        self.R = NB + 1
        self.CT = NB * LC
        self.TX = NB * S
        self.TA = self.CT + self.TX


def rope_tabs(cfg):
    S = cfg.S
    rows = S // 64
    row = np.repeat(np.arange(rows), 64).astype(np.float32)
    col = np.tile(np.arange(64), rows).astype(np.float32)
    half = 32
    inv = (1.0 / (10000.0 ** (np.arange(0, half, 2, dtype=np.float32) / half))).astype(np.float32)
    ang_r = row[:, None] * inv[None, :]
    ang_c = col[:, None] * inv[None, :]
    ang = np.concatenate([ang_r, ang_r, ang_c, ang_c], axis=-1)
    cos = np.cos(ang).astype(np.float32).T
    sin = np.sin(ang).astype(np.float32).T
    sgn = np.where((np.arange(64) % 32) < 16, -1.0, 1.0).astype(np.float32)[:, None]
    sins = sin * sgn
    tc = np.ones((128, cfg.TA), np.float32)
    ts_ = np.zeros((128, cfg.TA), np.float32)
    for b in range(cfg.NB):
        o = cfg.CT + b * S
        tc[:, o:o + S] = np.concatenate([cos, cos], 0)
        ts_[:, o:o + S] = np.concatenate([sins, sins], 0)
    return tc, ts_


def rot_src():
    j = np.arange(64)
    return (j // 32) * 32 + ((j % 32) + 16) % 32


def build(cfg):
    NB, S, L, R, CT, TX, TA = cfg.NB, cfg.S, cfg.L, cfg.R, cfg.CT, cfg.TX, cfg.TA
    nc = bass.Bass("TRN2", target_bir_lowering=False)
    ges = ExitStack()
    m = MK(nc, ges)
    EI = "ExternalInput"

    x_in = m.dram("x_in", [TX, D], F32, EI)
    c_in = m.dram("c_in", [CT, D], F32, EI)
    cT_in = m.dram("cT", [128, KC, R], F32, EI)
    w_mod = m.dram("w_mod", [L, D, 6 * D], F32, EI)
    bmodT = m.dram("bmodT", [L, 128, 48], F32, EI)
    n1T = m.dram("n1T", [L, 128, KC], F32, EI)
    n2T = m.dram("n2T", [L, 128, KC], F32, EI)
    fnT = m.dram("fnT", [128, KC], F32, EI)
    w_in = m.dram("w_in", [L, D, 5120], F32, EI)
    w_ex = m.dram("w_ex", [L, D, 1024], F32, EI)
    convT = m.dram("convT", [L, 128, 2, 3], F32, EI)
    wsT_in = m.dram("wsT", [L, 128, 4, 128], F32, EI)
    gb_in = m.dram("gb", [L, 128, 2, 128], F32, EI)
    sink_in = m.dram("sinkbc", [L, 128, NH], F32, EI)
    w_br = m.dram("w_br", [L, D, D], F32, EI)
    w_out = m.dram("w_out", [L, D, D], F32, EI)
    ND = (L + 1) // 2
    NM = max(L // 2, 1)
    ffn_gu = m.dram("ffn_gu", [ND, D, 2 * FD], F32, EI)
    ffn_d = m.dram("ffn_d", [ND, FD, D], F32, EI)
    moe_rt = m.dram("moe_rt", [NM, D, E], F32, EI)
    moe_gu = m.dram("moe_gu", [NM, E, D, 2 * FE], F32, EI)
    moe_d = m.dram("moe_d", [NM, E, FE, D], F32, EI)
    tabC = m.dram("tabC", [128, TA], F32, EI)
    tabS = m.dram("tabS", [128, TA], F32, EI)
    mask_in = m.dram("mask", [128, 384], F32, EI)
    out = m.dram("out", [TX, D], F32, "ExternalOutput")

    OK = "ExternalOutput" if cfg.dbg else "Internal"
    XR = [m.dram("XR%d" % i, [KC, 128, TA], F32, OK) for i in range(2)]
    H1 = m.dram("H1", [KC, 128, TA], BF16, OK)
    BGd = m.dram("BGd", [2, 128, TA], BF16, OK)
    CHd = m.dram("CHd", [2, 128, TA], BF16, OK)
    UGd = m.dram("UGd", [2, 128, TA], BF16, OK)
    Qd = m.dram("Qd", [4, 128, TA], BF16, OK)
    KKd = m.dram("KKd", [2, 128, TA], BF16, OK)
    VNXd = m.dram("VNXd", [TA, 512], BF16, OK)
    VAXd = m.dram("VAXd", [TA, 512], BF16, OK)
    Yd = m.dram("Yd", [KC, 128, TA], BF16, OK)
    MIXd = m.dram("MIXd", [KC, 128, TA], BF16, OK)
    H2d = m.dram("H2d", [KC, 128, TA], BF16, OK)
    GTd = m.dram("GTd", [E, TA], BF16, OK)
    Wb_in = [m.dram("Wb_in%d" % l, [D, 6144], BF16) for l in range(L)]
    Wb_br = [m.dram("Wb_br%d" % l, [D, D], BF16) for l in range(L)]
    Wb_out = [m.dram("Wb_out%d" % l, [D, D], BF16) for l in range(L)]
    Wb_gu, Wb_d = [], []
    for l in range(L):
        if l % 2 == 0:
            Wb_gu.append(m.dram("Wb_gu%d" % l, [1, D, 2 * FD], BF16))
            Wb_d.append(m.dram("Wb_d%d" % l, [1, FD, D], BF16))
        else:
            Wb_gu.append(m.dram("Wb_gu%d" % l, [E, D, 2 * FE], BF16))
            Wb_d.append(m.dram("Wb_d%d" % l, [E, FE, D], BF16))

    ident = m.sb("ident", [128, 128], F32)
    ones_bf = m.sb("ones_bf", [128, 128], BF16)
    epsc = m.sb("epsc", [128, 1], F32)
    m.op("pool", lambda e: e.memset(ident[:], 0.0), writes=[ident])
    m.op("pool", lambda e: e.affine_select(out=ident[:], in_=ident[:], pattern=[[-1, 128]],
                                            compare_op=ALU.not_equal, fill=1.0, base=0,
                                            channel_multiplier=1), reads=[ident], writes=[ident])
    m.op("pool", lambda e: e.memset(ones_bf[:], 1.0), writes=[ones_bf])
    m.op("pool", lambda e: e.memset(epsc[:], EPS), writes=[epsc])
    PS = [m.ps("ps%d" % i, [128, 512], F32) for i in range(8)]
    modT = m.sb("modT", [128, R, 48], F32)
    A1 = m.sb("A1", [128, R, KC], F32)
    A2 = m.sb("A2", [128, R, KC], F32)
    csil = m.sb("csil", [128, KC, R], F32)
    cst = m.sb("cst", [128, KC, R], F32)
    bmod = m.sb("bmod", [128, 48], F32)
    n1 = m.sb("n1", [128, KC], F32)
    n2 = m.sb("n2", [128, KC], F32)

    state = {"psi": 0}

    def nps():
        p = PS[state["psi"] % 8]
        state["psi"] += 1
        return p

    class Ring:
        def __init__(self, name, n, shape, dt, es=None):
            self.slots = []
            for i in range(n):
                nm = m.uname("%s_%d" % (name, i))
                t = (es or ges).enter_context(nc.sbuf_tensor(nm, list(shape), dt))
                self.slots.append(m.track(Buf(nm, t)))
            self.i = 0

        def next(self):
            s = self.slots[self.i % len(self.slots)]
            self.i += 1
            return s

    def stage_sb(es, name, shape, dt):
        nm = m.uname(name)
        t = es.enter_context(nc.sbuf_tensor(nm, list(shape), dt))
        return m.track(Buf(nm, t))

    nobar = set()

    def barrier():
        for e in MK.ENG:
            for k in list(m.sems.keys()):
                if k == e or k in nobar:
                    continue
                c = m.issued[k]
                if c > m.seen[e].get(k, 0):
                    m.h[e].wait_ge(m.sems[k], c)
                    m.seen[e][k] = c
        m.end_stage()

    cvb = Buf("cvsem")

    def conv_w(dst, dst_ap, src, src_ap):
        m.dma("pool", dst_ap, src_ap, reads=[src], writes=[dst], sembuf=dst)
        nobar.add(dst.dsem)

    def v2(ap, rows):
        return ap.rearrange("(p r) n -> p (r n)", p=128)

    def convert_layer(l):
        conv_w(Wb_in[l], Wb_in[l][:, 0:5120], w_in, w_in[l])
        conv_w(Wb_in[l], Wb_in[l][:, 5120:6144], w_ex, w_ex[l])
        conv_w(Wb_br[l], v2(Wb_br[l][:], D), w_br, v2(w_br[l], D))
        conv_w(Wb_out[l], v2(Wb_out[l][:], D), w_out, v2(w_out[l], D))
        if l % 2 == 0:
            conv_w(Wb_gu[l], v2(Wb_gu[l][0], D), ffn_gu, v2(ffn_gu[l // 2], D))
            conv_w(Wb_d[l], v2(Wb_d[l][0], FD), ffn_d, v2(ffn_d[l // 2], FD))
        else:
            for e in range(E):
                conv_w(Wb_gu[l], v2(Wb_gu[l][e], D), moe_gu, v2(moe_gu[l // 2, e], D))
                conv_w(Wb_d[l], v2(Wb_d[l][e], FE), moe_d, v2(moe_d[l // 2, e], FE))

    def tiles512():
        tl = [(0, CT, R - 1)] if CT <= 512 else [(i * 512, 512, R - 1) for i in range(CT // 512)]
        for b in range(NB):
            for i in range(S // 512):
                tl.append((CT + b * S + i * 512, 512, b))
        return tl

    def fm(dr, t0, ts, k0=0, k1=None):
        k1 = dr.t.shape[0] if k1 is None else k1
        return dr.t[k0:k1, :, t0:t0 + ts].rearrange("k p t -> p k t")

    def stage0():
        with ExitStack() as es:
            rin = Ring("s0in", 2, [128, 4, D], F32, es)
            rout = Ring("s0out", 2, [128, KC, 512], F32, es)
            srcs = [(c_in, i * 512, min(512, CT - i * 512), i * 512) for i in range((CT + 511) // 512)]
            srcs += [(x_in, i * 512, 512, CT + i * 512) for i in range(TX // 512)]
            for (src, r0, ts, t0) in srcs:
                nb = ts // 128
                it = rin.next()
                m.dma("sp", it[:, 0:nb, :], src.t[r0:r0 + ts, :].rearrange("(b p) d -> p b d", p=128),
                      reads=[src], writes=[it], sembuf=it)
                ot = rout.next()
                for blk in range(nb):
                    for kh in range(2):
                        p = nps()
                        for kk in range(4):
                            k = kh * 4 + kk
                            m.op("pe", lambda e, p=p, kk=kk, k=k, blk=blk: e.transpose(
                                p[:, kk * 128:(kk + 1) * 128], it[:, blk, k * 128:(k + 1) * 128], ident[:]),
                                reads=[it, ident], writes=[p])
                        eng = "act" if (blk + kh) % 2 == 0 else "dve"
                        src_v = p[:].rearrange("p (k t) -> p k t", k=4)
                        dst_v = ot[:, kh * 4:(kh + 1) * 4, blk * 128:(blk + 1) * 128]
                        if eng == "act":
                            m.op("act", lambda e, a=dst_v, b=src_v: e.activation(out=a, in_=b, func=AF.Copy),
                                 reads=[p], writes=[ot])
                        else:
                            m.op("dve", lambda e, a=dst_v, b=src_v: e.tensor_copy(out=a, in_=b),
                                 reads=[p], writes=[ot])
                m.dma("act", fm(XR[0], t0, ts), ot[:, :, 0:ts], reads=[ot], writes=[XR[0]], sembuf=ot)
        barrier()

    def ada(l, first):
        with ExitStack() as es:
            rw = Ring("adaw", 2, [128, KC, 512], F32, es)
            if first:
                m.dma("sp", cst[:], cT_in[:], reads=[cT_in], writes=[cst], sembuf=cst)
                m.op("act", lambda e: e.activation(out=csil[:], in_=cst[:], func=AF.Silu),
                     reads=[cst], writes=[csil])
            m.dma("sp", bmod[:], bmodT[l], reads=[bmodT], writes=[bmod], sembuf=bmod)
            m.dma("sp", n1[:], n1T[l], reads=[n1T], writes=[n1], sembuf=n1)
            m.dma("sp", n2[:], n2T[l], reads=[n2T], writes=[n2], sembuf=n2)
            for pc in range(12):
                wt = rw.next()
                m.dma("sp", wt[:], w_mod.t[l, :, pc * 512:(pc + 1) * 512].rearrange("(k p) n -> p k n", p=128),
                      reads=[w_mod], writes=[wt], sembuf=wt)
                p = nps()
                for jj in range(4):
                    for k in range(KC):
                        m.op("pe", lambda e, p=p, jj=jj, k=k: e.matmul(
                            p[:, jj * R:(jj + 1) * R], lhsT=wt[:, k, jj * 128:(jj + 1) * 128], rhs=csil[:, k, :],
                            start=(k == 0), stop=(k == KC - 1)), reads=[wt, csil], writes=[p])
                for jj in range(4):
                    ch = pc * 4 + jj
                    m.op("act", lambda e, p=p, jj=jj, ch=ch: e.activation(
                        out=modT[:, :, ch], in_=p[:, jj * R:(jj + 1) * R], func=AF.Identity,
                        bias=bmod[:, ch:ch + 1], scale=1.0), reads=[p, bmod], writes=[modT])
            for r in range(R):
                m.op("dve", lambda e, r=r: e.scalar_tensor_tensor(
                    out=A1[:, r, :], in0=modT[:, r, 8:16], scalar=1.0, in1=n1[:], op0=ALU.add, op1=ALU.mult),
                    reads=[modT, n1], writes=[A1])
                m.op("dve", lambda e, r=r: e.scalar_tensor_tensor(
                    out=A2[:, r, :], in0=modT[:, r, 32:40], scalar=1.0, in1=n2[:], op0=ALU.add, op1=ALU.mult),
                    reads=[modT, n2], writes=[A2])
        barrier()

    def norm_mod(xt, ts, Aap, Bap, sqb, rt, rstd, tmp, hdst, hf=None):
        m.op("act", lambda e: e.activation(out=sqb[:, :, 0:ts], in_=xt[:, :, 0:ts], func=AF.Square),
             reads=[xt], writes=[sqb])
        p = nps()
        for k in range(KC):
            m.op("pe", lambda e, k=k: e.matmul(p[:, 0:ts], lhsT=ones_bf[:], rhs=sqb[:, k, 0:ts],
                                               start=(k == 0), stop=(k == KC - 1)),
                 reads=[ones_bf, sqb], writes=[p])
        m.op("act", lambda e: e.activation(out=rt[:, 0:ts], in_=p[:, 0:ts], func=AF.Sqrt,
                                           bias=epsc[:], scale=1.0 / D), reads=[p, epsc], writes=[rt])
        m.op("dve", lambda e: e.reciprocal(out=rstd[:, 0:ts], in_=rt[:, 0:ts]), reads=[rt], writes=[rstd])
        for k in range(KC):
            m.op("dve", lambda e, k=k: e.scalar_tensor_tensor(
                out=tmp[:, k, 0:ts], in0=xt[:, k, 0:ts], scalar=Aap(k), in1=rstd[:, 0:ts],
                op0=ALU.mult, op1=ALU.mult), reads=[xt, rstd, A1, A2], writes=[tmp])
            if hf is None:
                m.op("act", lambda e, k=k: e.activation(out=hdst[:, k, 0:ts], in_=tmp[:, k, 0:ts],
                                                        func=AF.Identity, bias=Bap(k), scale=1.0),
                     reads=[tmp, modT], writes=[hdst])
            else:
                m.op("act", lambda e, k=k: e.activation(out=hf[:, k, 0:ts], in_=tmp[:, k, 0:ts],
                                                        func=AF.Identity, bias=Bap(k), scale=1.0),
                     reads=[tmp, modT], writes=[hf])
        if hf is not None:
            m.op("pool", lambda e: e.tensor_copy(out=hdst[:, :, 0:ts], in_=hf[:, :, 0:ts]),
                 reads=[hf], writes=[hdst])

    def stage1(l, xr):
        with ExitStack() as es:
            w1 = stage_sb(es, "w1", [128, KC, 3072], BF16)
            wv = Wb_in[l].t.rearrange("(k p) n -> p k n", p=128)
            m.dma("sp", w1[:, :, 0:2048], wv[:, :, 0:2048], reads=[Wb_in[l]], writes=[w1], sembuf=w1)
            m.dma("sp", w1[:, :, 2048:3072], wv[:, :, 5120:6144], reads=[Wb_in[l]], writes=[w1], sembuf=w1)
            rx = Ring("s1x", 2, [128, KC, 512], F32, es)
            rtab = Ring("s1tab", 2, [128, 2, 512], F32, es)
            sqb = stage_sb(es, "s1sq", [128, KC, 512], BF16)
            rt = stage_sb(es, "s1rt", [128, 512], F32)
            rstd = stage_sb(es, "s1rstd", [128, 512], F32)
            tmp = stage_sb(es, "s1tmp", [128, KC, 512], F32)
            rh = Ring("s1h", 2, [128, KC, 512], BF16, es)
            rfm = Ring("s1fm", 2, [128, 12, 512], BF16, es)
            cg = Ring("s1cg", 2, [128, 512], BF16, es)
            t1r = Ring("s1t1", 2, [128, 512], F32, es)
            t2r = Ring("s1t2", 2, [128, 512], F32, es)
            rvn = Ring("s1vn", 2, [128, 4, 512], BF16, es)
            rva = Ring("s1va", 2, [128, 4, 512], BF16, es)
            vg = Ring("s1vg", 2, [128, 256], F32, es)
            st6 = Ring("s1st", 2, [128, 6], F32, es)
            mv = Ring("s1mv", 2, [128, 2], F32, es)
            sd = Ring("s1sd", 2, [128, 1], F32, es)
            rs = Ring("s1rs", 2, [128, 1], F32, es)
            for s_ in rvn.slots + rva.slots:
                m.op("pool", lambda e, s_=s_: e.memset(s_[:], 0.0), writes=[s_])
            for (t0, ts, r) in tiles512():
                nb = ts // 128
                xt = rx.next()
                m.dma("sp", xt[:, :, 0:ts], fm(xr, t0, ts), reads=[xr], writes=[xt], sembuf=xt)
                tb = rtab.next()
                m.dma("sp", tb[:, 0, 0:ts], tabC[:, t0:t0 + ts], reads=[tabC], writes=[tb], sembuf=tb)
                m.dma("sp", tb[:, 1, 0:ts], tabS[:, t0:t0 + ts], reads=[tabS], writes=[tb], sembuf=tb)
                h = rh.next()
                norm_mod(xt, ts, lambda k: A1[:, r, k:k + 1], lambda k: modT[:, r, k:k + 1],
                         sqb, rt, rstd, tmp, h)
                m.dma("act", fm(H1, t0, ts), h[:, :, 0:ts], reads=[h], writes=[H1], sembuf=h)
                f = rfm.next()

                def proj(co):
                    p = nps()
                    for k in range(KC):
                        m.op("pe", lambda e, k=k: e.matmul(p[:, 0:ts], lhsT=w1[:, k, co:co + 128],
                                                           rhs=h[:, k, 0:ts], start=(k == 0), stop=(k == KC - 1)),
                             reads=[w1, h], writes=[p])
                    return p
                for j in range(2):
                    p = proj(0 + j * 128)
                    m.op("act", lambda e, p=p, j=j: e.activation(out=f[:, 0 + j, 0:ts], in_=p[:, 0:ts], func=AF.Copy),
                         reads=[p], writes=[f])
                for j in range(2):
                    p = proj(256 + j * 128)
                    c_ = cg.next()
                    m.op("act", lambda e, p=p, c_=c_: e.activation(out=c_[:, 0:ts], in_=p[:, 0:ts], func=AF.Copy),
                         reads=[p], writes=[c_])
                    p2 = proj(512 + j * 128)
                    m.op("dve", lambda e, p2=p2, c_=c_, j=j: e.tensor_tensor(
                        out=f[:, 2 + j, 0:ts], in0=p2[:, 0:ts], in1=c_[:, 0:ts], op=ALU.mult),
                        reads=[p2, c_], writes=[f])
                for j in range(2):
                    p = proj(768 + j * 128)
                    m.op("act", lambda e, p=p, j=j: e.activation(out=f[:, 4 + j, 0:ts], in_=p[:, 0:ts],
                                                                 func=AF.Gelu_apprx_tanh), reads=[p], writes=[f])
                for (co, cop, fo, n) in ((1280, 2048, 6, 4), (2560, 2816, 10, 2)):
                    for j in range(n):
                        p = proj(co + j * 128)
                        pp = proj(cop + j * 128)
                        a1 = t1r.next()
                        a2 = t2r.next()
                        m.op("dve", lambda e, p=p, a1=a1: e.tensor_tensor(
                            out=a1[:, 0:ts], in0=p[:, 0:ts], in1=tb[:, 0, 0:ts], op=ALU.mult),
                            reads=[p, tb], writes=[a1])
                        m.op("dve", lambda e, pp=pp, a2=a2: e.tensor_tensor(
                            out=a2[:, 0:ts], in0=pp[:, 0:ts], in1=tb[:, 1, 0:ts], op=ALU.mult),
                            reads=[pp, tb], writes=[a2])
                        m.op("pool", lambda e, a1=a1, a2=a2, fo=fo, j=j: e.tensor_tensor(
                            out=f[:, fo + j, 0:ts], in0=a1[:, 0:ts], in1=a2[:, 0:ts], op=ALU.add),
                            reads=[a1, a2], writes=[f])
                m.dma("act", fm(BGd, t0, ts), f[:, 0:2, 0:ts], reads=[f], writes=[BGd], sembuf=f)
                m.dma("act", fm(CHd, t0, ts), f[:, 2:4, 0:ts], reads=[f], writes=[CHd], sembuf=f)
                m.dma("act", fm(UGd, t0, ts), f[:, 4:6, 0:ts], reads=[f], writes=[UGd], sembuf=f)
                m.dma("act", fm(Qd, t0, ts), f[:, 6:10, 0:ts], reads=[f], writes=[Qd], sembuf=f)
                m.dma("act", fm(KKd, t0, ts), f[:, 10:12, 0:ts], reads=[f], writes=[KKd], sembuf=f)
                vn = rvn.next()
                va = rva.next()
                for blk in range(nb):
                    pv = nps()
                    for k in range(KC):
                        m.op("pe", lambda e, k=k, pv=pv, blk=blk: e.matmul(
                            pv[:, 0:256], lhsT=h[:, k, blk * 128:(blk + 1) * 128], rhs=w1[:, k, 1024:1280],
                            start=(k == 0), stop=(k == KC - 1)), reads=[w1, h], writes=[pv])
                    pa = nps()
                    for k in range(KC):
                        m.op("pe", lambda e, k=k, pa=pa, blk=blk: e.matmul(
                            pa[:, 0:128], lhsT=h[:, k, blk * 128:(blk + 1) * 128], rhs=w1[:, k, 1920:2048],
                            start=(k == 0), stop=(k == KC - 1)), reads=[w1, h], writes=[pa])
                    g_ = vg.next()
                    m.op("act", lambda e, pv=pv, g_=g_: e.activation(out=g_[:], in_=pv[:, 0:256],
                                                                     func=AF.Gelu_apprx_tanh),
                         reads=[pv], writes=[g_])
                    s6 = st6.next()
                    m.op("dve", lambda e, g_=g_, s6=s6: e.bn_stats(out=s6[:], in_=g_[:]), reads=[g_], writes=[s6])
                    mv_ = mv.next()
                    m.op("dve", lambda e, mv_=mv_, s6=s6: e.bn_aggr(out=mv_[:], in_=s6[:]), reads=[s6], writes=[mv_])
                    sd_ = sd.next()
                    m.op("act", lambda e, sd_=sd_, mv_=mv_: e.activation(out=sd_[:], in_=mv_[:, 1:2], func=AF.Sqrt,
                                                                         bias=epsc[:], scale=1.0),
                         reads=[mv_, epsc], writes=[sd_])
                    rs_ = rs.next()
                    m.op("dve", lambda e, sd_=sd_, rs_=rs_: e.reciprocal(out=rs_[:], in_=sd_[:]),
                         reads=[sd_], writes=[rs_])
                    for par in range(2):
                        src = g_[:].rearrange("p (g c) -> p g c", c=64)[:, par::2, :]
                        dst = vn[:, blk, :].rearrange("p (g c) -> p g c", c=128)[:, par::2, par * 64:(par + 1) * 64]
                        m.op("dve", lambda e, src=src, dst=dst, mv_=mv_, rs_=rs_: e.tensor_scalar(
                            out=dst, in0=src, scalar1=mv_[:, 0:1], scalar2=rs_[:, 0:1],
                            op0=ALU.subtract, op1=ALU.mult), reads=[g_, mv_, rs_], writes=[vn])
                        srca = pa[:, 0:128].rearrange("p (kv c) -> p kv c", c=64)
                        dsta = va[:, blk, :].rearrange("p (kv q c) -> p kv q c", kv=2, q=2)[:, :, par, par * 64:(par + 1) * 64]
                        m.op("act", lambda e, srca=srca, dsta=dsta: e.activation(out=dsta, in_=srca, func=AF.Copy),
                             reads=[pa], writes=[va])
                m.dma("act", VNXd.t[t0:t0 + ts, :].rearrange("(b p) c -> p b c", p=128), vn[:, 0:nb, :],
                      reads=[vn], writes=[VNXd], sembuf=vn)
                m.dma("act", VAXd.t[t0:t0 + ts, :].rearrange("(b p) c -> p b c", p=128), va[:, 0:nb, :],
                      reads=[va], writes=[VAXd], sembuf=va)
        barrier()

    def stage2(l, last):
        with ExitStack() as es:
            cw = stage_sb(es, "s2cw", [128, 2, 3], F32)
            wsf = stage_sb(es, "s2wsf", [128, 4, 128], F32)
            wsb = stage_sb(es, "s2wsb", [128, 4, 128], BF16)
            gbt = stage_sb(es, "s2gb", [128, 2, 128], F32)
            snk = stage_sb(es, "s2snk", [128, NH], F32)
            nsnk = stage_sb(es, "s2nsnk", [128, NH], F32)
            msk = stage_sb(es, "s2msk", [128, 384], F32)
            m.dma("sp", cw[:], convT[l], reads=[convT], writes=[cw], sembuf=cw)
            m.dma("sp", wsf[:], wsT_in[l], reads=[wsT_in], writes=[wsf], sembuf=wsf)
            m.dma("sp", gbt[:], gb_in[l], reads=[gb_in], writes=[gbt], sembuf=gbt)
            m.dma("sp", snk[:], sink_in[l], reads=[sink_in], writes=[snk], sembuf=snk)
            m.dma("sp", msk[:], mask_in[:], reads=[mask_in], writes=[msk], sembuf=msk)
            m.op("dve", lambda e: e.tensor_copy(out=wsb[:], in_=wsf[:]), reads=[wsf], writes=[wsb])
            m.op("dve", lambda e: e.tensor_scalar(out=nsnk[:], in0=snk[:], scalar1=-1.0, scalar2=None,
                                                  op0=ALU.mult), reads=[snk], writes=[nsnk])
            kkc = stage_sb(es, "s2kkc", [128, 2, LC], BF16)
            vaxc = stage_sb(es, "s2vaxc", [128, 2, 512], BF16)
            rch = Ring("s2ch", 2, [128, 2, 514], BF16, es)
            rbg = Ring("s2bg", 2, [128, 2, 512], BF16, es)
            rug = Ring("s2ug", 2, [128, 2, 512], BF16, es)
            rvn = Ring("s2vn", 2, [128, 4, 512], BF16, es)
            rq = Ring("s2q", 2, [128, 4, 512], BF16, es)
            rkk = Ring("s2kk", 2, [128, 2, 768], BF16, es)
            rvx = Ring("s2vx", 2, [128, 6, 512], BF16, es)
            ry = Ring("s2y", 2, [128, KC, 512], BF16, es)
            acc = Ring("s2acc", 2, [128, 512], F32, es)
            gt = Ring("s2gt", 2, [128, 2, 128], F32, es)
            sm = Ring("s2sm", 2, [128, 640], F32, es)
            pe_ = Ring("s2pe", 2, [128, 640], F32, es)
            pn = Ring("s2pn", 2, [128, 640], F32, es)
            pT = Ring("s2pT", 2, [128, 5, 128], BF16, es)
            sc = [Ring("s2sc%d" % i, 2, [128, 1], F32, es) for i in range(6)]
            pso = [0]
            for b in range(NB):
                m.dma("sp", kkc[:], fm(KKd, b * LC, LC), reads=[KKd], writes=[kkc], sembuf=kkc)
                m.dma("sp", vaxc[:], VAXd.t[b * LC:(b + 1) * LC, :].rearrange("(b p) c -> p b c", p=128),
                      reads=[VAXd], writes=[vaxc], sembuf=vaxc)
                tl = []
                if not last:
                    tl.append((b * LC, LC, 0, LC, True))
                for i in range(S // 512):
                    tl.append((CT + b * S + i * 512, 512, i * 512, S, False))
                for (t0, ts, s0, slen, isctx) in tl:
                    nb = ts // 128
                    hl = 1 if s0 > 0 else 0
                    hr = 1 if s0 + ts < slen else 0
                    ch = rch.next()
                    if not hl:
                        m.op("pool", lambda e, ch=ch: e.memset(ch[:, :, 0:1], 0.0), writes=[ch])
                    if not hr:
                        m.op("pool", lambda e, ch=ch: e.memset(ch[:, :, ts + 1:ts + 2], 0.0), writes=[ch])
                    m.dma("sp", ch[:, :, 1 - hl:ts + 1 + hr], fm(CHd, t0 - hl, ts + hl + hr),
                          reads=[CHd], writes=[ch], sembuf=ch)
                    bg = rbg.next()
                    m.dma("sp", bg[:, :, 0:ts], fm(BGd, t0, ts), reads=[BGd], writes=[bg], sembuf=bg)
                    ug = rug.next()
                    m.dma("sp", ug[:, :, 0:ts], fm(UGd, t0, ts), reads=[UGd], writes=[ug], sembuf=ug)
                    vn = rvn.next()
                    m.dma("sp", vn[:, 0:nb, :], VNXd.t[t0:t0 + ts, :].rearrange("(b p) c -> p b c", p=128),
                          reads=[VNXd], writes=[vn], sembuf=vn)
                    q = rq.next()
                    m.dma("sp", q[:, :, 0:ts], fm(Qd, t0, ts), reads=[Qd], writes=[q], sembuf=q)
                    kk = vx = None
                    if not isctx:
                        kl = 128 if s0 > 0 else 0
                        kr = 128 if s0 + ts < slen else 0
                        kk = rkk.next()
                        m.dma("sp", kk[:, :, 128 - kl:128 + ts + kr], fm(KKd, t0 - kl, ts + kl + kr),
                              reads=[KKd], writes=[kk], sembuf=kk)
                        vx = rvx.next()
                        nbl = (kl + ts + kr) // 128
                        b0 = 1 - kl // 128
                        m.dma("sp", vx[:, b0:b0 + nbl, :],
                              VAXd.t[t0 - kl:t0 + ts + kr, :].rearrange("(b p) c -> p b c", p=128),
                              reads=[VAXd], writes=[vx], sembuf=vx)
                    y = ry.next()
                    for j in range(2):
                        a = acc.next()
                        m.op("dve", lambda e, a=a, j=j: e.tensor_scalar(
                            out=a[:, 0:ts], in0=ch[:, j, 1:ts + 1], scalar1=cw[:, j, 1:2], scalar2=None,
                            op0=ALU.mult), reads=[ch, cw], writes=[a])
                        m.op("dve", lambda e, a=a, j=j: e.scalar_tensor_tensor(
                            out=a[:, 0:ts], in0=ch[:, j, 0:ts], scalar=cw[:, j, 0:1], in1=a[:, 0:ts],
                            op0=ALU.mult, op1=ALU.add), reads=[ch, cw, a], writes=[a])
                        m.op("dve", lambda e, a=a, j=j: e.scalar_tensor_tensor(
                            out=a[:, 0:ts], in0=ch[:, j, 2:ts + 2], scalar=cw[:, j, 2:3], in1=a[:, 0:ts],
                            op0=ALU.mult, op1=ALU.add), reads=[ch, cw, a], writes=[a])
                        m.op("pool", lambda e, a=a, j=j: e.tensor_tensor(
                            out=y[:, j, 0:ts], in0=a[:, 0:ts], in1=bg[:, j, 0:ts], op=ALU.mult),
                            reads=[a, bg], writes=[y])
                    for blk in range(nb):
                        p = nps()
                        for j in range(2):
                            for gg in range(2):
                                g = 2 * j + gg
                                m.op("pe", lambda e, p=p, j=j, g=g, gg=gg, blk=blk: e.matmul(
                                    p[:, j * 128:(j + 1) * 128], lhsT=vn[:, blk, g * 128:(g + 1) * 128],
                                    rhs=wsb[:, g, :], start=(gg == 0), stop=(gg == 1)),
                                    reads=[vn, wsb], writes=[p])
                        g_ = gt.next()
                        m.op("dve", lambda e, p=p, g_=g_: e.tensor_tensor(
                            out=g_[:], in0=p[:, 0:256].rearrange("p (j t) -> p j t", j=2), in1=gbt[:],
                            op=ALU.add), reads=[p, gbt], writes=[g_])
                        m.op("pool", lambda e, g_=g_, blk=blk: e.tensor_tensor(
                            out=y[:, 2:4, blk * 128:(blk + 1) * 128], in0=g_[:],
                            in1=ug[:, :, blk * 128:(blk + 1) * 128], op=ALU.mult),
                            reads=[g_, ug], writes=[y])
                    for blk in range(nb):
                        nbk = (s0 // 128) + blk
                        if isctx:
                            lo = hi = 384
                        else:
                            lo = 128 if nbk == 0 else 0
                            hi = 256 if nbk == slen // 128 - 1 else 384
                        for c in range(4):
                            pO = PS[6 + (pso[0] % 2)]
                            pso[0] += 1
                            first = True
                            for par in range(2):
                                hh = 2 * c + par
                                kv = hh // 4
                                pb = par * 64
                                pA = PS[hh % 2]
                                pB = PS[2 + hh % 2]
                                if hi > lo:
                                    m.op("pe", lambda e, pA=pA, pb=pb, c=c, kv=kv, blk=blk, lo=lo, hi=hi: e.matmul(
                                        pA[:, lo:hi], lhsT=q[pb:pb + 64, c, blk * 128:(blk + 1) * 128],
                                        rhs=kk[pb:pb + 64, kv, blk * 128 + lo:blk * 128 + hi], start=True, stop=True),
                                        reads=[q, kk], writes=[pA])
                                m.op("pe", lambda e, pB=pB, pb=pb, c=c, kv=kv, blk=blk: e.matmul(
                                    pB[:, 0:LC], lhsT=q[pb:pb + 64, c, blk * 128:(blk + 1) * 128],
                                    rhs=kkc[pb:pb + 64, kv, :], start=True, stop=True),
                                    reads=[q, kkc], writes=[pB])
                                s_ = sm.next()
                                if hi > lo:
                                    m.op("dve", lambda e, s_=s_, pA=pA, lo=lo, hi=hi: e.tensor_tensor(
                                        out=s_[:, lo:hi], in0=pA[:, lo:hi], in1=msk[:, lo:hi], op=ALU.add),
                                        reads=[pA, msk], writes=[s_])
                                if lo < hi < 384:
                                    m.op("pool", lambda e, s_=s_, hi=hi: e.memset(s_[:, hi:384], NEG), writes=[s_])
                                m.op("act", lambda e, s_=s_, pB=pB: e.activation(
                                    out=s_[:, 384:640], in_=pB[:, 0:LC], func=AF.Copy), reads=[pB], writes=[s_])
                                mx = sc[0].next()
                                m.op("dve", lambda e, s_=s_, mx=mx, lo=lo: e.tensor_reduce(
                                    out=mx[:], in_=s_[:, lo:640], axis=AX.X, op=ALU.max), reads=[s_], writes=[mx])
                                ngm = sc[1].next()
                                m.op("dve", lambda e, mx=mx, ngm=ngm, hh=hh: e.tensor_scalar(
                                    out=ngm[:], in0=mx[:], scalar1=-SCALE, scalar2=nsnk[:, hh:hh + 1],
                                    op0=ALU.mult, op1=ALU.min), reads=[mx, nsnk], writes=[ngm])
                                pe2 = pe_.next()
                                m.op("act", lambda e, pe2=pe2, s_=s_, ngm=ngm, lo=lo: e.activation(
                                    out=pe2[:, lo:640], in_=s_[:, lo:640], func=AF.Exp, bias=ngm[:], scale=SCALE),
                                    reads=[s_, ngm], writes=[pe2])
                                es_ = sc[2].next()
                                m.op("act", lambda e, es_=es_, ngm=ngm, hh=hh: e.activation(
                                    out=es_[:], in_=snk[:, hh:hh + 1], func=AF.Exp, bias=ngm[:], scale=1.0),
                                    reads=[snk, ngm], writes=[es_])
                                rsum = sc[3].next()
                                m.op("dve", lambda e, rsum=rsum, pe2=pe2, lo=lo: e.tensor_reduce(
                                    out=rsum[:], in_=pe2[:, lo:640], axis=AX.X, op=ALU.add),
                                    reads=[pe2], writes=[rsum])
                                den = sc[4].next()
                                m.op("dve", lambda e, den=den, rsum=rsum, es_=es_: e.tensor_tensor(
                                    out=den[:], in0=rsum[:], in1=es_[:], op=ALU.add), reads=[rsum, es_], writes=[den])
                                inv = sc[5].next()
                                m.op("dve", lambda e, inv=inv, den=den: e.reciprocal(out=inv[:], in_=den[:]),
                                     reads=[den], writes=[inv])
                                pn2 = pn.next()
                                m.op("dve", lambda e, pn2=pn2, pe2=pe2, inv=inv, lo=lo: e.tensor_scalar(
                                    out=pn2[:, lo:640], in0=pe2[:, lo:640], scalar1=inv[:, 0:1], scalar2=None,
                                    op0=ALU.mult), reads=[pe2, inv], writes=[pn2])
                                pC, pD = PS[4], PS[5]
                                kbs = list(range(lo // 128, hi // 128))
                                for kb in kbs:
                                    m.op("pe", lambda e, kb=kb, pn2=pn2: e.transpose(
                                        pC[:, kb * 128:(kb + 1) * 128], pn2[:, kb * 128:(kb + 1) * 128], ident[:]),
                                        reads=[pn2, ident], writes=[pC])
                                for cb in range(2):
                                    m.op("pe", lambda e, cb=cb, pn2=pn2: e.transpose(
                                        pD[:, cb * 128:(cb + 1) * 128], pn2[:, 384 + cb * 128:384 + (cb + 1) * 128],
                                        ident[:]), reads=[pn2, ident], writes=[pD])
                                pt = pT.next()
                                if kbs:
                                    k0, k1 = kbs[0], kbs[-1] + 1
                                    m.op("dve", lambda e, pt=pt, k0=k0, k1=k1: e.tensor_copy(
                                        out=pt[:, k0:k1, :], in_=pC[:, k0 * 128:k1 * 128].rearrange("p (k t) -> p k t", t=128)),
                                        reads=[pC], writes=[pt])
                                m.op("act", lambda e, pt=pt: e.activation(
                                    out=pt[:, 3:5, :], in_=pD[:, 0:256].rearrange("p (k t) -> p k t", t=128),
                                    func=AF.Copy), reads=[pD], writes=[pt])
                                seq = [(vx, blk + kb, kb) for kb in kbs] + [(vaxc, cb, 3 + cb) for cb in range(2)]
                                for i_, (vb, vi, pi) in enumerate(seq):
                                    lastmm = (par == 1 and i_ == len(seq) - 1)
                                    m.op("pe", lambda e, vb=vb, vi=vi, pi=pi, pt=pt, first=first, lastmm=lastmm, kv=kv, par=par, pO=pO: e.matmul(
                                        pO[:, 0:128], lhsT=vb[:, vi, kv * 256 + par * 128:kv * 256 + (par + 1) * 128],
                                        rhs=pt[:, pi, :], start=first, stop=lastmm),
                                        reads=[vb, pt], writes=[pO])
                                    first = False
                            m.op("act", lambda e, pO=pO, c=c, blk=blk: e.activation(
                                out=y[:, 4 + c, blk * 128:(blk + 1) * 128], in_=pO[:, 0:128], func=AF.Copy),
                                reads=[pO], writes=[y])
                    m.dma("act", fm(Yd, t0, ts), y[:, :, 0:ts], reads=[y], writes=[Yd], sembuf=y)
        barrier()

    def stage3a(l, last):
        with ExitStack() as es:
            wg = stage_sb(es, "s3wg", [128, KC, 3072], BF16)
            wbr = stage_sb(es, "s3wbr", [128, KC, D], BF16)
            wv = Wb_in[l].t.rearrange("(k p) n -> p k n", p=128)
            m.dma("sp", wg[:], wv[:, :, 2048:5120], reads=[Wb_in[l]], writes=[wg], sembuf=wg)
            m.dma("sp", wbr[:], Wb_br[l].t.rearrange("(k p) n -> p k n", p=128), reads=[Wb_br[l]], writes=[wbr], sembuf=wbr)
            rh = Ring("s3h", 2, [128, KC, 512], BF16, es)
            ry = Ring("s3y", 2, [128, KC, 512], BF16, es)
            rmix = Ring("s3mix", 2, [128, KC, 512], BF16, es)
            sg = Ring("s3sg", 3, [128, 512], F32, es)
            tmp = Ring("s3tmp", 3, [128, 512], F32, es)
            mixf = Ring("s3mixf", 2, [128, 512], F32, es)
            brk = ((0, 2), (2, 4), (4, 8))
            for (t0, ts, r) in tiles512():
                if last and r == R - 1:
                    continue
                h = rh.next()
                m.dma("sp", h[:, :, 0:ts], fm(H1, t0, ts), reads=[H1], writes=[h], sembuf=h)
                y = ry.next()
                m.dma("sp", y[:, :, 0:ts], fm(Yd, t0, ts), reads=[Yd], writes=[y], sembuf=y)
                mix = rmix.next()
                for j in range(KC):
                    mf = mixf.next()
                    for bi, (ka, kb) in enumerate(brk):
                        pg = nps()
                        for k in range(KC):
                            m.op("pe", lambda e, k=k, pg=pg, bi=bi, j=j: e.matmul(
                                pg[:, 0:ts], lhsT=wg[:, k, bi * 1024 + j * 128:bi * 1024 + (j + 1) * 128],
                                rhs=h[:, k, 0:ts], start=(k == 0), stop=(k == KC - 1)), reads=[wg, h], writes=[pg])
                        pp = nps()
                        for k in range(ka, kb):
                            m.op("pe", lambda e, k=k, pp=pp, j=j, ka=ka, kb=kb: e.matmul(
                                pp[:, 0:ts], lhsT=wbr[:, k, j * 128:(j + 1) * 128], rhs=y[:, k, 0:ts],
                                start=(k == ka), stop=(k == kb - 1)), reads=[wbr, y], writes=[pp])
                        s_ = sg.next()
                        m.op("act", lambda e, s_=s_, pg=pg: e.activation(out=s_[:, 0:ts], in_=pg[:, 0:ts],
                                                                         func=AF.Sigmoid), reads=[pg], writes=[s_])
                        if bi == 0:
                            m.op("dve", lambda e, mf=mf, pp=pp, s_=s_: e.tensor_tensor(
                                out=mf[:, 0:ts], in0=pp[:, 0:ts], in1=s_[:, 0:ts], op=ALU.mult),
                                reads=[pp, s_], writes=[mf])
                        else:
                            t_ = tmp.next()
                            m.op("dve", lambda e, t_=t_, pp=pp, s_=s_: e.tensor_tensor(
                                out=t_[:, 0:ts], in0=pp[:, 0:ts], in1=s_[:, 0:ts], op=ALU.mult),
                                reads=[pp, s_], writes=[t_])
                            if bi == 1:
                                m.op("pool", lambda e, mf=mf, t_=t_: e.tensor_tensor(
                                    out=mf[:, 0:ts], in0=mf[:, 0:ts], in1=t_[:, 0:ts], op=ALU.add),
                                    reads=[mf, t_], writes=[mf])
                            else:
                                m.op("pool", lambda e, mf=mf, t_=t_, j=j: e.tensor_tensor(
                                    out=mix[:, j, 0:ts], in0=mf[:, 0:ts], in1=t_[:, 0:ts], op=ALU.add),
                                    reads=[mf, t_], writes=[mix])
                m.dma("act", fm(MIXd, t0, ts), mix[:, :, 0:ts], reads=[mix], writes=[MIXd], sembuf=mix)
        barrier()

    def stage3b(l, last, xr, xw):
        moe = (l % 2 == 1)
        with ExitStack() as es:
            wo = stage_sb(es, "s3wo", [128, KC, D], BF16)
            m.dma("sp", wo[:], Wb_out[l].t.rearrange("(k p) n -> p k n", p=128), reads=[Wb_out[l]], writes=[wo], sembuf=wo)
            rtw = stage_sb(es, "s3rt", [128, KC, E], F32)
            if moe:
                m.dma("sp", rtw[:], moe_rt.t[l // 2].rearrange("(k p) e -> p k e", p=128),
                      reads=[moe_rt], writes=[rtw], sembuf=rtw)
            rmix = Ring("s3bmix", 2, [128, KC, 512], BF16, es)
            rx = Ring("s3bx", 2, [128, KC, 512], F32, es)
            sqb = stage_sb(es, "s3bsq", [128, KC, 512], BF16)
            rt = stage_sb(es, "s3brt", [128, 512], F32)
            rstd = stage_sb(es, "s3brstd", [128, 512], F32)
            tmp = stage_sb(es, "s3btmp", [128, KC, 512], F32)
            hf = stage_sb(es, "s3bhf", [128, KC, 512], F32)
            rh2 = Ring("s3bh2", 2, [128, KC, 512], BF16, es)
            rgt = Ring("s3bgt", 2, [E, 512], BF16, es)
            sm8 = [Ring("s3bs%d" % i, 2, [128, E], F32, es) for i in range(5)]
            sc1 = [Ring("s3bc%d" % i, 2, [128, 1], F32, es) for i in range(7)]
            for (t0, ts, r) in tiles512():
                if last and r == R - 1:
                    continue
                nb = ts // 128
                mix = rmix.next()
                m.dma("sp", mix[:, :, 0:ts], fm(MIXd, t0, ts), reads=[MIXd], writes=[mix], sembuf=mix)
                xt = rx.next()
                m.dma("sp", xt[:, :, 0:ts], fm(xr, t0, ts), reads=[xr], writes=[xt], sembuf=xt)
                for j in range(KC):
                    p = nps()
                    for k in range(KC):
                        m.op("pe", lambda e, k=k, p=p, j=j: e.matmul(
                            p[:, 0:ts], lhsT=wo[:, k, j * 128:(j + 1) * 128], rhs=mix[:, k, 0:ts],
                            start=(k == 0), stop=(k == KC - 1)), reads=[wo, mix], writes=[p])
                    m.op("dve", lambda e, p=p, j=j: e.scalar_tensor_tensor(
                        out=xt[:, j, 0:ts], in0=p[:, 0:ts], scalar=modT[:, r, 16 + j:17 + j], in1=xt[:, j, 0:ts],
                        op0=ALU.mult, op1=ALU.add), reads=[p, modT, xt], writes=[xt])
                m.dma("act", fm(xw, t0, ts), xt[:, :, 0:ts], reads=[xt], writes=[xw], sembuf=xt)
                h2 = rh2.next()
                norm_mod(xt, ts, lambda k: A2[:, r, k:k + 1], lambda k: modT[:, r, 24 + k:25 + k],
                         sqb, rt, rstd, tmp, h2, hf=hf if moe else None)
                m.dma("act", fm(H2d, t0, ts), h2[:, :, 0:ts], reads=[h2], writes=[H2d], sembuf=h2)
                if moe:
                    gtt = rgt.next()
                    for blk in range(nb):
                        p = nps()
                        for k in range(KC):
                            m.op("pe", lambda e, k=k, p=p, blk=blk: e.matmul(
                                p[:, 0:E], lhsT=hf[:, k, blk * 128:(blk + 1) * 128], rhs=rtw[:, k, :],
                                start=(k == 0), stop=(k == KC - 1)), reads=[hf, rtw], writes=[p])
                        lg = sm8[0].next()
                        m.op("act", lambda e, lg=lg, p=p: e.activation(out=lg[:], in_=p[:, 0:E], func=AF.Copy),
                             reads=[p], writes=[lg])
                        m1 = sc1[0].next()
                        m.op("dve", lambda e, m1=m1, lg=lg: e.tensor_reduce(out=m1[:], in_=lg[:], axis=AX.X, op=ALU.max),
                             reads=[lg], writes=[m1])
                        eq1 = sm8[1].next()
                        m.op("dve", lambda e, eq1=eq1, lg=lg, m1=m1: e.tensor_scalar(
                            out=eq1[:], in0=lg[:], scalar1=m1[:, 0:1], scalar2=None, op0=ALU.is_equal),
                            reads=[lg, m1], writes=[eq1])
                        msk2 = sm8[2].next()
                        m.op("dve", lambda e, msk2=msk2, eq1=eq1, lg=lg: e.scalar_tensor_tensor(
                            out=msk2[:], in0=eq1[:], scalar=NEG, in1=lg[:], op0=ALU.mult, op1=ALU.add),
                            reads=[eq1, lg], writes=[msk2])
                        m2 = sc1[1].next()
                        m.op("dve", lambda e, m2=m2, msk2=msk2: e.tensor_reduce(out=m2[:], in_=msk2[:], axis=AX.X, op=ALU.max),
                             reads=[msk2], writes=[m2])
                        eq2 = sm8[3].next()
                        m.op("dve", lambda e, eq2=eq2, msk2=msk2, m2=m2: e.tensor_scalar(
                            out=eq2[:], in0=msk2[:], scalar1=m2[:, 0:1], scalar2=None, op0=ALU.is_equal),
                            reads=[msk2, m2], writes=[eq2])
                        dd = sc1[2].next()
                        m.op("dve", lambda e, dd=dd, m2=m2, m1=m1: e.tensor_tensor(out=dd[:], in0=m2[:], in1=m1[:], op=ALU.subtract),
                             reads=[m1, m2], writes=[dd])
                        ee = sc1[3].next()
                        m.op("act", lambda e, ee=ee, dd=dd: e.activation(out=ee[:], in_=dd[:], func=AF.Exp),
                             reads=[dd], writes=[ee])
                        dn = sc1[4].next()
                        m.op("dve", lambda e, dn=dn, ee=ee: e.tensor_scalar(out=dn[:], in0=ee[:], scalar1=1.0, scalar2=None, op0=ALU.add),
                             reads=[ee], writes=[dn])
                        w1_ = sc1[5].next()
                        m.op("dve", lambda e, w1_=w1_, dn=dn: e.reciprocal(out=w1_[:], in_=dn[:]), reads=[dn], writes=[w1_])
                        w2_ = sc1[6].next()
                        m.op("dve", lambda e, w2_=w2_, w1_=w1_, ee=ee: e.tensor_tensor(out=w2_[:], in0=w1_[:], in1=ee[:], op=ALU.mult),
                             reads=[w1_, ee], writes=[w2_])
                        ga = sm8[4].next()
                        m.op("dve", lambda e, ga=ga, eq1=eq1, w1_=w1_: e.tensor_scalar(
                            out=ga[:], in0=eq1[:], scalar1=w1_[:, 0:1], scalar2=None, op0=ALU.mult),
                            reads=[eq1, w1_], writes=[ga])
                        m.op("dve", lambda e, ga=ga, eq2=eq2, w2_=w2_: e.scalar_tensor_tensor(
                            out=ga[:], in0=eq2[:], scalar=w2_[:, 0:1], in1=ga[:], op0=ALU.mult, op1=ALU.add),
                            reads=[eq2, w2_, ga], writes=[ga])
                        p2 = nps()
                        m.op("pe", lambda e, p2=p2, ga=ga: e.transpose(p2[0:E, 0:128], ga[:], ident[:]),
                             reads=[ga, ident], writes=[p2])
                        m.op("act", lambda e, p2=p2, gtt=gtt, blk=blk: e.activation(
                            out=gtt[:, blk * 128:(blk + 1) * 128], in_=p2[0:E, 0:128], func=AF.Copy),
                            reads=[p2], writes=[gtt])
                    m.dma("act", GTd.t[:, t0:t0 + ts], gtt[:, 0:ts], reads=[gtt], writes=[GTd], sembuf=gtt)
        barrier()

    def stage4(l, last, xr, xw):
        moe = (l % 2 == 1)
        NE = E if moe else 1
        F = FE if moe else FD
        GF = 4 if moe else 2
        GW = GF * 128
        NG = F // GW
        TT = 1024
        tl = []
        if not last:
            for i in range((CT + TT - 1) // TT):
                tl.append((i * TT, min(TT, CT - i * TT), R - 1))
        for b in range(NB):
            for i in range(S // TT):
                tl.append((CT + b * S + i * TT, TT, b))
        with ExitStack() as es:
            rh2 = Ring("s4h", 2, [128, KC, TT], BF16, es)
            yacc = stage_sb(es, "s4yacc", [128, KC, TT], F32)
            gbc = stage_sb(es, "s4gbc", [128, E, TT], BF16) if moe else None
            ract = Ring("s4act", 3, [128, GF, 512], BF16, es)
            rs = Ring("s4s", 3, [128, 512], BF16, es)
            rt_ = Ring("s4t", 3, [128, 512], BF16, es)
            rxc = Ring("s4xc", 2, [128, TT], F32, es)
            rwgu = Ring("s4wgu", 3, [128, KC, 2 * GW], BF16, es)
            rwd = Ring("s4wd", 3, [128, GF, D], BF16, es)
            guv = [Wb_gu[l].t[e].rearrange("(k p) n -> p k n", p=128) for e in range(NE)]
            dv = [Wb_d[l].t[e] for e in range(NE)]
            gring = [0]
            for (t0, ts, r) in tl:
                h2 = rh2.next()
                m.dma("act", h2[:, :, 0:ts], fm(H2d, t0, ts), reads=[H2d], writes=[h2], sembuf=h2)
                if moe:
                    for e_ in range(E):
                        m.dma("act", gbc[:, e_, 0:ts], GTd.t[e_:e_ + 1, t0:t0 + ts].partition_broadcast(128),
                              reads=[GTd], writes=[gbc], sembuf=gbc)
                halves = [(o, min(512, ts - o)) for o in range(0, ts, 512)]
                pend = None
                first_unit = [True]

                def down(act_hs, wd, fu):
                    for (ho, hs), act in act_hs:
                        for j in range(KC):
                            py = PS[4 + (gring[0] % 4)]
                            gring[0] += 1
                            for f_ in range(GF):
                                m.op("pe", lambda e, py=py, f_=f_, j=j, act=act, hs=hs: e.matmul(
                                    py[:, 0:hs], lhsT=wd[:, f_, j * 128:(j + 1) * 128], rhs=act[:, f_, 0:hs],
                                    start=(f_ == 0), stop=(f_ == GF - 1)), reads=[wd, act], writes=[py])
                            if fu:
                                m.op("act", lambda e, py=py, j=j, ho=ho, hs=hs: e.activation(
                                    out=yacc[:, j, ho:ho + hs], in_=py[:, 0:hs], func=AF.Copy),
                                    reads=[py], writes=[yacc])
                            else:
                                m.op("dve", lambda e, py=py, j=j, ho=ho, hs=hs: e.tensor_tensor(
                                    out=yacc[:, j, ho:ho + hs], in0=py[:, 0:hs], in1=yacc[:, j, ho:ho + hs], op=ALU.add),
                                    reads=[py, yacc], writes=[yacc])

                ui = 0
                for e_ in range(NE):
                    for gi in range(NG):
                        wgu = rwgu.next()
                        m.dma("sp", wgu[:, :, 0:GW], guv[e_][:, :, gi * GW:(gi + 1) * GW],
                              reads=[Wb_gu[l]], writes=[wgu], sembuf=wgu)
                        m.dma("sp", wgu[:, :, GW:2 * GW], guv[e_][:, :, F + gi * GW:F + (gi + 1) * GW],
                              reads=[Wb_gu[l]], writes=[wgu], sembuf=wgu)
                        wd = rwd.next()
                        m.dma("sp", wd[:], dv[e_][gi * GW:(gi + 1) * GW, :].rearrange("(f p) d -> p f d", p=128),
                              reads=[Wb_d[l]], writes=[wd], sembuf=wd)
                        act_hs = []
                        for (ho, hs) in halves:
                            act = ract.next()
                            for f_ in range(GF):
                                pg = PS[(ui % 2)]
                                pu = PS[2 + (ui % 2)]
                                ui += 1
                                for k in range(KC):
                                    m.op("pe", lambda e, k=k, pg=pg, f_=f_, ho=ho, hs=hs: e.matmul(
                                        pg[:, 0:hs], lhsT=wgu[:, k, f_ * 128:(f_ + 1) * 128], rhs=h2[:, k, ho:ho + hs],
                                        start=(k == 0), stop=(k == KC - 1)), reads=[wgu, h2], writes=[pg])
                                for k in range(KC):
                                    m.op("pe", lambda e, k=k, pu=pu, f_=f_, ho=ho, hs=hs: e.matmul(
                                        pu[:, 0:hs], lhsT=wgu[:, k, GW + f_ * 128:GW + (f_ + 1) * 128],
                                        rhs=h2[:, k, ho:ho + hs], start=(k == 0), stop=(k == KC - 1)),
                                        reads=[wgu, h2], writes=[pu])
                                s_ = rs.next()
                                m.op("act", lambda e, s_=s_, pg=pg, hs=hs: e.activation(
                                    out=s_[:, 0:hs], in_=pg[:, 0:hs], func=AF.Silu), reads=[pg], writes=[s_])
                                if moe:
                                    t_ = rt_.next()
                                    m.op("dve", lambda e, t_=t_, pu=pu, s_=s_, hs=hs: e.tensor_tensor(
                                        out=t_[:, 0:hs], in0=pu[:, 0:hs], in1=s_[:, 0:hs], op=ALU.mult),
                                        reads=[pu, s_], writes=[t_])
                                    m.op("pool", lambda e, t_=t_, act=act, f_=f_, e_=e_, ho=ho, hs=hs: e.tensor_tensor(
                                        out=act[:, f_, 0:hs], in0=t_[:, 0:hs], in1=gbc[:, e_, ho:ho + hs], op=ALU.mult),
                                        reads=[t_, gbc], writes=[act])
                                else:
                                    m.op("dve", lambda e, act=act, pu=pu, s_=s_, f_=f_, hs=hs: e.tensor_tensor(
                                        out=act[:, f_, 0:hs], in0=pu[:, 0:hs], in1=s_[:, 0:hs], op=ALU.mult),
                                        reads=[pu, s_], writes=[act])
                            act_hs.append(((ho, hs), act))
                            if pend is not None and (ho, hs) == halves[0]:
                                down(*pend)
                                pend = None
                        pend = (act_hs, wd, first_unit[0])
                        first_unit[0] = False
                down(*pend)
                for j in range(KC):
                    xc = rxc.next()
                    m.dma("act", xc[:, 0:ts], xr.t[j, :, t0:t0 + ts], reads=[xr], writes=[xc], sembuf=xc)
                    m.op("dve", lambda e, xc=xc, j=j: e.scalar_tensor_tensor(
                        out=xc[:, 0:ts], in0=yacc[:, j, 0:ts], scalar=modT[:, r, 40 + j:41 + j], in1=xc[:, 0:ts],
                        op0=ALU.mult, op1=ALU.add), reads=[yacc, modT, xc], writes=[xc])
                    m.dma("act", xw.t[j, :, t0:t0 + ts], xc[:, 0:ts], reads=[xc], writes=[xw], sembuf=xc)
        barrier()

    def stage_final(xr):
        with ExitStack() as es:
            fn = stage_sb(es, "fn", [128, KC], F32)
            m.dma("sp", fn[:], fnT[:], reads=[fnT], writes=[fn], sembuf=fn)
            rx = Ring("sfx", 2, [128, KC, 512], F32, es)
            sqb = stage_sb(es, "sfsq", [128, KC, 512], BF16)
            rt = stage_sb(es, "sfrt", [128, 512], F32)
            rstd = stage_sb(es, "sfrstd", [128, 512], F32)
            yt = stage_sb(es, "sfy", [128, KC, 512], F32)
            ro = Ring("sfo", 2, [128, 4, D], F32, es)
            for i in range(TX // 512):
                t0 = CT + i * 512
                ts = 512
                xt = rx.next()
                m.dma("sp", xt[:], fm(xr, t0, ts), reads=[xr], writes=[xt], sembuf=xt)
                m.op("act", lambda e: e.activation(out=sqb[:], in_=xt[:], func=AF.Square), reads=[xt], writes=[sqb])
                p = nps()
                for k in range(KC):
                    m.op("pe", lambda e, k=k: e.matmul(p[:], lhsT=ones_bf[:], rhs=sqb[:, k, :], start=(k == 0),
                                                       stop=(k == KC - 1)), reads=[ones_bf, sqb], writes=[p])
                m.op("act", lambda e: e.activation(out=rt[:], in_=p[:], func=AF.Sqrt, bias=epsc[:], scale=1.0 / D),
                     reads=[p, epsc], writes=[rt])
                m.op("dve", lambda e: e.reciprocal(out=rstd[:], in_=rt[:]), reads=[rt], writes=[rstd])
                for k in range(KC):
                    m.op("dve", lambda e, k=k: e.scalar_tensor_tensor(
                        out=yt[:, k, :], in0=xt[:, k, :], scalar=fn[:, k:k + 1], in1=rstd[:],
                        op0=ALU.mult, op1=ALU.mult), reads=[xt, fn, rstd], writes=[yt])
                ot = ro.next()
                for blk in range(4):
                    for kh in range(2):
                        pp = nps()
                        for kk in range(4):
                            k = kh * 4 + kk
                            m.op("pe", lambda e, pp=pp, kk=kk, k=k, blk=blk: e.transpose(
                                pp[:, kk * 128:(kk + 1) * 128], yt[:, k, blk * 128:(blk + 1) * 128], ident[:]),
                                reads=[yt, ident], writes=[pp])
                        if (blk + kh) % 2 == 0:
                            m.op("act", lambda e, pp=pp, blk=blk, kh=kh: e.activation(
                                out=ot[:, blk, kh * 512:(kh + 1) * 512], in_=pp[:], func=AF.Copy),
                                reads=[pp], writes=[ot])
                        else:
                            m.op("dve", lambda e, pp=pp, blk=blk, kh=kh: e.tensor_copy(
                                out=ot[:, blk, kh * 512:(kh + 1) * 512], in_=pp[:]), reads=[pp], writes=[ot])
                m.dma("act", out.t[i * 512:(i + 1) * 512, :].rearrange("(b p) d -> p b d", p=128), ot[:],
                      reads=[ot], writes=[out], sembuf=ot)
        barrier()

    for l in range(L):
        convert_layer(l)
    stage0()
    cur = 0
    stop = cfg.stop
    done = False
    for l in range(L):
        last = (l == L - 1)
        ada(l, l == 0)
        stage1(l, XR[cur])
        if stop == (l, 1): done = True; break
        stage2(l, last)
        if stop == (l, 2): done = True; break
        stage3a(l, last)
        stage3b(l, last, XR[cur], XR[1 - cur])
        cur = 1 - cur
        if stop == (l, 3): done = True; break
        stage4(l, last, XR[cur], XR[1 - cur])
        cur = 1 - cur
        if stop == (l, 4): done = True; break
    if not done:
        stage_final(XR[cur])
    nobar.clear()
    barrier()
    ges.close()
    return nc, m


def host_prep(cfg, core, inp):
    NB, S, L = cfg.NB, cfg.S, cfg.L
    f = lambda a: np.ascontiguousarray(a, dtype=np.float32)
    b0 = core * NB
    d = {}
    d["x_in"] = f(inp["x"][b0:b0 + NB].reshape(NB * S, D))
    d["c_in"] = f(inp["ctx"][b0:b0 + NB].reshape(NB * LC, D))
    cv = np.concatenate([inp["c"][b0:b0 + NB], inp["c_ctx"][None]], 0)
    d["cT"] = f(cv.reshape(cfg.R, KC, 128).transpose(2, 1, 0))
    return d


def host_shared(cfg, inp):
    L = cfg.L
    f = lambda a: np.ascontiguousarray(a, dtype=np.float32)
    d = {}
    d["w_mod"] = f(inp["w_mod"])
    d["bmodT"] = f(inp["b_mod"].reshape(L, 48, 128).transpose(0, 2, 1))
    d["n1T"] = f(inp["norm1_g"].reshape(L, KC, 128).transpose(0, 2, 1))
    d["n2T"] = f(inp["norm2_g"].reshape(L, KC, 128).transpose(0, 2, 1))
    d["fnT"] = f(inp["final_norm_g"].reshape(KC, 128).T)
    w_in = inp["w_in"]
    d["w_in"] = f(w_in)
    rs = rot_src()
    qcols = np.concatenate([1280 + hh * 64 + rs for hh in range(8)])
    kk = [np.concatenate([1792 + kv * 64 + np.arange(64)] * 2) for kv in range(2)]
    kkp = [np.concatenate([1792 + kv * 64 + rs] * 2) for kv in range(2)]
    cols = np.concatenate([qcols] + kk + kkp)
    d["w_ex"] = f(w_in[:, :, cols])
    d["convT"] = f(inp["conv_w"].transpose(0, 2, 1).reshape(L, 2, 128, 3).transpose(0, 2, 1, 3))
    d["wsT"] = f(inp["gmlp_ws"].transpose(0, 3, 1, 2))
    gbv = inp["gmlp_b"]
    gb = np.repeat(gbv[:, :, None, :], 64, axis=2)
    d["gb"] = f(gb.reshape(L, 2, 128, 128).transpose(0, 2, 1, 3))
    d["sinkbc"] = f(np.broadcast_to(inp["attn_sink"][:, None, :], (L, 128, NH)))
    d["w_br"] = f(np.concatenate([inp["w_br_conv"], inp["w_br_gmlp"], inp["w_br_attn"]], axis=1))
    d["w_out"] = f(inp["w_out"])
    d["ffn_gu"] = f(inp["ffn_w_gu"])
    d["ffn_d"] = f(inp["ffn_w_d"])
    d["moe_rt"] = f(inp["moe_router"])
    d["moe_gu"] = f(inp["moe_w_gu"])
    d["moe_d"] = f(inp["moe_w_d"])
    tc, ts_ = rope_tabs(cfg)
    d["tabC"], d["tabS"] = tc, ts_
    qi = np.arange(128)[:, None]
    jj = np.arange(384)[None, :]
    d["mask"] = np.where((jj >= qi) & (jj <= qi + 256), 0.0, NEG).astype(np.float32)
    return d


_CACHE = {}


def run(cfg, inp, ncores):
    key = (cfg.NB, cfg.S, cfg.L, cfg.dbg, cfg.stop)
    if key not in _CACHE:
        _CACHE[key] = build(cfg)
    nc, m = _CACHE[key]
    sh = host_shared(cfg, inp)
    in_maps = []
    for c in range(ncores):
        dd = dict(sh)
        dd.update(host_prep(cfg, c, inp))
        in_maps.append(dd)
    res = run_bass_kernel_spmd(nc, in_maps, core_ids=list(range(ncores)))
    return res


def kernel(**inputs):
    cfg = Cfg(NB=2, S=4096, L=4)
    inp = {k: np.asarray(v) for k, v in inputs.items()}
    res = run(cfg, inp, 8)
    outs = [r["out"].reshape(cfg.NB, cfg.S, D) for r in res.results]
    return np.ascontiguousarray(np.concatenate(outs, axis=0), dtype=np.float32)
```

```python
import numpy as np
import concourse.bass as bass
import concourse.mybir as mybir
from contextlib import ExitStack

F32 = mybir.dt.float32
BF16 = mybir.dt.bfloat16
AF = mybir.ActivationFunctionType
ALU = mybir.AluOpType
AX = mybir.AxisListType


class Buf:
    __slots__ = ("name", "t", "writers", "readers", "dsem")

    def __init__(self, name, t=None):
        self.name = name
        self.t = t
        self.writers = {}
        self.readers = {}
        self.dsem = None

    def __getitem__(self, k):
        return self.t[k]


class MK:
    ENG = ("pe", "act", "dve", "pool", "sp")

    def __init__(self, nc, es):
        self.nc = nc
        self.es = es
        self.h = {"pe": nc.tensor, "act": nc.scalar, "dve": nc.vector,
                  "pool": nc.gpsimd, "sp": nc.sync}
        self.sems = {}
        self.issued = {}
        self.seen = {e: {} for e in self.ENG}
        for e in self.ENG:
            self.sems[e] = nc.alloc_semaphore(name="s_" + e)
            self.issued[e] = 0
        self.ndsem = 0
        self.ninstr = 0
        self.stage_bufs = []
        self.free_dsems = []
        self.dkeys = set()

    def sb(self, name, shape, dt):
        t = self.es.enter_context(self.nc.sbuf_tensor(name, list(shape), dt))
        return Buf(name, t)

    def uname(self, name):
        self.uid = getattr(self, "uid", 0) + 1
        return "%s_u%d" % (name, self.uid)

    def track(self, b):
        self.stage_bufs.append(b)
        return b

    def end_stage(self):
        for b in self.stage_bufs:
            if b.dsem is not None:
                self.free_dsems.append(b.dsem)
                b.dsem = None
        self.stage_bufs = []

    def ps(self, name, shape, dt):
        t = self.es.enter_context(self.nc.psum_tensor(name, list(shape), dt))
        return Buf(name, t)

    def dram(self, name, shape, dt, kind="Internal"):
        t = self.nc.dram_tensor(name, list(shape), dt, kind=kind)
        return Buf(name, t.ap())

    def _dsem(self, b):
        if b.dsem is None:
            if self.free_dsems:
                b.dsem = self.free_dsems.pop()
                return b.dsem
            k = "q%d" % self.ndsem
            self.ndsem += 1
            self.sems[k] = self.nc.alloc_semaphore(name="s_" + k)
            self.issued[k] = 0
            self.dkeys.add(k)
            b.dsem = k
        return b.dsem

    def _need(self, eng, reads, writes):
        need = {}

        def add(k, c, kind):
            if k == eng:
                if eng == "pe":
                    return
                if kind == "war":
                    return
            if c > need.get(k, 0):
                need[k] = c

        for b in reads:
            for k, c in b.writers.items():
                add(k, c, "raw")
        for b in writes:
            for k, c in b.writers.items():
                add(k, c, "waw")
            for k, c in b.readers.items():
                add(k, c, "war")
        seen = self.seen[eng]
        hnd = self.h[eng]
        for k, c in need.items():
            if seen.get(k, 0) >= c:
                continue
            if k in self.dkeys:
                c = max(c, self.issued[k])
            hnd.wait_ge(self.sems[k], c)
            seen[k] = c

    def _mark(self, key, cnt, reads, writes):
        for b in writes:
            b.writers = {key: cnt}
            b.readers = {}
        for b in reads:
            if b not in writes:
                b.readers[key] = cnt

    def op(self, eng, fn, reads=(), writes=()):
        self._need(eng, reads, writes)
        ins = fn(self.h[eng])
        self.issued[eng] += 1
        ins.then_inc(self.sems[eng], 1)
        self._mark(eng, self.issued[eng], reads, writes)
        self.ninstr += 1
        return ins

    def dma(self, q, out, in_, reads=(), writes=(), sembuf=None, **kw):
        self._need(q, reads, writes)
        k = self._dsem(sembuf)
        ins = self.h[q].dma_start(out=out, in_=in_, **kw)
        self.issued[k] += 16
        ins.then_inc(self.sems[k], 16)
        self._mark(k, self.issued[k], reads, writes)
        self.ninstr += 1
        return ins

    def wait_all(self, eng, bufs):
        self._need(eng, bufs, ())

from concourse.bass_utils import run_bass_kernel_spmd

D = 1024
KC = 8
LC = 256
NH = 8
E = 8
FD = 2816
FE = 3584
EPS = 1e-6
SCALE = 0.125
NEG = -1e30


class Cfg:
    def __init__(self, NB=2, S=4096, L=4, dbg=False, stop=None):
        self.NB, self.S, self.L, self.dbg, self.stop = NB, S, L, dbg, stop
        self.R = NB + 1
        self.CT = NB * LC
        self.TX = NB * S
        self.TA = self.CT + self.TX


def rope_tabs(cfg):
    S = cfg.S
    rows = S // 64
    row = np.repeat(np.arange(rows), 64).astype(np.float32)
    col = np.tile(np.arange(64), rows).astype(np.float32)
    half = 32
    inv = (1.0 / (10000.0 ** (np.arange(0, half, 2, dtype=np.float32) / half))).astype(np.float32)
    ang_r = row[:, None] * inv[None, :]
    ang_c = col[:, None] * inv[None, :]
    ang = np.concatenate([ang_r, ang_r, ang_c, ang_c], axis=-1)
    cos = np.cos(ang).astype(np.float32).T
    sin = np.sin(ang).astype(np.float32).T
    sgn = np.where((np.arange(64) % 32) < 16, -1.0, 1.0).astype(np.float32)[:, None]
    sins = sin * sgn
    tc = np.ones((128, cfg.TA), np.float32)
    ts_ = np.zeros((128, cfg.TA), np.float32)
    for b in range(cfg.NB):
        o = cfg.CT + b * S
        tc[:, o:o + S] = np.concatenate([cos, cos], 0)
        ts_[:, o:o + S] = np.concatenate([sins, sins], 0)
    return tc, ts_


def rot_src():
    j = np.arange(64)
    return (j // 32) * 32 + ((j % 32) + 16) % 32


def build(cfg):
    NB, S, L, R, CT, TX, TA = cfg.NB, cfg.S, cfg.L, cfg.R, cfg.CT, cfg.TX, cfg.TA
    nc = bass.Bass("TRN2", target_bir_lowering=False)
    ges = ExitStack()
    m = MK(nc, ges)
    EI = "ExternalInput"

    x_in = m.dram("x_in", [TX, D], F32, EI)
    c_in = m.dram("c_in", [CT, D], F32, EI)
    cT_in = m.dram("cT", [128, KC, R], F32, EI)
    w_mod = m.dram("w_mod", [L, D, 6 * D], F32, EI)
    bmodT = m.dram("bmodT", [L, 128, 48], F32, EI)
    n1T = m.dram("n1T", [L, 128, KC], F32, EI)
    n2T = m.dram("n2T", [L, 128, KC], F32, EI)
    fnT = m.dram("fnT", [128, KC], F32, EI)
    w_in = m.dram("w_in", [L, D, 5120], F32, EI)
    w_ex = m.dram("w_ex", [L, D, 1024], F32, EI)
    convT = m.dram("convT", [L, 128, 2, 3], F32, EI)
    wsT_in = m.dram("wsT", [L, 128, 4, 128], F32, EI)
    gb_in = m.dram("gb", [L, 128, 2, 128], F32, EI)
    sink_in = m.dram("sinkbc", [L, 128, NH], F32, EI)
    w_br = m.dram("w_br", [L, D, D], F32, EI)
    w_out = m.dram("w_out", [L, D, D], F32, EI)
    ND = (L + 1) // 2
    NM = max(L // 2, 1)
    ffn_gu = m.dram("ffn_gu", [ND, D, 2 * FD], F32, EI)
    ffn_d = m.dram("ffn_d", [ND, FD, D], F32, EI)
    moe_rt = m.dram("moe_rt", [NM, D, E], F32, EI)
    moe_gu = m.dram("moe_gu", [NM, E, D, 2 * FE], F32, EI)
    moe_d = m.dram("moe_d", [NM, E, FE, D], F32, EI)
    tabC = m.dram("tabC", [128, TA], F32, EI)
    tabS = m.dram("tabS", [128, TA], F32, EI)
    mask_in = m.dram("mask", [128, 384], F32, EI)
    out = m.dram("out", [TX, D], F32, "ExternalOutput")

    OK = "ExternalOutput" if cfg.dbg else "Internal"
    XR = [m.dram("XR%d" % i, [KC, 128, TA], F32, OK) for i in range(2)]
    H1 = m.dram("H1", [KC, 128, TA], BF16, OK)
    BGd = m.dram("BGd", [2, 128, TA], BF16, OK)
    CHd = m.dram("CHd", [2, 128, TA], BF16, OK)
    UGd = m.dram("UGd", [2, 128, TA], BF16, OK)
    Qd = m.dram("Qd", [4, 128, TA], BF16, OK)
    KKd = m.dram("KKd", [2, 128, TA], BF16, OK)
    VNXd = m.dram("VNXd", [TA, 512], BF16, OK)
    VAXd = m.dram("VAXd", [TA, 512], BF16, OK)
    Yd = m.dram("Yd", [KC, 128, TA], BF16, OK)
    MIXd = m.dram("MIXd", [KC, 128, TA], BF16, OK)
    H2d = m.dram("H2d", [KC, 128, TA], BF16, OK)
    GTd = m.dram("GTd", [E, TA], BF16, OK)
    Wb_in = [m.dram("Wb_in%d" % l, [D, 6144], BF16) for l in range(L)]
    Wb_br = [m.dram("Wb_br%d" % l, [D, D], BF16) for l in range(L)]
    Wb_out = [m.dram("Wb_out%d" % l, [D, D], BF16) for l in range(L)]
    Wb_gu, Wb_d = [], []
    for l in range(L):
        if l % 2 == 0:
            Wb_gu.append(m.dram("Wb_gu%d" % l, [1, D, 2 * FD], BF16))
            Wb_d.append(m.dram("Wb_d%d" % l, [1, FD, D], BF16))
        else:
            Wb_gu.append(m.dram("Wb_gu%d" % l, [E, D, 2 * FE], BF16))
            Wb_d.append(m.dram("Wb_d%d" % l, [E, FE, D], BF16))

    ident = m.sb("ident", [128, 128], F32)
    ones_bf = m.sb("ones_bf", [128, 128], BF16)
    epsc = m.sb("epsc", [128, 1], F32)
    m.op("pool", lambda e: e.memset(ident[:], 0.0), writes=[ident])
    m.op("pool", lambda e: e.affine_select(out=ident[:], in_=ident[:], pattern=[[-1, 128]],
                                            compare_op=ALU.not_equal, fill=1.0, base=0,
                                            channel_multiplier=1), reads=[ident], writes=[ident])
    m.op("pool", lambda e: e.memset(ones_bf[:], 1.0), writes=[ones_bf])
    m.op("pool", lambda e: e.memset(epsc[:], EPS), writes=[epsc])
    psall_t = ges.enter_context(nc.psum_tensor("psall", [128, 8, 512], F32))
    psall = psall_t[:]
    PS = [Buf("ps%d" % i, psall[:, i, :]) for i in range(8)]
    modT = m.sb("modT", [128, R, 48], F32)
    A1 = m.sb("A1", [128, R, KC], F32)
    A2 = m.sb("A2", [128, R, KC], F32)
    csil = m.sb("csil", [128, KC, R], F32)
    cst = m.sb("cst", [128, KC, R], F32)
    bmod = m.sb("bmod", [128, 48], F32)
    n1 = m.sb("n1", [128, KC], F32)
    n2 = m.sb("n2", [128, KC], F32)

    state = {"psi": 0}

    def nps():
        p = PS[state["psi"] % 8]
        state["psi"] += 1
        return p

    class Ring:
        def __init__(self, name, n, shape, dt, es=None):
            self.slots = []
            for i in range(n):
                nm = m.uname("%s_%d" % (name, i))
                t = (es or ges).enter_context(nc.sbuf_tensor(nm, list(shape), dt))
                self.slots.append(m.track(Buf(nm, t)))
            self.i = 0

        def next(self):
            s = self.slots[self.i % len(self.slots)]
            self.i += 1
            return s

    def stage_sb(es, name, shape, dt):
        nm = m.uname(name)
        t = es.enter_context(nc.sbuf_tensor(nm, list(shape), dt))
        return m.track(Buf(nm, t))

    nobar = set()

    def barrier():
        for e in MK.ENG:
            for k in list(m.sems.keys()):
                if k == e or k in nobar:
                    continue
                c = m.issued[k]
                if c > m.seen[e].get(k, 0):
                    m.h[e].wait_ge(m.sems[k], c)
                    m.seen[e][k] = c
        m.end_stage()

    cvb = Buf("cvsem")

    def conv_w(dst, dst_ap, src, src_ap):
        m.dma("pool", dst_ap, src_ap, reads=[src], writes=[dst], sembuf=dst)
        nobar.add(dst.dsem)

    def v2(ap, rows):
        return ap.rearrange("(p r) n -> p (r n)", p=128)

    def convert_layer(l):
        conv_w(Wb_in[l], Wb_in[l][:, 0:5120], w_in, w_in[l])
        conv_w(Wb_in[l], Wb_in[l][:, 5120:6144], w_ex, w_ex[l])
        conv_w(Wb_br[l], v2(Wb_br[l][:], D), w_br, v2(w_br[l], D))
        conv_w(Wb_out[l], v2(Wb_out[l][:], D), w_out, v2(w_out[l], D))
        if l % 2 == 0:
            conv_w(Wb_gu[l], v2(Wb_gu[l][0], D), ffn_gu, v2(ffn_gu[l // 2], D))
            conv_w(Wb_d[l], v2(Wb_d[l][0], FD), ffn_d, v2(ffn_d[l // 2], FD))
        else:
            for e in range(E):
                conv_w(Wb_gu[l], v2(Wb_gu[l][e], D), moe_gu, v2(moe_gu[l // 2, e], D))
                conv_w(Wb_d[l], v2(Wb_d[l][e], FE), moe_d, v2(moe_d[l // 2, e], FE))

    def tiles512():
        tl = [(0, CT, R - 1)] if CT <= 512 else [(i * 512, 512, R - 1) for i in range(CT // 512)]
        for b in range(NB):
            for i in range(S // 512):
                tl.append((CT + b * S + i * 512, 512, b))
        return tl

    def fm(dr, t0, ts, k0=0, k1=None):
        k1 = dr.t.shape[0] if k1 is None else k1
        return dr.t[k0:k1, :, t0:t0 + ts].rearrange("k p t -> p k t")

    def stage0():
        with ExitStack() as es:
            rin = Ring("s0in", 2, [128, 4, D], F32, es)
            rout = Ring("s0out", 2, [128, KC, 512], F32, es)
            srcs = [(c_in, i * 512, min(512, CT - i * 512), i * 512) for i in range((CT + 511) // 512)]
            srcs += [(x_in, i * 512, 512, CT + i * 512) for i in range(TX // 512)]
            for (src, r0, ts, t0) in srcs:
                nb = ts // 128
                it = rin.next()
                m.dma("sp", it[:, 0:nb, :], src.t[r0:r0 + ts, :].rearrange("(b p) d -> p b d", p=128),
                      reads=[src], writes=[it], sembuf=it)
                ot = rout.next()
                for blk in range(nb):
                    for kh in range(2):
                        p = nps()
                        for kk in range(4):
                            k = kh * 4 + kk
                            m.op("pe", lambda e, p=p, kk=kk, k=k, blk=blk: e.transpose(
                                p[:, kk * 128:(kk + 1) * 128], it[:, blk, k * 128:(k + 1) * 128], ident[:]),
                                reads=[it, ident], writes=[p])
                        eng = "act" if (blk + kh) % 2 == 0 else "dve"
                        src_v = p[:].rearrange("p (k t) -> p k t", k=4)
                        dst_v = ot[:, kh * 4:(kh + 1) * 4, blk * 128:(blk + 1) * 128]
                        if eng == "act":
                            m.op("act", lambda e, a=dst_v, b=src_v: e.activation(out=a, in_=b, func=AF.Copy),
                                 reads=[p], writes=[ot])
                        else:
                            m.op("dve", lambda e, a=dst_v, b=src_v: e.tensor_copy(out=a, in_=b),
                                 reads=[p], writes=[ot])
                m.dma("act", fm(XR[0], t0, ts), ot[:, :, 0:ts], reads=[ot], writes=[XR[0]], sembuf=ot)
        barrier()

    def ada(l, first):
        with ExitStack() as es:
            rw = Ring("adaw", 2, [128, KC, 512], F32, es)
            if first:
                m.dma("sp", cst[:], cT_in[:], reads=[cT_in], writes=[cst], sembuf=cst)
                m.op("act", lambda e: e.activation(out=csil[:], in_=cst[:], func=AF.Silu),
                     reads=[cst], writes=[csil])
            m.dma("sp", bmod[:], bmodT[l], reads=[bmodT], writes=[bmod], sembuf=bmod)
            m.dma("sp", n1[:], n1T[l], reads=[n1T], writes=[n1], sembuf=n1)
            m.dma("sp", n2[:], n2T[l], reads=[n2T], writes=[n2], sembuf=n2)
            for pc in range(12):
                wt = rw.next()
                m.dma("sp", wt[:], w_mod.t[l, :, pc * 512:(pc + 1) * 512].rearrange("(k p) n -> p k n", p=128),
                      reads=[w_mod], writes=[wt], sembuf=wt)
                p = nps()
                for jj in range(4):
                    for k in range(KC):
                        m.op("pe", lambda e, p=p, jj=jj, k=k: e.matmul(
                            p[:, jj * R:(jj + 1) * R], lhsT=wt[:, k, jj * 128:(jj + 1) * 128], rhs=csil[:, k, :],
                            start=(k == 0), stop=(k == KC - 1)), reads=[wt, csil], writes=[p])
                for jj in range(4):
                    ch = pc * 4 + jj
                    m.op("act", lambda e, p=p, jj=jj, ch=ch: e.activation(
                        out=modT[:, :, ch], in_=p[:, jj * R:(jj + 1) * R], func=AF.Identity,
                        bias=bmod[:, ch:ch + 1], scale=1.0), reads=[p, bmod], writes=[modT])
            for r in range(R):
                m.op("dve", lambda e, r=r: e.scalar_tensor_tensor(
                    out=A1[:, r, :], in0=modT[:, r, 8:16], scalar=1.0, in1=n1[:], op0=ALU.add, op1=ALU.mult),
                    reads=[modT, n1], writes=[A1])
                m.op("dve", lambda e, r=r: e.scalar_tensor_tensor(
                    out=A2[:, r, :], in0=modT[:, r, 32:40], scalar=1.0, in1=n2[:], op0=ALU.add, op1=ALU.mult),
                    reads=[modT, n2], writes=[A2])
        barrier()

    def norm_mod(xt, ts, Aap, Bap, sqb, rt, rstd, tmp, hdst, hf=None):
        m.op("act", lambda e: e.activation(out=sqb[:, :, 0:ts], in_=xt[:, :, 0:ts], func=AF.Square),
             reads=[xt], writes=[sqb])
        p = nps()
        for k in range(KC):
            m.op("pe", lambda e, k=k: e.matmul(p[:, 0:ts], lhsT=ones_bf[:], rhs=sqb[:, k, 0:ts],
                                               start=(k == 0), stop=(k == KC - 1)),
                 reads=[ones_bf, sqb], writes=[p])
        m.op("act", lambda e: e.activation(out=rt[:, 0:ts], in_=p[:, 0:ts], func=AF.Sqrt,
                                           bias=epsc[:], scale=1.0 / D), reads=[p, epsc], writes=[rt])
        m.op("dve", lambda e: e.reciprocal(out=rstd[:, 0:ts], in_=rt[:, 0:ts]), reads=[rt], writes=[rstd])
        for k in range(KC):
            m.op("dve", lambda e, k=k: e.scalar_tensor_tensor(
                out=tmp[:, k, 0:ts], in0=xt[:, k, 0:ts], scalar=Aap(k), in1=rstd[:, 0:ts],
                op0=ALU.mult, op1=ALU.mult), reads=[xt, rstd, A1, A2], writes=[tmp])
            if hf is None:
                m.op("act", lambda e, k=k: e.activation(out=hdst[:, k, 0:ts], in_=tmp[:, k, 0:ts],
                                                        func=AF.Identity, bias=Bap(k), scale=1.0),
                     reads=[tmp, modT], writes=[hdst])
            else:
                m.op("act", lambda e, k=k: e.activation(out=hf[:, k, 0:ts], in_=tmp[:, k, 0:ts],
                                                        func=AF.Identity, bias=Bap(k), scale=1.0),
                     reads=[tmp, modT], writes=[hf])
        if hf is not None:
            m.op("pool", lambda e: e.tensor_copy(out=hdst[:, :, 0:ts], in_=hf[:, :, 0:ts]),
                 reads=[hf], writes=[hdst])

    def stage1(l, xr):
        with ExitStack() as es:
            w1 = stage_sb(es, "w1", [128, KC, 3072], BF16)
            wv = Wb_in[l].t.rearrange("(k p) n -> p k n", p=128)
            m.dma("sp", w1[:, :, 0:2048], wv[:, :, 0:2048], reads=[Wb_in[l]], writes=[w1], sembuf=w1)
            m.dma("sp", w1[:, :, 2048:3072], wv[:, :, 5120:6144], reads=[Wb_in[l]], writes=[w1], sembuf=w1)
            rx = Ring("s1x", 2, [128, KC, 512], F32, es)
            rtab = Ring("s1tab", 2, [128, 2, 512], F32, es)
            sqb = stage_sb(es, "s1sq", [128, KC, 512], BF16)
            rt = stage_sb(es, "s1rt", [128, 512], F32)
            rstd = stage_sb(es, "s1rstd", [128, 512], F32)
            tmp = stage_sb(es, "s1tmp", [128, KC, 512], F32)
            rh = Ring("s1h", 2, [128, KC, 512], BF16, es)
            rfm = Ring("s1fm", 2, [128, 12, 512], BF16, es)
            cg = Ring("s1cg", 2, [128, 512], BF16, es)
            t1r = Ring("s1t1", 2, [128, 512], F32, es)
            t2r = Ring("s1t2", 2, [128, 512], F32, es)
            rvn = Ring("s1vn", 2, [128, 4, 512], BF16, es)
            rva = Ring("s1va", 2, [128, 4, 512], BF16, es)
            vg = Ring("s1vg", 2, [128, 256], F32, es)
            st6 = Ring("s1st", 2, [128, 6], F32, es)
            mv = Ring("s1mv", 2, [128, 2], F32, es)
            sd = Ring("s1sd", 2, [128, 1], F32, es)
            rs = Ring("s1rs", 2, [128, 1], F32, es)
            for s_ in rvn.slots + rva.slots:
                m.op("pool", lambda e, s_=s_: e.memset(s_[:], 0.0), writes=[s_])
            for (t0, ts, r) in tiles512():
                nb = ts // 128
                xt = rx.next()
                m.dma("sp", xt[:, :, 0:ts], fm(xr, t0, ts), reads=[xr], writes=[xt], sembuf=xt)
                tb = rtab.next()
                m.dma("sp", tb[:, 0, 0:ts], tabC[:, t0:t0 + ts], reads=[tabC], writes=[tb], sembuf=tb)
                m.dma("sp", tb[:, 1, 0:ts], tabS[:, t0:t0 + ts], reads=[tabS], writes=[tb], sembuf=tb)
                h = rh.next()
                norm_mod(xt, ts, lambda k: A1[:, r, k:k + 1], lambda k: modT[:, r, k:k + 1],
                         sqb, rt, rstd, tmp, h)
                m.dma("act", fm(H1, t0, ts), h[:, :, 0:ts], reads=[h], writes=[H1], sembuf=h)
                f = rfm.next()

                def proj(co):
                    p = nps()
                    for k in range(KC):
                        m.op("pe", lambda e, k=k: e.matmul(p[:, 0:ts], lhsT=w1[:, k, co:co + 128],
                                                           rhs=h[:, k, 0:ts], start=(k == 0), stop=(k == KC - 1)),
                             reads=[w1, h], writes=[p])
                    return p
                for j in range(2):
                    p = proj(0 + j * 128)
                    m.op("act", lambda e, p=p, j=j: e.activation(out=f[:, 0 + j, 0:ts], in_=p[:, 0:ts], func=AF.Copy),
                         reads=[p], writes=[f])
                for j in range(2):
                    p = proj(256 + j * 128)
                    c_ = cg.next()
                    m.op("act", lambda e, p=p, c_=c_: e.activation(out=c_[:, 0:ts], in_=p[:, 0:ts], func=AF.Copy),
                         reads=[p], writes=[c_])
                    p2 = proj(512 + j * 128)
                    m.op("dve", lambda e, p2=p2, c_=c_, j=j: e.tensor_tensor(
                        out=f[:, 2 + j, 0:ts], in0=p2[:, 0:ts], in1=c_[:, 0:ts], op=ALU.mult),
                        reads=[p2, c_], writes=[f])
                for j in range(2):
                    p = proj(768 + j * 128)
                    m.op("act", lambda e, p=p, j=j: e.activation(out=f[:, 4 + j, 0:ts], in_=p[:, 0:ts],
                                                                 func=AF.Gelu_apprx_tanh), reads=[p], writes=[f])
                for (co, cop, fo, n) in ((1280, 2048, 6, 4), (2560, 2816, 10, 2)):
                    for j in range(n):
                        p = proj(co + j * 128)
                        pp = proj(cop + j * 128)
                        a1 = t1r.next()
                        a2 = t2r.next()
                        m.op("dve", lambda e, p=p, a1=a1: e.tensor_tensor(
                            out=a1[:, 0:ts], in0=p[:, 0:ts], in1=tb[:, 0, 0:ts], op=ALU.mult),
                            reads=[p, tb], writes=[a1])
                        m.op("dve", lambda e, pp=pp, a2=a2: e.tensor_tensor(
                            out=a2[:, 0:ts], in0=pp[:, 0:ts], in1=tb[:, 1, 0:ts], op=ALU.mult),
                            reads=[pp, tb], writes=[a2])
                        m.op("pool", lambda e, a1=a1, a2=a2, fo=fo, j=j: e.tensor_tensor(
                            out=f[:, fo + j, 0:ts], in0=a1[:, 0:ts], in1=a2[:, 0:ts], op=ALU.add),
                            reads=[a1, a2], writes=[f])
                m.dma("act", fm(BGd, t0, ts), f[:, 0:2, 0:ts], reads=[f], writes=[BGd], sembuf=f)
                m.dma("act", fm(CHd, t0, ts), f[:, 2:4, 0:ts], reads=[f], writes=[CHd], sembuf=f)
                m.dma("act", fm(UGd, t0, ts), f[:, 4:6, 0:ts], reads=[f], writes=[UGd], sembuf=f)
                m.dma("act", fm(Qd, t0, ts), f[:, 6:10, 0:ts], reads=[f], writes=[Qd], sembuf=f)
                m.dma("act", fm(KKd, t0, ts), f[:, 10:12, 0:ts], reads=[f], writes=[KKd], sembuf=f)
                vn = rvn.next()
                va = rva.next()
                for blk in range(nb):
                    pv = nps()
                    for k in range(KC):
                        m.op("pe", lambda e, k=k, pv=pv, blk=blk: e.matmul(
                            pv[:, 0:256], lhsT=h[:, k, blk * 128:(blk + 1) * 128], rhs=w1[:, k, 1024:1280],
                            start=(k == 0), stop=(k == KC - 1)), reads=[w1, h], writes=[pv])
                    pa = nps()
                    for k in range(KC):
                        m.op("pe", lambda e, k=k, pa=pa, blk=blk: e.matmul(
                            pa[:, 0:128], lhsT=h[:, k, blk * 128:(blk + 1) * 128], rhs=w1[:, k, 1920:2048],
                            start=(k == 0), stop=(k == KC - 1)), reads=[w1, h], writes=[pa])
                    g_ = vg.next()
                    m.op("act", lambda e, pv=pv, g_=g_: e.activation(out=g_[:], in_=pv[:, 0:256],
                                                                     func=AF.Gelu_apprx_tanh),
                         reads=[pv], writes=[g_])
                    s6 = st6.next()
                    m.op("dve", lambda e, g_=g_, s6=s6: e.bn_stats(out=s6[:], in_=g_[:]), reads=[g_], writes=[s6])
                    mv_ = mv.next()
                    m.op("dve", lambda e, mv_=mv_, s6=s6: e.bn_aggr(out=mv_[:], in_=s6[:]), reads=[s6], writes=[mv_])
                    sd_ = sd.next()
                    m.op("act", lambda e, sd_=sd_, mv_=mv_: e.activation(out=sd_[:], in_=mv_[:, 1:2], func=AF.Sqrt,
                                                                         bias=epsc[:], scale=1.0),
                         reads=[mv_, epsc], writes=[sd_])
                    rs_ = rs.next()
                    m.op("dve", lambda e, sd_=sd_, rs_=rs_: e.reciprocal(out=rs_[:], in_=sd_[:]),
                         reads=[sd_], writes=[rs_])
                    for par in range(2):
                        src = g_[:].rearrange("p (g c) -> p g c", c=64)[:, par::2, :]
                        dst = vn[:, blk, :].rearrange("p (g c) -> p g c", c=128)[:, par::2, par * 64:(par + 1) * 64]
                        m.op("dve", lambda e, src=src, dst=dst, mv_=mv_, rs_=rs_: e.tensor_scalar(
                            out=dst, in0=src, scalar1=mv_[:, 0:1], scalar2=rs_[:, 0:1],
                            op0=ALU.subtract, op1=ALU.mult), reads=[g_, mv_, rs_], writes=[vn])
                        srca = pa[:, 0:128].rearrange("p (kv c) -> p kv c", c=64)
                        dsta = va[:, blk, :].rearrange("p (kv q c) -> p kv q c", kv=2, q=2)[:, :, par, par * 64:(par + 1) * 64]
                        m.op("act", lambda e, srca=srca, dsta=dsta: e.activation(out=dsta, in_=srca, func=AF.Copy),
                             reads=[pa], writes=[va])
                m.dma("act", VNXd.t[t0:t0 + ts, :].rearrange("(b p) c -> p b c", p=128), vn[:, 0:nb, :],
                      reads=[vn], writes=[VNXd], sembuf=vn)
                m.dma("act", VAXd.t[t0:t0 + ts, :].rearrange("(b p) c -> p b c", p=128), va[:, 0:nb, :],
                      reads=[va], writes=[VAXd], sembuf=va)
        barrier()

    def stage2(l, last):
        with ExitStack() as es:
            cw = stage_sb(es, "s2cw", [128, 2, 3], F32)
            wsf = stage_sb(es, "s2wsf", [128, 4, 128], F32)
            wsb = stage_sb(es, "s2wsb", [128, 4, 128], BF16)
            gbt = stage_sb(es, "s2gb", [128, 2, 128], F32)
            snk = stage_sb(es, "s2snk", [128, NH], F32)
            nsnk = stage_sb(es, "s2nsnk", [128, NH], F32)
            msk = stage_sb(es, "s2msk", [128, 384], F32)
            m.dma("sp", cw[:], convT[l], reads=[convT], writes=[cw], sembuf=cw)
            m.dma("sp", wsf[:], wsT_in[l], reads=[wsT_in], writes=[wsf], sembuf=wsf)
            m.dma("sp", gbt[:], gb_in[l], reads=[gb_in], writes=[gbt], sembuf=gbt)
            m.dma("sp", snk[:], sink_in[l], reads=[sink_in], writes=[snk], sembuf=snk)
            m.dma("sp", msk[:], mask_in[:], reads=[mask_in], writes=[msk], sembuf=msk)
            m.op("dve", lambda e: e.tensor_copy(out=wsb[:], in_=wsf[:]), reads=[wsf], writes=[wsb])
            m.op("dve", lambda e: e.tensor_scalar(out=nsnk[:], in0=snk[:], scalar1=-1.0, scalar2=None,
                                                  op0=ALU.mult), reads=[snk], writes=[nsnk])
            kkc = stage_sb(es, "s2kkc", [128, 2, LC], BF16)
            vaxc = stage_sb(es, "s2vaxc", [128, 2, 512], BF16)
            rch = Ring("s2ch", 2, [128, 2, 514], BF16, es)
            rbg = Ring("s2bg", 2, [128, 2, 512], BF16, es)
            rug = Ring("s2ug", 2, [128, 2, 512], BF16, es)
            rvn = Ring("s2vn", 2, [128, 4, 512], BF16, es)
            rq = Ring("s2q", 2, [128, 4, 512], BF16, es)
            rkk = Ring("s2kk", 2, [128, 2, 768], BF16, es)
            rvx = Ring("s2vx", 2, [128, 6, 512], BF16, es)
            ry = Ring("s2y", 2, [128, KC, 512], BF16, es)
            acc = Ring("s2acc", 2, [128, 512], F32, es)
            gt = Ring("s2gt", 2, [128, 2, 128], F32, es)
            sm4 = Ring("s2sm", 2, [128, 4, 640], F32, es)
            pe4 = Ring("s2pe", 2, [128, 4, 640], F32, es)
            pn4 = Ring("s2pn", 2, [128, 4, 640], F32, es)
            pT4 = Ring("s2pT", 2, [128, 4, 5, 128], BF16, es)
            sc4 = [Ring("s2sc%d" % i, 2, [128, 4], F32, es) for i in range(7)]
            for b in range(NB):
                m.dma("sp", kkc[:], fm(KKd, b * LC, LC), reads=[KKd], writes=[kkc], sembuf=kkc)
                m.dma("sp", vaxc[:], VAXd.t[b * LC:(b + 1) * LC, :].rearrange("(b p) c -> p b c", p=128),
                      reads=[VAXd], writes=[vaxc], sembuf=vaxc)
                tl = []
                if not last:
                    tl.append((b * LC, LC, 0, LC, True))
                for i in range(S // 512):
                    tl.append((CT + b * S + i * 512, 512, i * 512, S, False))
                for (t0, ts, s0, slen, isctx) in tl:
                    nb = ts // 128
                    hl = 1 if s0 > 0 else 0
                    hr = 1 if s0 + ts < slen else 0
                    ch = rch.next()
                    if not hl:
                        m.op("pool", lambda e, ch=ch: e.memset(ch[:, :, 0:1], 0.0), writes=[ch])
                    if not hr:
                        m.op("pool", lambda e, ch=ch: e.memset(ch[:, :, ts + 1:ts + 2], 0.0), writes=[ch])
                    m.dma("sp", ch[:, :, 1 - hl:ts + 1 + hr], fm(CHd, t0 - hl, ts + hl + hr),
                          reads=[CHd], writes=[ch], sembuf=ch)
                    bg = rbg.next()
                    m.dma("sp", bg[:, :, 0:ts], fm(BGd, t0, ts), reads=[BGd], writes=[bg], sembuf=bg)
                    ug = rug.next()
                    m.dma("sp", ug[:, :, 0:ts], fm(UGd, t0, ts), reads=[UGd], writes=[ug], sembuf=ug)
                    vn = rvn.next()
                    m.dma("sp", vn[:, 0:nb, :], VNXd.t[t0:t0 + ts, :].rearrange("(b p) c -> p b c", p=128),
                          reads=[VNXd], writes=[vn], sembuf=vn)
                    q = rq.next()
                    m.dma("sp", q[:, :, 0:ts], fm(Qd, t0, ts), reads=[Qd], writes=[q], sembuf=q)
                    kk = vx = None
                    if not isctx:
                        kl = 128 if s0 > 0 else 0
                        kr = 128 if s0 + ts < slen else 0
                        kk = rkk.next()
                        m.dma("sp", kk[:, :, 128 - kl:128 + ts + kr], fm(KKd, t0 - kl, ts + kl + kr),
                              reads=[KKd], writes=[kk], sembuf=kk)
                        vx = rvx.next()
                        nbl = (kl + ts + kr) // 128
                        b0 = 1 - kl // 128
                        m.dma("sp", vx[:, b0:b0 + nbl, :],
                              VAXd.t[t0 - kl:t0 + ts + kr, :].rearrange("(b p) c -> p b c", p=128),
                              reads=[VAXd], writes=[vx], sembuf=vx)
                    y = ry.next()
                    for j in range(2):
                        a = acc.next()
                        m.op("dve", lambda e, a=a, j=j: e.tensor_scalar(
                            out=a[:, 0:ts], in0=ch[:, j, 1:ts + 1], scalar1=cw[:, j, 1:2], scalar2=None,
                            op0=ALU.mult), reads=[ch, cw], writes=[a])
                        m.op("dve", lambda e, a=a, j=j: e.scalar_tensor_tensor(
                            out=a[:, 0:ts], in0=ch[:, j, 0:ts], scalar=cw[:, j, 0:1], in1=a[:, 0:ts],
                            op0=ALU.mult, op1=ALU.add), reads=[ch, cw, a], writes=[a])
                        m.op("dve", lambda e, a=a, j=j: e.scalar_tensor_tensor(
                            out=a[:, 0:ts], in0=ch[:, j, 2:ts + 2], scalar=cw[:, j, 2:3], in1=a[:, 0:ts],
                            op0=ALU.mult, op1=ALU.add), reads=[ch, cw, a], writes=[a])
                        m.op("pool", lambda e, a=a, j=j: e.tensor_tensor(
                            out=y[:, j, 0:ts], in0=a[:, 0:ts], in1=bg[:, j, 0:ts], op=ALU.mult),
                            reads=[a, bg], writes=[y])
                    for blk in range(nb):
                        p = nps()
                        for j in range(2):
                            for gg in range(2):
                                g = 2 * j + gg
                                m.op("pe", lambda e, p=p, j=j, g=g, gg=gg, blk=blk: e.matmul(
                                    p[:, j * 128:(j + 1) * 128], lhsT=vn[:, blk, g * 128:(g + 1) * 128],
                                    rhs=wsb[:, g, :], start=(gg == 0), stop=(gg == 1)),
                                    reads=[vn, wsb], writes=[p])
                        g_ = gt.next()
                        m.op("dve", lambda e, p=p, g_=g_: e.tensor_tensor(
                            out=g_[:], in0=p[:, 0:256].rearrange("p (j t) -> p j t", j=2), in1=gbt[:],
                            op=ALU.add), reads=[p, gbt], writes=[g_])
                        m.op("pool", lambda e, g_=g_, blk=blk: e.tensor_tensor(
                            out=y[:, 2:4, blk * 128:(blk + 1) * 128], in0=g_[:],
                            in1=ug[:, :, blk * 128:(blk + 1) * 128], op=ALU.mult),
                            reads=[g_, ug], writes=[y])
                    for blk in range(nb):
                        nbk = (s0 // 128) + blk
                        if isctx:
                            lo = hi = 384
                        else:
                            lo = 128 if nbk == 0 else 0
                            hi = 256 if nbk == slen // 128 - 1 else 384
                        kbs = list(range(lo // 128, hi // 128))
                        for kv in range(2):
                            h0 = kv * 4
                            A = PS[0:4]
                            Bk = PS[4:6]
                            O = PS[6:8]
                            s4 = sm4.next()
                            if hi == lo:
                                m.op("pool", lambda e: e.memset(s4[:, :, 0:384], NEG), writes=[s4])
                            else:
                                if lo > 0:
                                    m.op("pool", lambda e: e.memset(s4[:, :, 0:lo], NEG), writes=[s4])
                                if hi < 384:
                                    m.op("pool", lambda e: e.memset(s4[:, :, hi:384], NEG), writes=[s4])
                            for hq in range(4):
                                hh = h0 + hq
                                c = hh // 2
                                pb = (hh % 2) * 64
                                if hi > lo:
                                    m.op("pe", lambda e: e.matmul(
                                        A[hq][:, lo:hi], lhsT=q[pb:pb + 64, c, blk * 128:(blk + 1) * 128],
                                        rhs=kk[pb:pb + 64, kv, blk * 128 + lo:blk * 128 + hi], start=True, stop=True),
                                        reads=[q, kk], writes=[A[hq]])
                                m.op("pe", lambda e: e.matmul(
                                    Bk[hq % 2][:, (hq // 2) * 256:(hq // 2) * 256 + 256],
                                    lhsT=q[pb:pb + 64, c, blk * 128:(blk + 1) * 128],
                                    rhs=kkc[pb:pb + 64, kv, :], start=True, stop=True),
                                    reads=[q, kkc], writes=[Bk[hq % 2]])
                            if hi > lo:
                                m.op("dve", lambda e: e.tensor_tensor(
                                    out=s4[:, :, lo:hi], in0=psall[:, 0:4, lo:hi],
                                    in1=msk[:, lo:hi].unsqueeze(1).to_broadcast([128, 4, hi - lo]), op=ALU.add),
                                    reads=A + [msk], writes=[s4])
                            m.op("act", lambda e: e.activation(
                                out=s4[:, :, 384:640].rearrange("p (h b) c -> p h b c", b=2),
                                in_=psall[:, 4:6, :].rearrange("p b (h c) -> p h b c", h=2),
                                func=AF.Copy), reads=Bk, writes=[s4])
                            mx = sc4[0].next()
                            m.op("dve", lambda e: e.tensor_reduce(out=mx[:], in_=s4[:], axis=AX.X, op=ALU.max),
                                 reads=[s4], writes=[mx])
                            ngm = sc4[1].next()
                            m.op("dve", lambda e: e.scalar_tensor_tensor(
                                out=ngm[:], in0=mx[:], scalar=-SCALE, in1=nsnk[:, h0:h0 + 4], op0=ALU.mult, op1=ALU.min),
                                reads=[mx, nsnk], writes=[ngm])
                            p4 = pe4.next()
                            for hq in range(4):
                                m.op("act", lambda e: e.activation(
                                    out=p4[:, hq, :], in_=s4[:, hq, :], func=AF.Exp, bias=ngm[:, hq:hq + 1], scale=SCALE),
                                    reads=[s4, ngm], writes=[p4])
                            tt = sc4[2].next()
                            m.op("dve", lambda e: e.tensor_tensor(out=tt[:], in0=snk[:, h0:h0 + 4], in1=ngm[:], op=ALU.add),
                                 reads=[snk, ngm], writes=[tt])
                            es_ = sc4[3].next()
                            m.op("act", lambda e: e.activation(out=es_[:], in_=tt[:], func=AF.Exp), reads=[tt], writes=[es_])
                            rsum = sc4[4].next()
                            m.op("dve", lambda e: e.tensor_reduce(out=rsum[:], in_=p4[:], axis=AX.X, op=ALU.add),
                                 reads=[p4], writes=[rsum])
                            den = sc4[5].next()
                            m.op("dve", lambda e: e.tensor_tensor(out=den[:], in0=rsum[:], in1=es_[:], op=ALU.add),
                                 reads=[rsum, es_], writes=[den])
                            inv = sc4[6].next()
                            m.op("dve", lambda e: e.reciprocal(out=inv[:], in_=den[:]), reads=[den], writes=[inv])
                            n4 = pn4.next()
                            m.op("dve", lambda e: e.tensor_tensor(
                                out=n4[:], in0=p4[:], in1=inv[:].unsqueeze(2).to_broadcast([128, 4, 640]), op=ALU.mult),
                                reads=[p4, inv], writes=[n4])
                            for hq in range(4):
                                for kb in kbs:
                                    m.op("pe", lambda e: e.transpose(
                                        A[hq][:, kb * 128:(kb + 1) * 128], n4[:, hq, kb * 128:(kb + 1) * 128], ident[:]),
                                        reads=[n4, ident], writes=[A[hq]])
                                for cb in range(2):
                                    o_ = (hq // 2) * 256 + cb * 128
                                    m.op("pe", lambda e: e.transpose(
                                        Bk[hq % 2][:, o_:o_ + 128], n4[:, hq, 384 + cb * 128:384 + (cb + 1) * 128], ident[:]),
                                        reads=[n4, ident], writes=[Bk[hq % 2]])
                            pt = pT4.next()
                            if kbs:
                                k0, k1 = kbs[0], kbs[-1] + 1
                                m.op("dve", lambda e: e.tensor_copy(
                                    out=pt[:, :, k0:k1, :],
                                    in_=psall[:, 0:4, k0 * 128:k1 * 128].rearrange("p h (k t) -> p h k t", t=128)),
                                    reads=A, writes=[pt])
                            for h2_ in range(2):
                                m.op("act", lambda e: e.activation(
                                    out=pt[:, 2 * h2_:2 * h2_ + 2, 3:5, :],
                                    in_=psall[:, 4:6, h2_ * 256:(h2_ + 1) * 256].rearrange("p b (k t) -> p b k t", k=2),
                                    func=AF.Copy), reads=Bk, writes=[pt])
                            for cp in range(2):
                                pO = O[cp]
                                first = True
                                for par in range(2):
                                    hq = 2 * cp + par
                                    seq = [(vx, blk + kb, kb) for kb in kbs] + [(vaxc, cb, 3 + cb) for cb in range(2)]
                                    for i_, (vb, vi, pi) in enumerate(seq):
                                        lastmm = (par == 1 and i_ == len(seq) - 1)
                                        m.op("pe", lambda e: e.matmul(
                                            pO[:, 0:128], lhsT=vb[:, vi, kv * 256 + par * 128:kv * 256 + (par + 1) * 128],
                                            rhs=pt[:, hq, pi, :], start=first, stop=lastmm),
                                            reads=[vb, pt], writes=[pO])
                                        first = False
                            m.op("act", lambda e: e.activation(
                                out=y[:, 4 + kv * 2:6 + kv * 2, blk * 128:(blk + 1) * 128], in_=psall[:, 6:8, 0:128],
                                func=AF.Copy), reads=O, writes=[y])
                    m.dma("act", fm(Yd, t0, ts), y[:, :, 0:ts], reads=[y], writes=[Yd], sembuf=y)
        barrier()

    def stage3a(l, last):
        with ExitStack() as es:
            wg = stage_sb(es, "s3wg", [128, KC, 3072], BF16)
            wbr = stage_sb(es, "s3wbr", [128, KC, D], BF16)
            wv = Wb_in[l].t.rearrange("(k p) n -> p k n", p=128)
            m.dma("sp", wg[:], wv[:, :, 2048:5120], reads=[Wb_in[l]], writes=[wg], sembuf=wg)
            m.dma("sp", wbr[:], Wb_br[l].t.rearrange("(k p) n -> p k n", p=128), reads=[Wb_br[l]], writes=[wbr], sembuf=wbr)
            rh = Ring("s3h", 2, [128, KC, 512], BF16, es)
            ry = Ring("s3y", 2, [128, KC, 512], BF16, es)
            rmix = Ring("s3mix", 2, [128, KC, 512], BF16, es)
            sg = Ring("s3sg", 3, [128, 512], F32, es)
            tmp = Ring("s3tmp", 3, [128, 512], F32, es)
            mixf = Ring("s3mixf", 2, [128, 512], F32, es)
            brk = ((0, 2), (2, 4), (4, 8))
            for (t0, ts, r) in tiles512():
                if last and r == R - 1:
                    continue
                h = rh.next()
                m.dma("sp", h[:, :, 0:ts], fm(H1, t0, ts), reads=[H1], writes=[h], sembuf=h)
                y = ry.next()
                m.dma("sp", y[:, :, 0:ts], fm(Yd, t0, ts), reads=[Yd], writes=[y], sembuf=y)
                mix = rmix.next()
                for j in range(KC):
                    mf = mixf.next()
                    for bi, (ka, kb) in enumerate(brk):
                        pg = nps()
                        for k in range(KC):
                            m.op("pe", lambda e, k=k, pg=pg, bi=bi, j=j: e.matmul(
                                pg[:, 0:ts], lhsT=wg[:, k, bi * 1024 + j * 128:bi * 1024 + (j + 1) * 128],
                                rhs=h[:, k, 0:ts], start=(k == 0), stop=(k == KC - 1)), reads=[wg, h], writes=[pg])
                        pp = nps()
                        for k in range(ka, kb):
                            m.op("pe", lambda e, k=k, pp=pp, j=j, ka=ka, kb=kb: e.matmul(
                                pp[:, 0:ts], lhsT=wbr[:, k, j * 128:(j + 1) * 128], rhs=y[:, k, 0:ts],
                                start=(k == ka), stop=(k == kb - 1)), reads=[wbr, y], writes=[pp])
                        s_ = sg.next()
                        m.op("act", lambda e, s_=s_, pg=pg: e.activation(out=s_[:, 0:ts], in_=pg[:, 0:ts],
                                                                         func=AF.Sigmoid), reads=[pg], writes=[s_])
                        if bi == 0:
                            m.op("dve", lambda e, mf=mf, pp=pp, s_=s_: e.tensor_tensor(
                                out=mf[:, 0:ts], in0=pp[:, 0:ts], in1=s_[:, 0:ts], op=ALU.mult),
                                reads=[pp, s_], writes=[mf])
                        else:
                            t_ = tmp.next()
                            m.op("dve", lambda e, t_=t_, pp=pp, s_=s_: e.tensor_tensor(
                                out=t_[:, 0:ts], in0=pp[:, 0:ts], in1=s_[:, 0:ts], op=ALU.mult),
                                reads=[pp, s_], writes=[t_])
                            if bi == 1:
                                m.op("pool", lambda e, mf=mf, t_=t_: e.tensor_tensor(
                                    out=mf[:, 0:ts], in0=mf[:, 0:ts], in1=t_[:, 0:ts], op=ALU.add),
                                    reads=[mf, t_], writes=[mf])
                            else:
                                m.op("pool", lambda e, mf=mf, t_=t_, j=j: e.tensor_tensor(
                                    out=mix[:, j, 0:ts], in0=mf[:, 0:ts], in1=t_[:, 0:ts], op=ALU.add),
                                    reads=[mf, t_], writes=[mix])
                m.dma("act", fm(MIXd, t0, ts), mix[:, :, 0:ts], reads=[mix], writes=[MIXd], sembuf=mix)
        barrier()

    def stage3b(l, last, xr, xw):
        moe = (l % 2 == 1)
        with ExitStack() as es:
            wo = stage_sb(es, "s3wo", [128, KC, D], BF16)
            m.dma("sp", wo[:], Wb_out[l].t.rearrange("(k p) n -> p k n", p=128), reads=[Wb_out[l]], writes=[wo], sembuf=wo)
            rtw = stage_sb(es, "s3rt", [128, KC, E], F32)
            if moe:
                m.dma("sp", rtw[:], moe_rt.t[l // 2].rearrange("(k p) e -> p k e", p=128),
                      reads=[moe_rt], writes=[rtw], sembuf=rtw)
            rmix = Ring("s3bmix", 2, [128, KC, 512], BF16, es)
            rx = Ring("s3bx", 2, [128, KC, 512], F32, es)
            sqb = stage_sb(es, "s3bsq", [128, KC, 512], BF16)
            rt = stage_sb(es, "s3brt", [128, 512], F32)
            rstd = stage_sb(es, "s3brstd", [128, 512], F32)
            tmp = stage_sb(es, "s3btmp", [128, KC, 512], F32)
            hf = stage_sb(es, "s3bhf", [128, KC, 512], F32)
            rh2 = Ring("s3bh2", 2, [128, KC, 512], BF16, es)
            rgt = Ring("s3bgt", 2, [E, 512], BF16, es)
            sm8 = [Ring("s3bs%d" % i, 2, [128, E], F32, es) for i in range(5)]
            sc1 = [Ring("s3bc%d" % i, 2, [128, 1], F32, es) for i in range(7)]
            for (t0, ts, r) in tiles512():
                if last and r == R - 1:
                    continue
                nb = ts // 128
                mix = rmix.next()
                m.dma("sp", mix[:, :, 0:ts], fm(MIXd, t0, ts), reads=[MIXd], writes=[mix], sembuf=mix)
                xt = rx.next()
                m.dma("sp", xt[:, :, 0:ts], fm(xr, t0, ts), reads=[xr], writes=[xt], sembuf=xt)
                for j in range(KC):
                    p = nps()
                    for k in range(KC):
                        m.op("pe", lambda e, k=k, p=p, j=j: e.matmul(
                            p[:, 0:ts], lhsT=wo[:, k, j * 128:(j + 1) * 128], rhs=mix[:, k, 0:ts],
                            start=(k == 0), stop=(k == KC - 1)), reads=[wo, mix], writes=[p])
                    m.op("dve", lambda e, p=p, j=j: e.scalar_tensor_tensor(
                        out=xt[:, j, 0:ts], in0=p[:, 0:ts], scalar=modT[:, r, 16 + j:17 + j], in1=xt[:, j, 0:ts],
                        op0=ALU.mult, op1=ALU.add), reads=[p, modT, xt], writes=[xt])
                m.dma("act", fm(xw, t0, ts), xt[:, :, 0:ts], reads=[xt], writes=[xw], sembuf=xt)
                h2 = rh2.next()
                norm_mod(xt, ts, lambda k: A2[:, r, k:k + 1], lambda k: modT[:, r, 24 + k:25 + k],
                         sqb, rt, rstd, tmp, h2, hf=hf if moe else None)
                m.dma("act", fm(H2d, t0, ts), h2[:, :, 0:ts], reads=[h2], writes=[H2d], sembuf=h2)
                if moe:
                    gtt = rgt.next()
                    for blk in range(nb):
                        p = nps()
                        for k in range(KC):
                            m.op("pe", lambda e, k=k, p=p, blk=blk: e.matmul(
                                p[:, 0:E], lhsT=hf[:, k, blk * 128:(blk + 1) * 128], rhs=rtw[:, k, :],
                                start=(k == 0), stop=(k == KC - 1)), reads=[hf, rtw], writes=[p])
                        lg = sm8[0].next()
                        m.op("act", lambda e, lg=lg, p=p: e.activation(out=lg[:], in_=p[:, 0:E], func=AF.Copy),
                             reads=[p], writes=[lg])
                        m1 = sc1[0].next()
                        m.op("dve", lambda e, m1=m1, lg=lg: e.tensor_reduce(out=m1[:], in_=lg[:], axis=AX.X, op=ALU.max),
                             reads=[lg], writes=[m1])
                        eq1 = sm8[1].next()
                        m.op("dve", lambda e, eq1=eq1, lg=lg, m1=m1: e.tensor_scalar(
                            out=eq1[:], in0=lg[:], scalar1=m1[:, 0:1], scalar2=None, op0=ALU.is_equal),
                            reads=[lg, m1], writes=[eq1])
                        msk2 = sm8[2].next()
                        m.op("dve", lambda e, msk2=msk2, eq1=eq1, lg=lg: e.scalar_tensor_tensor(
                            out=msk2[:], in0=eq1[:], scalar=NEG, in1=lg[:], op0=ALU.mult, op1=ALU.add),
                            reads=[eq1, lg], writes=[msk2])
                        m2 = sc1[1].next()
                        m.op("dve", lambda e, m2=m2, msk2=msk2: e.tensor_reduce(out=m2[:], in_=msk2[:], axis=AX.X, op=ALU.max),
                             reads=[msk2], writes=[m2])
                        eq2 = sm8[3].next()
                        m.op("dve", lambda e, eq2=eq2, msk2=msk2, m2=m2: e.tensor_scalar(
                            out=eq2[:], in0=msk2[:], scalar1=m2[:, 0:1], scalar2=None, op0=ALU.is_equal),
                            reads=[msk2, m2], writes=[eq2])
                        dd = sc1[2].next()
                        m.op("dve", lambda e, dd=dd, m2=m2, m1=m1: e.tensor_tensor(out=dd[:], in0=m2[:], in1=m1[:], op=ALU.subtract),
                             reads=[m1, m2], writes=[dd])
                        ee = sc1[3].next()
                        m.op("act", lambda e, ee=ee, dd=dd: e.activation(out=ee[:], in_=dd[:], func=AF.Exp),
                             reads=[dd], writes=[ee])
                        dn = sc1[4].next()
                        m.op("dve", lambda e, dn=dn, ee=ee: e.tensor_scalar(out=dn[:], in0=ee[:], scalar1=1.0, scalar2=None, op0=ALU.add),
                             reads=[ee], writes=[dn])
                        w1_ = sc1[5].next()
                        m.op("dve", lambda e, w1_=w1_, dn=dn: e.reciprocal(out=w1_[:], in_=dn[:]), reads=[dn], writes=[w1_])
                        w2_ = sc1[6].next()
                        m.op("dve", lambda e, w2_=w2_, w1_=w1_, ee=ee: e.tensor_tensor(out=w2_[:], in0=w1_[:], in1=ee[:], op=ALU.mult),
                             reads=[w1_, ee], writes=[w2_])
                        ga = sm8[4].next()
                        m.op("dve", lambda e, ga=ga, eq1=eq1, w1_=w1_: e.tensor_scalar(
                            out=ga[:], in0=eq1[:], scalar1=w1_[:, 0:1], scalar2=None, op0=ALU.mult),
                            reads=[eq1, w1_], writes=[ga])
                        m.op("dve", lambda e, ga=ga, eq2=eq2, w2_=w2_: e.scalar_tensor_tensor(
                            out=ga[:], in0=eq2[:], scalar=w2_[:, 0:1], in1=ga[:], op0=ALU.mult, op1=ALU.add),
                            reads=[eq2, w2_, ga], writes=[ga])
                        p2 = nps()
                        m.op("pe", lambda e, p2=p2, ga=ga: e.transpose(p2[0:E, 0:128], ga[:], ident[:]),
                             reads=[ga, ident], writes=[p2])
                        m.op("act", lambda e, p2=p2, gtt=gtt, blk=blk: e.activation(
                            out=gtt[:, blk * 128:(blk + 1) * 128], in_=p2[0:E, 0:128], func=AF.Copy),
                            reads=[p2], writes=[gtt])
                    m.dma("act", GTd.t[:, t0:t0 + ts], gtt[:, 0:ts], reads=[gtt], writes=[GTd], sembuf=gtt)
        barrier()

    def stage4(l, last, xr, xw):
        moe = (l % 2 == 1)
        NE = E if moe else 1
        F = FE if moe else FD
        GF = 4 if moe else 2
        GW = GF * 128
        NG = F // GW
        TT = 1024
        tl = []
        if not last:
            for i in range((CT + TT - 1) // TT):
                tl.append((i * TT, min(TT, CT - i * TT), R - 1))
        for b in range(NB):
            for i in range(S // TT):
                tl.append((CT + b * S + i * TT, TT, b))
        with ExitStack() as es:
            rh2 = Ring("s4h", 2, [128, KC, TT], BF16, es)
            yacc = stage_sb(es, "s4yacc", [128, KC, TT], F32)
            gbc = stage_sb(es, "s4gbc", [128, E, TT], BF16) if moe else None
            ract = Ring("s4act", 3, [128, GF, 512], BF16, es)
            rs = Ring("s4s", 3, [128, 512], BF16, es)
            rt_ = Ring("s4t", 3, [128, 512], BF16, es)
            rxc = Ring("s4xc", 2, [128, TT], F32, es)
            rwgu = Ring("s4wgu", 3, [128, KC, 2 * GW], BF16, es)
            rwd = Ring("s4wd", 3, [128, GF, D], BF16, es)
            guv = [Wb_gu[l].t[e].rearrange("(k p) n -> p k n", p=128) for e in range(NE)]
            dv = [Wb_d[l].t[e] for e in range(NE)]
            gring = [0]
            for (t0, ts, r) in tl:
                h2 = rh2.next()
                m.dma("act", h2[:, :, 0:ts], fm(H2d, t0, ts), reads=[H2d], writes=[h2], sembuf=h2)
                if moe:
                    for e_ in range(E):
                        m.dma("act", gbc[:, e_, 0:ts], GTd.t[e_:e_ + 1, t0:t0 + ts].partition_broadcast(128),
                              reads=[GTd], writes=[gbc], sembuf=gbc)
                halves = [(o, min(512, ts - o)) for o in range(0, ts, 512)]
                pend = None
                first_unit = [True]

                def down(act_hs, wd, fu):
                    for (ho, hs), act in act_hs:
                        for j in range(KC):
                            py = PS[4 + (gring[0] % 4)]
                            gring[0] += 1
                            for f_ in range(GF):
                                m.op("pe", lambda e, py=py, f_=f_, j=j, act=act, hs=hs: e.matmul(
                                    py[:, 0:hs], lhsT=wd[:, f_, j * 128:(j + 1) * 128], rhs=act[:, f_, 0:hs],
                                    start=(f_ == 0), stop=(f_ == GF - 1)), reads=[wd, act], writes=[py])
                            if fu:
                                m.op("act", lambda e, py=py, j=j, ho=ho, hs=hs: e.activation(
                                    out=yacc[:, j, ho:ho + hs], in_=py[:, 0:hs], func=AF.Copy),
                                    reads=[py], writes=[yacc])
                            else:
                                m.op("dve", lambda e, py=py, j=j, ho=ho, hs=hs: e.tensor_tensor(
                                    out=yacc[:, j, ho:ho + hs], in0=py[:, 0:hs], in1=yacc[:, j, ho:ho + hs], op=ALU.add),
                                    reads=[py, yacc], writes=[yacc])

                ui = 0
                for e_ in range(NE):
                    for gi in range(NG):
                        wgu = rwgu.next()
                        m.dma("sp", wgu[:, :, 0:GW], guv[e_][:, :, gi * GW:(gi + 1) * GW],
                              reads=[Wb_gu[l]], writes=[wgu], sembuf=wgu)
                        m.dma("sp", wgu[:, :, GW:2 * GW], guv[e_][:, :, F + gi * GW:F + (gi + 1) * GW],
                              reads=[Wb_gu[l]], writes=[wgu], sembuf=wgu)
                        wd = rwd.next()
                        m.dma("sp", wd[:], dv[e_][gi * GW:(gi + 1) * GW, :].rearrange("(f p) d -> p f d", p=128),
                              reads=[Wb_d[l]], writes=[wd], sembuf=wd)
                        act_hs = []
                        for (ho, hs) in halves:
                            act = ract.next()
                            for f_ in range(GF):
                                pg = PS[(ui % 2)]
                                pu = PS[2 + (ui % 2)]
                                ui += 1
                                for k in range(KC):
                                    m.op("pe", lambda e, k=k, pg=pg, f_=f_, ho=ho, hs=hs: e.matmul(
                                        pg[:, 0:hs], lhsT=wgu[:, k, f_ * 128:(f_ + 1) * 128], rhs=h2[:, k, ho:ho + hs],
                                        start=(k == 0), stop=(k == KC - 1)), reads=[wgu, h2], writes=[pg])
                                for k in range(KC):
                                    m.op("pe", lambda e, k=k, pu=pu, f_=f_, ho=ho, hs=hs: e.matmul(
                                        pu[:, 0:hs], lhsT=wgu[:, k, GW + f_ * 128:GW + (f_ + 1) * 128],
                                        rhs=h2[:, k, ho:ho + hs], start=(k == 0), stop=(k == KC - 1)),
                                        reads=[wgu, h2], writes=[pu])
                                s_ = rs.next()
                                m.op("act", lambda e, s_=s_, pg=pg, hs=hs: e.activation(
                                    out=s_[:, 0:hs], in_=pg[:, 0:hs], func=AF.Silu), reads=[pg], writes=[s_])
                                if moe:
                                    t_ = rt_.next()
                                    m.op("dve", lambda e, t_=t_, pu=pu, s_=s_, hs=hs: e.tensor_tensor(
                                        out=t_[:, 0:hs], in0=pu[:, 0:hs], in1=s_[:, 0:hs], op=ALU.mult),
                                        reads=[pu, s_], writes=[t_])
                                    m.op("pool", lambda e, t_=t_, act=act, f_=f_, e_=e_, ho=ho, hs=hs: e.tensor_tensor(
                                        out=act[:, f_, 0:hs], in0=t_[:, 0:hs], in1=gbc[:, e_, ho:ho + hs], op=ALU.mult),
                                        reads=[t_, gbc], writes=[act])
                                else:
                                    m.op("dve", lambda e, act=act, pu=pu, s_=s_, f_=f_, hs=hs: e.tensor_tensor(
                                        out=act[:, f_, 0:hs], in0=pu[:, 0:hs], in1=s_[:, 0:hs], op=ALU.mult),
                                        reads=[pu, s_], writes=[act])
                            act_hs.append(((ho, hs), act))
                            if pend is not None and (ho, hs) == halves[0]:
                                down(*pend)
                                pend = None
                        pend = (act_hs, wd, first_unit[0])
                        first_unit[0] = False
                down(*pend)
                for j in range(KC):
                    xc = rxc.next()
                    m.dma("act", xc[:, 0:ts], xr.t[j, :, t0:t0 + ts], reads=[xr], writes=[xc], sembuf=xc)
                    m.op("dve", lambda e, xc=xc, j=j: e.scalar_tensor_tensor(
                        out=xc[:, 0:ts], in0=yacc[:, j, 0:ts], scalar=modT[:, r, 40 + j:41 + j], in1=xc[:, 0:ts],
                        op0=ALU.mult, op1=ALU.add), reads=[yacc, modT, xc], writes=[xc])
                    m.dma("act", xw.t[j, :, t0:t0 + ts], xc[:, 0:ts], reads=[xc], writes=[xw], sembuf=xc)
        barrier()

    def stage_final(xr):
        with ExitStack() as es:
            fn = stage_sb(es, "fn", [128, KC], F32)
            m.dma("sp", fn[:], fnT[:], reads=[fnT], writes=[fn], sembuf=fn)
            rx = Ring("sfx", 2, [128, KC, 512], F32, es)
            sqb = stage_sb(es, "sfsq", [128, KC, 512], BF16)
            rt = stage_sb(es, "sfrt", [128, 512], F32)
            rstd = stage_sb(es, "sfrstd", [128, 512], F32)
            yt = stage_sb(es, "sfy", [128, KC, 512], F32)
            ro = Ring("sfo", 2, [128, 4, D], F32, es)
            for i in range(TX // 512):
                t0 = CT + i * 512
                ts = 512
                xt = rx.next()
                m.dma("sp", xt[:], fm(xr, t0, ts), reads=[xr], writes=[xt], sembuf=xt)
                m.op("act", lambda e: e.activation(out=sqb[:], in_=xt[:], func=AF.Square), reads=[xt], writes=[sqb])
                p = nps()
                for k in range(KC):
                    m.op("pe", lambda e, k=k: e.matmul(p[:], lhsT=ones_bf[:], rhs=sqb[:, k, :], start=(k == 0),
                                                       stop=(k == KC - 1)), reads=[ones_bf, sqb], writes=[p])
                m.op("act", lambda e: e.activation(out=rt[:], in_=p[:], func=AF.Sqrt, bias=epsc[:], scale=1.0 / D),
                     reads=[p, epsc], writes=[rt])
                m.op("dve", lambda e: e.reciprocal(out=rstd[:], in_=rt[:]), reads=[rt], writes=[rstd])
                for k in range(KC):
                    m.op("dve", lambda e, k=k: e.scalar_tensor_tensor(
                        out=yt[:, k, :], in0=xt[:, k, :], scalar=fn[:, k:k + 1], in1=rstd[:],
                        op0=ALU.mult, op1=ALU.mult), reads=[xt, fn, rstd], writes=[yt])
                ot = ro.next()
                for blk in range(4):
                    for kh in range(2):
                        pp = nps()
                        for kk in range(4):
                            k = kh * 4 + kk
                            m.op("pe", lambda e, pp=pp, kk=kk, k=k, blk=blk: e.transpose(
                                pp[:, kk * 128:(kk + 1) * 128], yt[:, k, blk * 128:(blk + 1) * 128], ident[:]),
                                reads=[yt, ident], writes=[pp])
                        if (blk + kh) % 2 == 0:
                            m.op("act", lambda e, pp=pp, blk=blk, kh=kh: e.activation(
                                out=ot[:, blk, kh * 512:(kh + 1) * 512], in_=pp[:], func=AF.Copy),
                                reads=[pp], writes=[ot])
                        else:
                            m.op("dve", lambda e, pp=pp, blk=blk, kh=kh: e.tensor_copy(
                                out=ot[:, blk, kh * 512:(kh + 1) * 512], in_=pp[:]), reads=[pp], writes=[ot])
                m.dma("act", out.t[i * 512:(i + 1) * 512, :].rearrange("(b p) d -> p b d", p=128), ot[:],
                      reads=[ot], writes=[out], sembuf=ot)
        barrier()

    convert_layer(0)
    stage0()
    cur = 0
    stop = cfg.stop
    done = False
    for l in range(L):
        last = (l == L - 1)
        ada(l, l == 0)
        stage1(l, XR[cur])
        if stop == (l, 1): done = True; break
        stage2(l, last)
        if stop == (l, 2): done = True; break
        if l + 1 < L:
            convert_layer(l + 1)
        stage3a(l, last)
        stage3b(l, last, XR[cur], XR[1 - cur])
        cur = 1 - cur
        if stop == (l, 3): done = True; break
        stage4(l, last, XR[cur], XR[1 - cur])
        cur = 1 - cur
        if stop == (l, 4): done = True; break
    if not done:
        stage_final(XR[cur])
    nobar.clear()
    barrier()
    ges.close()
    return nc, m


def host_prep(cfg, core, inp):
    NB, S, L = cfg.NB, cfg.S, cfg.L
    f = lambda a: np.ascontiguousarray(a, dtype=np.float32)
    b0 = core * NB
    d = {}
    d["x_in"] = f(inp["x"][b0:b0 + NB].reshape(NB * S, D))
    d["c_in"] = f(inp["ctx"][b0:b0 + NB].reshape(NB * LC, D))
    cv = np.concatenate([inp["c"][b0:b0 + NB], inp["c_ctx"][None]], 0)
    d["cT"] = f(cv.reshape(cfg.R, KC, 128).transpose(2, 1, 0))
    return d


def host_shared(cfg, inp):
    L = cfg.L
    f = lambda a: np.ascontiguousarray(a, dtype=np.float32)
    d = {}
    d["w_mod"] = f(inp["w_mod"])
    d["bmodT"] = f(inp["b_mod"].reshape(L, 48, 128).transpose(0, 2, 1))
    d["n1T"] = f(inp["norm1_g"].reshape(L, KC, 128).transpose(0, 2, 1))
    d["n2T"] = f(inp["norm2_g"].reshape(L, KC, 128).transpose(0, 2, 1))
    d["fnT"] = f(inp["final_norm_g"].reshape(KC, 128).T)
    w_in = inp["w_in"]
    d["w_in"] = f(w_in)
    rs = rot_src()
    qcols = np.concatenate([1280 + hh * 64 + rs for hh in range(8)])
    kk = [np.concatenate([1792 + kv * 64 + np.arange(64)] * 2) for kv in range(2)]
    kkp = [np.concatenate([1792 + kv * 64 + rs] * 2) for kv in range(2)]
    cols = np.concatenate([qcols] + kk + kkp)
    d["w_ex"] = f(w_in[:, :, cols])
    d["convT"] = f(inp["conv_w"].transpose(0, 2, 1).reshape(L, 2, 128, 3).transpose(0, 2, 1, 3))
    d["wsT"] = f(inp["gmlp_ws"].transpose(0, 3, 1, 2))
    gbv = inp["gmlp_b"]
    gb = np.repeat(gbv[:, :, None, :], 64, axis=2)
    d["gb"] = f(gb.reshape(L, 2, 128, 128).transpose(0, 2, 1, 3))
    d["sinkbc"] = f(np.broadcast_to(inp["attn_sink"][:, None, :], (L, 128, NH)))
    d["w_br"] = f(np.concatenate([inp["w_br_conv"], inp["w_br_gmlp"], inp["w_br_attn"]], axis=1))
    d["w_out"] = f(inp["w_out"])
    d["ffn_gu"] = f(inp["ffn_w_gu"])
    d["ffn_d"] = f(inp["ffn_w_d"])
    d["moe_rt"] = f(inp["moe_router"])
    d["moe_gu"] = f(inp["moe_w_gu"])
    d["moe_d"] = f(inp["moe_w_d"])
    tc, ts_ = rope_tabs(cfg)
    d["tabC"], d["tabS"] = tc, ts_
    qi = np.arange(128)[:, None]
    jj = np.arange(384)[None, :]
    d["mask"] = np.where((jj >= qi) & (jj <= qi + 256), 0.0, NEG).astype(np.float32)
    return d


_CACHE = {}


def run(cfg, inp, ncores):
    key = (cfg.NB, cfg.S, cfg.L, cfg.dbg, cfg.stop)
    if key not in _CACHE:
        _CACHE[key] = build(cfg)
    nc, m = _CACHE[key]
    sh = host_shared(cfg, inp)
    in_maps = []
    for c in range(ncores):
        dd = dict(sh)
        dd.update(host_prep(cfg, c, inp))
        in_maps.append(dd)
    res = run_bass_kernel_spmd(nc, in_maps, core_ids=list(range(ncores)))
    return res


def kernel(**inputs):
    cfg = Cfg(NB=2, S=4096, L=4)
    inp = {k: np.asarray(v) for k, v in inputs.items()}
    res = run(cfg, inp, 8)
    outs = [r["out"].reshape(cfg.NB, cfg.S, D) for r in res.results]
    return np.ascontiguousarray(np.concatenate(outs, axis=0), dtype=np.float32)
```

```python
import numpy as np
import concourse.bass as bass
import concourse.mybir as mybir
from contextlib import ExitStack

F32 = mybir.dt.float32
BF16 = mybir.dt.bfloat16
AF = mybir.ActivationFunctionType
ALU = mybir.AluOpType
AX = mybir.AxisListType


class Buf:
    __slots__ = ("name", "t", "writers", "readers", "dsem")

    def __init__(self, name, t=None):
        self.name = name
        self.t = t
        self.writers = {}
        self.readers = {}
        self.dsem = None

    def __getitem__(self, k):
        return self.t[k]


class MK:
    ENG = ("pe", "act", "dve", "pool", "sp")

    def __init__(self, nc, es):
        self.nc = nc
        self.es = es
        self.h = {"pe": nc.tensor, "act": nc.scalar, "dve": nc.vector,
                  "pool": nc.gpsimd, "sp": nc.sync}
        self.sems = {}
        self.issued = {}
        self.seen = {e: {} for e in self.ENG}
        for e in self.ENG:
            self.sems[e] = nc.alloc_semaphore(name="s_" + e)
            self.issued[e] = 0
        self.ndsem = 0
        self.ninstr = 0
        self.stage_bufs = []
        self.free_dsems = []
        self.dkeys = set()

    def sb(self, name, shape, dt):
        t = self.es.enter_context(self.nc.sbuf_tensor(name, list(shape), dt))
        return Buf(name, t)

    def uname(self, name):
        self.uid = getattr(self, "uid", 0) + 1
        return "%s_u%d" % (name, self.uid)

    def track(self, b):
        self.stage_bufs.append(b)
        return b

    def end_stage(self):
        for b in self.stage_bufs:
            if b.dsem is not None:
                self.free_dsems.append(b.dsem)
                b.dsem = None
        self.stage_bufs = []

    def ps(self, name, shape, dt):
        t = self.es.enter_context(self.nc.psum_tensor(name, list(shape), dt))
        return Buf(name, t)

    def dram(self, name, shape, dt, kind="Internal"):
        t = self.nc.dram_tensor(name, list(shape), dt, kind=kind)
        return Buf(name, t.ap())

    def _dsem(self, b):
        if b.dsem is None:
            if self.free_dsems:
                b.dsem = self.free_dsems.pop()
                return b.dsem
            k = "q%d" % self.ndsem
            self.ndsem += 1
            self.sems[k] = self.nc.alloc_semaphore(name="s_" + k)
            self.issued[k] = 0
            self.dkeys.add(k)
            b.dsem = k
        return b.dsem

    def _need(self, eng, reads, writes):
        need = {}

        def add(k, c, kind):
            if k == eng:
                if eng == "pe":
                    return
                if kind == "war":
                    return
            if c > need.get(k, 0):
                need[k] = c

        for b in reads:
            for k, c in b.writers.items():
                add(k, c, "raw")
        for b in writes:
            for k, c in b.writers.items():
                add(k, c, "waw")
            for k, c in b.readers.items():
                add(k, c, "war")
        seen = self.seen[eng]
        hnd = self.h[eng]
        for k, c in need.items():
            if seen.get(k, 0) >= c:
                continue
            if k in self.dkeys:
                c = max(c, self.issued[k])
            hnd.wait_ge(self.sems[k], c)
            seen[k] = c

    def _mark(self, key, cnt, reads, writes):
        for b in writes:
            b.writers = {key: cnt}
            b.readers = {}
        for b in reads:
            if b not in writes:
                b.readers[key] = cnt

    def op(self, eng, fn, reads=(), writes=()):
        self._need(eng, reads, writes)
        ins = fn(self.h[eng])
        self.issued[eng] += 1
        ins.then_inc(self.sems[eng], 1)
        self._mark(eng, self.issued[eng], reads, writes)
        self.ninstr += 1
        return ins

    def dma(self, q, out, in_, reads=(), writes=(), sembuf=None, **kw):
        self._need(q, reads, writes)
        k = self._dsem(sembuf)
        ins = self.h[q].dma_start(out=out, in_=in_, **kw)
        self.issued[k] += 16
        ins.then_inc(self.sems[k], 16)
        self._mark(k, self.issued[k], reads, writes)
        self.ninstr += 1
        return ins

    def wait_all(self, eng, bufs):
        self._need(eng, bufs, ())

from concourse.bass_utils import run_bass_kernel_spmd

D = 1024
KC = 8
LC = 256
NH = 8
E = 8
FD = 2816
FE = 3584
EPS = 1e-6
SCALE = 0.125
NEG = -1e30


class Cfg:
    def __init__(self, NB=2, S=4096, L=4, dbg=False, stop=None):
        self.NB, self.S, self.L, self.dbg, self.stop = NB, S, L, dbg, stop
        self.R = NB + 1
        self.CT = NB * LC
        self.TX = NB * S
        self.TA = self.CT + self.TX


def rope_tabs(cfg):
    S = cfg.S
    rows = S // 64
    row = np.repeat(np.arange(rows), 64).astype(np.float32)
    col = np.tile(np.arange(64), rows).astype(np.float32)
    half = 32
    inv = (1.0 / (10000.0 ** (np.arange(0, half, 2, dtype=np.float32) / half))).astype(np.float32)
    ang_r = row[:, None] * inv[None, :]
    ang_c = col[:, None] * inv[None, :]
    ang = np.concatenate([ang_r, ang_r, ang_c, ang_c], axis=-1)
    cos = np.cos(ang).astype(np.float32).T
    sin = np.sin(ang).astype(np.float32).T
    sgn = np.where((np.arange(64) % 32) < 16, -1.0, 1.0).astype(np.float32)[:, None]
    sins = sin * sgn
    tc = np.ones((128, cfg.TA), np.float32)
    ts_ = np.zeros((128, cfg.TA), np.float32)
    for b in range(cfg.NB):
        o = cfg.CT + b * S
        tc[:, o:o + S] = np.concatenate([cos, cos], 0)
        ts_[:, o:o + S] = np.concatenate([sins, sins], 0)
    return tc, ts_


def rot_src():
    j = np.arange(64)
    return (j // 32) * 32 + ((j % 32) + 16) % 32


def build(cfg):
    NB, S, L, R, CT, TX, TA = cfg.NB, cfg.S, cfg.L, cfg.R, cfg.CT, cfg.TX, cfg.TA
    nc = bass.Bass("TRN2", target_bir_lowering=False)
    ges = ExitStack()
    m = MK(nc, ges)
    EI = "ExternalInput"

    x_in = m.dram("x_in", [TX, D], F32, EI)
    c_in = m.dram("c_in", [CT, D], F32, EI)
    cT_in = m.dram("cT", [128, KC, R], F32, EI)
    w_mod = m.dram("w_mod", [L, D, 6 * D], F32, EI)
    bmodT = m.dram("bmodT", [L, 128, 48], F32, EI)
    n1T = m.dram("n1T", [L, 128, KC], F32, EI)
    n2T = m.dram("n2T", [L, 128, KC], F32, EI)
    fnT = m.dram("fnT", [128, KC], F32, EI)
    w_in = m.dram("w_in", [L, D, 5120], F32, EI)
    w_ex = m.dram("w_ex", [L, D, 1024], F32, EI)
    convT = m.dram("convT", [L, 128, 2, 3], F32, EI)
    wsT_in = m.dram("wsT", [L, 128, 4, 128], F32, EI)
    gb_in = m.dram("gb", [L, 128, 2, 128], F32, EI)
    sink_in = m.dram("sinkbc", [L, 128, NH], F32, EI)
    w_br = m.dram("w_br", [L, D, D], F32, EI)
    w_out = m.dram("w_out", [L, D, D], F32, EI)
    ND = (L + 1) // 2
    NM = max(L // 2, 1)
    ffn_gu = m.dram("ffn_gu", [ND, D, 2 * FD], F32, EI)
    ffn_d = m.dram("ffn_d", [ND, FD, D], F32, EI)
    moe_rt = m.dram("moe_rt", [NM, D, E], F32, EI)
    moe_gu = m.dram("moe_gu", [NM, E, D, 2 * FE], F32, EI)
    moe_d = m.dram("moe_d", [NM, E, FE, D], F32, EI)
    tabC = m.dram("tabC", [128, TA], F32, EI)
    tabS = m.dram("tabS", [128, TA], F32, EI)
    mask_in = m.dram("mask", [128, 384], F32, EI)
    out = m.dram("out", [TX, D], F32, "ExternalOutput")

    OK = "ExternalOutput" if cfg.dbg else "Internal"
    XR = [m.dram("XR%d" % i, [KC, 128, TA], F32, OK) for i in range(2)]
    H1 = m.dram("H1", [KC, 128, TA], BF16, OK)
    BGd = m.dram("BGd", [2, 128, TA], BF16, OK)
    CHd = m.dram("CHd", [2, 128, TA], BF16, OK)
    UGd = m.dram("UGd", [2, 128, TA], BF16, OK)
    Qd = m.dram("Qd", [4, 128, TA], BF16, OK)
    KKd = m.dram("KKd", [2, 128, TA], BF16, OK)
    VNXd = m.dram("VNXd", [TA, 512], BF16, OK)
    VAXd = m.dram("VAXd", [TA, 512], BF16, OK)
    Yd = m.dram("Yd", [KC, 128, TA], BF16, OK)
    MIXd = m.dram("MIXd", [KC, 128, TA], BF16, OK)
    H2d = m.dram("H2d", [KC, 128, TA], BF16, OK)
    GTd = m.dram("GTd", [E, TA], BF16, OK)
    Wb_in = [m.dram("Wb_in%d" % l, [D, 6144], BF16) for l in range(L)]
    Wb_br = [m.dram("Wb_br%d" % l, [D, D], BF16) for l in range(L)]
    Wb_out = [m.dram("Wb_out%d" % l, [D, D], BF16) for l in range(L)]
    Wb_gu, Wb_d = [], []
    for l in range(L):
        if l % 2 == 0:
            Wb_gu.append(m.dram("Wb_gu%d" % l, [1, D, 2 * FD], BF16))
            Wb_d.append(m.dram("Wb_d%d" % l, [1, FD, D], BF16))
        else:
            Wb_gu.append(m.dram("Wb_gu%d" % l, [E, D, 2 * FE], BF16))
            Wb_d.append(m.dram("Wb_d%d" % l, [E, FE, D], BF16))

    ident = m.sb("ident", [128, 128], F32)
    ones_bf = m.sb("ones_bf", [128, 128], BF16)
    epsc = m.sb("epsc", [128, 1], F32)
    m.op("pool", lambda e: e.memset(ident[:], 0.0), writes=[ident])
    m.op("pool", lambda e: e.affine_select(out=ident[:], in_=ident[:], pattern=[[-1, 128]],
                                            compare_op=ALU.not_equal, fill=1.0, base=0,
                                            channel_multiplier=1), reads=[ident], writes=[ident])
    m.op("pool", lambda e: e.memset(ones_bf[:], 1.0), writes=[ones_bf])
    m.op("pool", lambda e: e.memset(epsc[:], EPS), writes=[epsc])
    psall_t = ges.enter_context(nc.psum_tensor("psall", [128, 8, 512], F32))
    psall = psall_t[:]
    PS = [Buf("ps%d" % i, psall[:, i, :]) for i in range(8)]
    modT = m.sb("modT", [128, R, 48], F32)
    A1 = m.sb("A1", [128, R, KC], F32)
    A2 = m.sb("A2", [128, R, KC], F32)
    csil = m.sb("csil", [128, KC, R], F32)
    cst = m.sb("cst", [128, KC, R], F32)
    bmod = m.sb("bmod", [128, 48], F32)
    n1 = m.sb("n1", [128, KC], F32)
    n2 = m.sb("n2", [128, KC], F32)

    state = {"psi": 0}

    def nps():
        p = PS[state["psi"] % 8]
        state["psi"] += 1
        return p

    class Ring:
        def __init__(self, name, n, shape, dt, es=None):
            self.slots = []
            for i in range(n):
                nm = m.uname("%s_%d" % (name, i))
                t = (es or ges).enter_context(nc.sbuf_tensor(nm, list(shape), dt))
                self.slots.append(m.track(Buf(nm, t)))
            self.i = 0

        def next(self):
            s = self.slots[self.i % len(self.slots)]
            self.i += 1
            return s

    def stage_sb(es, name, shape, dt):
        nm = m.uname(name)
        t = es.enter_context(nc.sbuf_tensor(nm, list(shape), dt))
        return m.track(Buf(nm, t))

    nobar = set()

    def barrier():
        for e in MK.ENG:
            for k in list(m.sems.keys()):
                if k == e or k in nobar:
                    continue
                c = m.issued[k]
                if c > m.seen[e].get(k, 0):
                    m.h[e].wait_ge(m.sems[k], c)
                    m.seen[e][k] = c
        m.end_stage()

    cvb = Buf("cvsem")

    def conv_w(dst, dst_ap, src, src_ap):
        m.dma("pool", dst_ap, src_ap, reads=[src], writes=[dst], sembuf=dst)
        nobar.add(dst.dsem)

    conv_w_impl = [conv_w]

    def conv_pop(n):
        for _ in range(n):
            if conv_q:
                conv_w(*conv_q.pop(0))

    def v2(ap, rows):
        return ap.rearrange("(p r) n -> p (r n)", p=128)

    conv_q = []

    def convert_layer(l, defer=False):
        if defer:
            jobs = []
            real = conv_w_impl[0]
            conv_w_impl[0] = lambda *a: jobs.append(a)
            convert_layer(l)
            conv_w_impl[0] = real
            conv_q.extend(jobs)
            return
        cw_ = lambda *a: conv_w_impl[0](*a)
        cw_(Wb_in[l], Wb_in[l][:, 0:5120], w_in, w_in[l])
        cw_(Wb_in[l], Wb_in[l][:, 5120:6144], w_ex, w_ex[l])
        cw_(Wb_br[l], v2(Wb_br[l][:], D), w_br, v2(w_br[l], D))
        cw_(Wb_out[l], v2(Wb_out[l][:], D), w_out, v2(w_out[l], D))
        if l % 2 == 0:
            cw_(Wb_gu[l], v2(Wb_gu[l][0], D), ffn_gu, v2(ffn_gu[l // 2], D))
            cw_(Wb_d[l], v2(Wb_d[l][0], FD), ffn_d, v2(ffn_d[l // 2], FD))
        else:
            for e in range(E):
                cw_(Wb_gu[l], v2(Wb_gu[l][e], D), moe_gu, v2(moe_gu[l // 2, e], D))
                cw_(Wb_d[l], v2(Wb_d[l][e], FE), moe_d, v2(moe_d[l // 2, e], FE))

    def tiles512():
        tl = [(0, CT, R - 1)] if CT <= 512 else [(i * 512, 512, R - 1) for i in range(CT // 512)]
        for b in range(NB):
            for i in range(S // 512):
                tl.append((CT + b * S + i * 512, 512, b))
        return tl

    def fm(dr, t0, ts, k0=0, k1=None):
        k1 = dr.t.shape[0] if k1 is None else k1
        return dr.t[k0:k1, :, t0:t0 + ts].rearrange("k p t -> p k t")

    def stage0():
        with ExitStack() as es:
            rin = Ring("s0in", 2, [128, 4, D], F32, es)
            rout = Ring("s0out", 2, [128, KC, 512], F32, es)
            srcs = [(c_in, i * 512, min(512, CT - i * 512), i * 512) for i in range((CT + 511) // 512)]
            srcs += [(x_in, i * 512, 512, CT + i * 512) for i in range(TX // 512)]
            for (src, r0, ts, t0) in srcs:
                nb = ts // 128
                it = rin.next()
                m.dma("sp", it[:, 0:nb, :], src.t[r0:r0 + ts, :].rearrange("(b p) d -> p b d", p=128),
                      reads=[src], writes=[it], sembuf=it)
                ot = rout.next()
                for blk in range(nb):
                    for kh in range(2):
                        p = nps()
                        for kk in range(4):
                            k = kh * 4 + kk
                            m.op("pe", lambda e, p=p, kk=kk, k=k, blk=blk: e.transpose(
                                p[:, kk * 128:(kk + 1) * 128], it[:, blk, k * 128:(k + 1) * 128], ident[:]),
                                reads=[it, ident], writes=[p])
                        eng = "act" if (blk + kh) % 2 == 0 else "dve"
                        src_v = p[:].rearrange("p (k t) -> p k t", k=4)
                        dst_v = ot[:, kh * 4:(kh + 1) * 4, blk * 128:(blk + 1) * 128]
                        if eng == "act":
                            m.op("act", lambda e, a=dst_v, b=src_v: e.activation(out=a, in_=b, func=AF.Copy),
                                 reads=[p], writes=[ot])
                        else:
                            m.op("dve", lambda e, a=dst_v, b=src_v: e.tensor_copy(out=a, in_=b),
                                 reads=[p], writes=[ot])
                m.dma("act", fm(XR[0], t0, ts), ot[:, :, 0:ts], reads=[ot], writes=[XR[0]], sembuf=ot)
        barrier()

    def ada(l, first):
        with ExitStack() as es:
            rw = Ring("adaw", 2, [128, KC, 512], F32, es)
            if first:
                m.dma("sp", cst[:], cT_in[:], reads=[cT_in], writes=[cst], sembuf=cst)
                m.op("act", lambda e: e.activation(out=csil[:], in_=cst[:], func=AF.Silu),
                     reads=[cst], writes=[csil])
            m.dma("sp", bmod[:], bmodT[l], reads=[bmodT], writes=[bmod], sembuf=bmod)
            m.dma("sp", n1[:], n1T[l], reads=[n1T], writes=[n1], sembuf=n1)
            m.dma("sp", n2[:], n2T[l], reads=[n2T], writes=[n2], sembuf=n2)
            for pc in range(12):
                wt = rw.next()
                m.dma("sp", wt[:], w_mod.t[l, :, pc * 512:(pc + 1) * 512].rearrange("(k p) n -> p k n", p=128),
                      reads=[w_mod], writes=[wt], sembuf=wt)
                p = nps()
                for jj in range(4):
                    for k in range(KC):
                        m.op("pe", lambda e, p=p, jj=jj, k=k: e.matmul(
                            p[:, jj * R:(jj + 1) * R], lhsT=wt[:, k, jj * 128:(jj + 1) * 128], rhs=csil[:, k, :],
                            start=(k == 0), stop=(k == KC - 1)), reads=[wt, csil], writes=[p])
                for jj in range(4):
                    ch = pc * 4 + jj
                    m.op("act", lambda e, p=p, jj=jj, ch=ch: e.activation(
                        out=modT[:, :, ch], in_=p[:, jj * R:(jj + 1) * R], func=AF.Identity,
                        bias=bmod[:, ch:ch + 1], scale=1.0), reads=[p, bmod], writes=[modT])
            for r in range(R):
                m.op("dve", lambda e, r=r: e.scalar_tensor_tensor(
                    out=A1[:, r, :], in0=modT[:, r, 8:16], scalar=1.0, in1=n1[:], op0=ALU.add, op1=ALU.mult),
                    reads=[modT, n1], writes=[A1])
                m.op("dve", lambda e, r=r: e.scalar_tensor_tensor(
                    out=A2[:, r, :], in0=modT[:, r, 32:40], scalar=1.0, in1=n2[:], op0=ALU.add, op1=ALU.mult),
                    reads=[modT, n2], writes=[A2])
        barrier()

    def norm_mod(xt, ts, Aap, Bap, sqb, rt, rstd, tmp, hdst, hf=None):
        m.op("act", lambda e: e.activation(out=sqb[:, :, 0:ts], in_=xt[:, :, 0:ts], func=AF.Square),
             reads=[xt], writes=[sqb])
        p = nps()
        for k in range(KC):
            m.op("pe", lambda e, k=k: e.matmul(p[:, 0:ts], lhsT=ones_bf[:], rhs=sqb[:, k, 0:ts],
                                               start=(k == 0), stop=(k == KC - 1)),
                 reads=[ones_bf, sqb], writes=[p])
        m.op("act", lambda e: e.activation(out=rt[:, 0:ts], in_=p[:, 0:ts], func=AF.Sqrt,
                                           bias=epsc[:], scale=1.0 / D), reads=[p, epsc], writes=[rt])
        m.op("dve", lambda e: e.reciprocal(out=rstd[:, 0:ts], in_=rt[:, 0:ts]), reads=[rt], writes=[rstd])
        for k in range(KC):
            m.op("dve", lambda e, k=k: e.scalar_tensor_tensor(
                out=tmp[:, k, 0:ts], in0=xt[:, k, 0:ts], scalar=Aap(k), in1=rstd[:, 0:ts],
                op0=ALU.mult, op1=ALU.mult), reads=[xt, rstd, A1, A2], writes=[tmp])
            if hf is None:
                m.op("act", lambda e, k=k: e.activation(out=hdst[:, k, 0:ts], in_=tmp[:, k, 0:ts],
                                                        func=AF.Identity, bias=Bap(k), scale=1.0),
                     reads=[tmp, modT], writes=[hdst])
            else:
                m.op("act", lambda e, k=k: e.activation(out=hf[:, k, 0:ts], in_=tmp[:, k, 0:ts],
                                                        func=AF.Identity, bias=Bap(k), scale=1.0),
                     reads=[tmp, modT], writes=[hf])
        if hf is not None:
            m.op("pool", lambda e: e.tensor_copy(out=hdst[:, :, 0:ts], in_=hf[:, :, 0:ts]),
                 reads=[hf], writes=[hdst])

    def stage1(l, xr):
        with ExitStack() as es:
            w1 = stage_sb(es, "w1", [128, KC, 3072], BF16)
            wv = Wb_in[l].t.rearrange("(k p) n -> p k n", p=128)
            m.dma("sp", w1[:, :, 0:2048], wv[:, :, 0:2048], reads=[Wb_in[l]], writes=[w1], sembuf=w1)
            m.dma("sp", w1[:, :, 2048:3072], wv[:, :, 5120:6144], reads=[Wb_in[l]], writes=[w1], sembuf=w1)
            rx = Ring("s1x", 2, [128, KC, 512], F32, es)
            rtab = Ring("s1tab", 2, [128, 2, 512], F32, es)
            sqb = stage_sb(es, "s1sq", [128, KC, 512], BF16)
            rt = stage_sb(es, "s1rt", [128, 512], F32)
            rstd = stage_sb(es, "s1rstd", [128, 512], F32)
            tmp = stage_sb(es, "s1tmp", [128, KC, 512], F32)
            rh = Ring("s1h", 2, [128, KC, 512], BF16, es)
            rfm = Ring("s1fm", 2, [128, 12, 512], BF16, es)
            cg = Ring("s1cg", 2, [128, 512], BF16, es)
            t1r = Ring("s1t1", 2, [128, 512], F32, es)
            t2r = Ring("s1t2", 2, [128, 512], F32, es)
            rvn = Ring("s1vn", 2, [128, 4, 512], BF16, es)
            rva = Ring("s1va", 2, [128, 4, 512], BF16, es)
            vg = Ring("s1vg", 2, [128, 256], F32, es)
            st6 = Ring("s1st", 2, [128, 6], F32, es)
            mv = Ring("s1mv", 2, [128, 2], F32, es)
            sd = Ring("s1sd", 2, [128, 1], F32, es)
            rs = Ring("s1rs", 2, [128, 1], F32, es)
            for s_ in rvn.slots + rva.slots:
                m.op("pool", lambda e, s_=s_: e.memset(s_[:], 0.0), writes=[s_])
            def front(t0, ts, r):
                xt = rx.next()
                m.dma("sp", xt[:, :, 0:ts], fm(xr, t0, ts), reads=[xr], writes=[xt], sembuf=xt)
                tb = rtab.next()
                m.dma("sp", tb[:, 0, 0:ts], tabC[:, t0:t0 + ts], reads=[tabC], writes=[tb], sembuf=tb)
                m.dma("sp", tb[:, 1, 0:ts], tabS[:, t0:t0 + ts], reads=[tabS], writes=[tb], sembuf=tb)
                h = rh.next()
                norm_mod(xt, ts, lambda k: A1[:, r, k:k + 1], lambda k: modT[:, r, k:k + 1],
                         sqb, rt, rstd, tmp, h)
                m.dma("act", fm(H1, t0, ts), h[:, :, 0:ts], reads=[h], writes=[H1], sembuf=h)
                return h, tb

            tls = tiles512()
            nxt = front(*tls[0])
            for ti, (t0, ts, r) in enumerate(tls):
                nb = ts // 128
                h, tb = nxt
                if ti + 1 < len(tls):
                    nxt = front(*tls[ti + 1])
                f = rfm.next()

                def proj(co):
                    p = nps()
                    for k in range(KC):
                        m.op("pe", lambda e, k=k: e.matmul(p[:, 0:ts], lhsT=w1[:, k, co:co + 128],
                                                           rhs=h[:, k, 0:ts], start=(k == 0), stop=(k == KC - 1)),
                             reads=[w1, h], writes=[p])
                    return p
                for j in range(2):
                    p = proj(0 + j * 128)
                    m.op("act", lambda e, p=p, j=j: e.activation(out=f[:, 0 + j, 0:ts], in_=p[:, 0:ts], func=AF.Copy),
                         reads=[p], writes=[f])
                for j in range(2):
                    p = proj(256 + j * 128)
                    c_ = cg.next()
                    m.op("act", lambda e, p=p, c_=c_: e.activation(out=c_[:, 0:ts], in_=p[:, 0:ts], func=AF.Copy),
                         reads=[p], writes=[c_])
                    p2 = proj(512 + j * 128)
                    m.op("dve", lambda e, p2=p2, c_=c_, j=j: e.tensor_tensor(
                        out=f[:, 2 + j, 0:ts], in0=p2[:, 0:ts], in1=c_[:, 0:ts], op=ALU.mult),
                        reads=[p2, c_], writes=[f])
                for j in range(2):
                    p = proj(768 + j * 128)
                    m.op("act", lambda e, p=p, j=j: e.activation(out=f[:, 4 + j, 0:ts], in_=p[:, 0:ts],
                                                                 func=AF.Gelu_apprx_tanh), reads=[p], writes=[f])
                for (co, cop, fo, n) in ((1280, 2048, 6, 4), (2560, 2816, 10, 2)):
                    for j in range(n):
                        p = proj(co + j * 128)
                        pp = proj(cop + j * 128)
                        a1 = t1r.next()
                        a2 = t2r.next()
                        m.op("dve", lambda e, p=p, a1=a1: e.tensor_tensor(
                            out=a1[:, 0:ts], in0=p[:, 0:ts], in1=tb[:, 0, 0:ts], op=ALU.mult),
                            reads=[p, tb], writes=[a1])
                        m.op("dve", lambda e, pp=pp, a2=a2: e.tensor_tensor(
                            out=a2[:, 0:ts], in0=pp[:, 0:ts], in1=tb[:, 1, 0:ts], op=ALU.mult),
                            reads=[pp, tb], writes=[a2])
                        m.op("pool", lambda e, a1=a1, a2=a2, fo=fo, j=j: e.tensor_tensor(
                            out=f[:, fo + j, 0:ts], in0=a1[:, 0:ts], in1=a2[:, 0:ts], op=ALU.add),
                            reads=[a1, a2], writes=[f])
                m.dma("act", fm(BGd, t0, ts), f[:, 0:2, 0:ts], reads=[f], writes=[BGd], sembuf=f)
                m.dma("act", fm(CHd, t0, ts), f[:, 2:4, 0:ts], reads=[f], writes=[CHd], sembuf=f)
                m.dma("act", fm(UGd, t0, ts), f[:, 4:6, 0:ts], reads=[f], writes=[UGd], sembuf=f)
                m.dma("act", fm(Qd, t0, ts), f[:, 6:10, 0:ts], reads=[f], writes=[Qd], sembuf=f)
                m.dma("act", fm(KKd, t0, ts), f[:, 10:12, 0:ts], reads=[f], writes=[KKd], sembuf=f)
                vn = rvn.next()
                va = rva.next()
                for blk in range(nb):
                    pv = nps()
                    for k in range(KC):
                        m.op("pe", lambda e, k=k, pv=pv, blk=blk: e.matmul(
                            pv[:, 0:256], lhsT=h[:, k, blk * 128:(blk + 1) * 128], rhs=w1[:, k, 1024:1280],
                            start=(k == 0), stop=(k == KC - 1)), reads=[w1, h], writes=[pv])
                    pa = nps()
                    for k in range(KC):
                        m.op("pe", lambda e, k=k, pa=pa, blk=blk: e.matmul(
                            pa[:, 0:128], lhsT=h[:, k, blk * 128:(blk + 1) * 128], rhs=w1[:, k, 1920:2048],
                            start=(k == 0), stop=(k == KC - 1)), reads=[w1, h], writes=[pa])
                    g_ = vg.next()
                    m.op("act", lambda e, pv=pv, g_=g_: e.activation(out=g_[:], in_=pv[:, 0:256],
                                                                     func=AF.Gelu_apprx_tanh),
                         reads=[pv], writes=[g_])
                    s6 = st6.next()
                    m.op("dve", lambda e, g_=g_, s6=s6: e.bn_stats(out=s6[:], in_=g_[:]), reads=[g_], writes=[s6])
                    mv_ = mv.next()
                    m.op("dve", lambda e, mv_=mv_, s6=s6: e.bn_aggr(out=mv_[:], in_=s6[:]), reads=[s6], writes=[mv_])
                    sd_ = sd.next()
                    m.op("act", lambda e, sd_=sd_, mv_=mv_: e.activation(out=sd_[:], in_=mv_[:, 1:2], func=AF.Sqrt,
                                                                         bias=epsc[:], scale=1.0),
                         reads=[mv_, epsc], writes=[sd_])
                    rs_ = rs.next()
                    m.op("dve", lambda e, sd_=sd_, rs_=rs_: e.reciprocal(out=rs_[:], in_=sd_[:]),
                         reads=[sd_], writes=[rs_])
                    for par in range(2):
                        src = g_[:].rearrange("p (g c) -> p g c", c=64)[:, par::2, :]
                        dst = vn[:, blk, :].rearrange("p (g c) -> p g c", c=128)[:, par::2, par * 64:(par + 1) * 64]
                        m.op("dve", lambda e, src=src, dst=dst, mv_=mv_, rs_=rs_: e.tensor_scalar(
                            out=dst, in0=src, scalar1=mv_[:, 0:1], scalar2=rs_[:, 0:1],
                            op0=ALU.subtract, op1=ALU.mult), reads=[g_, mv_, rs_], writes=[vn])
                        srca = pa[:, 0:128].rearrange("p (kv c) -> p kv c", c=64)
                        dsta = va[:, blk, :].rearrange("p (kv q c) -> p kv q c", kv=2, q=2)[:, :, par, par * 64:(par + 1) * 64]
                        m.op("act", lambda e, srca=srca, dsta=dsta: e.activation(out=dsta, in_=srca, func=AF.Copy),
                             reads=[pa], writes=[va])
                m.dma("act", VNXd.t[t0:t0 + ts, :].rearrange("(b p) c -> p b c", p=128), vn[:, 0:nb, :],
                      reads=[vn], writes=[VNXd], sembuf=vn)
                m.dma("act", VAXd.t[t0:t0 + ts, :].rearrange("(b p) c -> p b c", p=128), va[:, 0:nb, :],
                      reads=[va], writes=[VAXd], sembuf=va)
        barrier()

    def stage2(l, last):
        with ExitStack() as es:
            cw = stage_sb(es, "s2cw", [128, 2, 3], F32)
            wsf = stage_sb(es, "s2wsf", [128, 4, 128], F32)
            wsb = stage_sb(es, "s2wsb", [128, 4, 128], BF16)
            gbt = stage_sb(es, "s2gb", [128, 2, 128], F32)
            snk = stage_sb(es, "s2snk", [128, NH], F32)
            nsnk = stage_sb(es, "s2nsnk", [128, NH], F32)
            msk = stage_sb(es, "s2msk", [128, 384], F32)
            m.dma("sp", cw[:], convT[l], reads=[convT], writes=[cw], sembuf=cw)
            m.dma("sp", wsf[:], wsT_in[l], reads=[wsT_in], writes=[wsf], sembuf=wsf)
            m.dma("sp", gbt[:], gb_in[l], reads=[gb_in], writes=[gbt], sembuf=gbt)
            m.dma("sp", snk[:], sink_in[l], reads=[sink_in], writes=[snk], sembuf=snk)
            m.dma("sp", msk[:], mask_in[:], reads=[mask_in], writes=[msk], sembuf=msk)
            m.op("dve", lambda e: e.tensor_copy(out=wsb[:], in_=wsf[:]), reads=[wsf], writes=[wsb])
            m.op("dve", lambda e: e.tensor_scalar(out=nsnk[:], in0=snk[:], scalar1=-1.0, scalar2=None,
                                                  op0=ALU.mult), reads=[snk], writes=[nsnk])
            kkc = stage_sb(es, "s2kkc", [128, 2, LC], BF16)
            vaxc = stage_sb(es, "s2vaxc", [128, 2, 512], BF16)
            rch = Ring("s2ch", 2, [128, 2, 514], BF16, es)
            rbg = Ring("s2bg", 2, [128, 2, 512], BF16, es)
            rug = Ring("s2ug", 2, [128, 2, 512], BF16, es)
            rvn = Ring("s2vn", 2, [128, 4, 512], BF16, es)
            rq = Ring("s2q", 2, [128, 4, 512], BF16, es)
            rkk = Ring("s2kk", 2, [128, 2, 768], BF16, es)
            rvx = Ring("s2vx", 2, [128, 6, 512], BF16, es)
            ry = Ring("s2y", 2, [128, KC, 512], BF16, es)
            acc = Ring("s2acc", 2, [128, 512], F32, es)
            gt = Ring("s2gt", 2, [128, 2, 128], F32, es)
            sm4 = Ring("s2sm", 2, [128, 4, 640], F32, es)
            pe4 = Ring("s2pe", 2, [128, 4, 640], F32, es)
            pn4 = Ring("s2pn", 2, [128, 4, 640], F32, es)
            pT4 = Ring("s2pT", 2, [128, 4, 5, 128], BF16, es)
            sc4 = [Ring("s2sc%d" % i, 2, [128, 4], F32, es) for i in range(7)]
            for b in range(NB):
                m.dma("sp", kkc[:], fm(KKd, b * LC, LC), reads=[KKd], writes=[kkc], sembuf=kkc)
                m.dma("sp", vaxc[:], VAXd.t[b * LC:(b + 1) * LC, :].rearrange("(b p) c -> p b c", p=128),
                      reads=[VAXd], writes=[vaxc], sembuf=vaxc)
                tl = []
                if not last:
                    tl.append((b * LC, LC, 0, LC, True))
                for i in range(S // 512):
                    tl.append((CT + b * S + i * 512, 512, i * 512, S, False))
                for (t0, ts, s0, slen, isctx) in tl:
                    nb = ts // 128
                    hl = 1 if s0 > 0 else 0
                    hr = 1 if s0 + ts < slen else 0
                    ch = rch.next()
                    if not hl:
                        m.op("pool", lambda e, ch=ch: e.memset(ch[:, :, 0:1], 0.0), writes=[ch])
                    if not hr:
                        m.op("pool", lambda e, ch=ch: e.memset(ch[:, :, ts + 1:ts + 2], 0.0), writes=[ch])
                    m.dma("sp", ch[:, :, 1 - hl:ts + 1 + hr], fm(CHd, t0 - hl, ts + hl + hr),
                          reads=[CHd], writes=[ch], sembuf=ch)
                    bg = rbg.next()
                    m.dma("sp", bg[:, :, 0:ts], fm(BGd, t0, ts), reads=[BGd], writes=[bg], sembuf=bg)
                    ug = rug.next()
                    m.dma("sp", ug[:, :, 0:ts], fm(UGd, t0, ts), reads=[UGd], writes=[ug], sembuf=ug)
                    vn = rvn.next()
                    m.dma("sp", vn[:, 0:nb, :], VNXd.t[t0:t0 + ts, :].rearrange("(b p) c -> p b c", p=128),
                          reads=[VNXd], writes=[vn], sembuf=vn)
                    q = rq.next()
                    m.dma("sp", q[:, :, 0:ts], fm(Qd, t0, ts), reads=[Qd], writes=[q], sembuf=q)
                    kk = vx = None
                    if not isctx:
                        kl = 128 if s0 > 0 else 0
                        kr = 128 if s0 + ts < slen else 0
                        kk = rkk.next()
                        m.dma("sp", kk[:, :, 128 - kl:128 + ts + kr], fm(KKd, t0 - kl, ts + kl + kr),
                              reads=[KKd], writes=[kk], sembuf=kk)
                        vx = rvx.next()
                        nbl = (kl + ts + kr) // 128
                        b0 = 1 - kl // 128
                        m.dma("sp", vx[:, b0:b0 + nbl, :],
                              VAXd.t[t0 - kl:t0 + ts + kr, :].rearrange("(b p) c -> p b c", p=128),
                              reads=[VAXd], writes=[vx], sembuf=vx)
                    y = ry.next()
                    for j in range(2):
                        a = acc.next()
                        m.op("dve", lambda e, a=a, j=j: e.tensor_scalar(
                            out=a[:, 0:ts], in0=ch[:, j, 1:ts + 1], scalar1=cw[:, j, 1:2], scalar2=None,
                            op0=ALU.mult), reads=[ch, cw], writes=[a])
                        m.op("dve", lambda e, a=a, j=j: e.scalar_tensor_tensor(
                            out=a[:, 0:ts], in0=ch[:, j, 0:ts], scalar=cw[:, j, 0:1], in1=a[:, 0:ts],
                            op0=ALU.mult, op1=ALU.add), reads=[ch, cw, a], writes=[a])
                        m.op("dve", lambda e, a=a, j=j: e.scalar_tensor_tensor(
                            out=a[:, 0:ts], in0=ch[:, j, 2:ts + 2], scalar=cw[:, j, 2:3], in1=a[:, 0:ts],
                            op0=ALU.mult, op1=ALU.add), reads=[ch, cw, a], writes=[a])
                        m.op("pool", lambda e, a=a, j=j: e.tensor_tensor(
                            out=y[:, j, 0:ts], in0=a[:, 0:ts], in1=bg[:, j, 0:ts], op=ALU.mult),
                            reads=[a, bg], writes=[y])
                    for blk in range(nb):
                        p = nps()
                        for j in range(2):
                            for gg in range(2):
                                g = 2 * j + gg
                                m.op("pe", lambda e, p=p, j=j, g=g, gg=gg, blk=blk: e.matmul(
                                    p[:, j * 128:(j + 1) * 128], lhsT=vn[:, blk, g * 128:(g + 1) * 128],
                                    rhs=wsb[:, g, :], start=(gg == 0), stop=(gg == 1)),
                                    reads=[vn, wsb], writes=[p])
                        g_ = gt.next()
                        m.op("dve", lambda e, p=p, g_=g_: e.tensor_tensor(
                            out=g_[:], in0=p[:, 0:256].rearrange("p (j t) -> p j t", j=2), in1=gbt[:],
                            op=ALU.add), reads=[p, gbt], writes=[g_])
                        m.op("pool", lambda e, g_=g_, blk=blk: e.tensor_tensor(
                            out=y[:, 2:4, blk * 128:(blk + 1) * 128], in0=g_[:],
                            in1=ug[:, :, blk * 128:(blk + 1) * 128], op=ALU.mult),
                            reads=[g_, ug], writes=[y])
                    for blk in range(nb):
                        nbk = (s0 // 128) + blk
                        if isctx:
                            lo = hi = 384
                        else:
                            lo = 128 if nbk == 0 else 0
                            hi = 256 if nbk == slen // 128 - 1 else 384
                        kbs = list(range(lo // 128, hi // 128))
                        for kv in range(2):
                            h0 = kv * 4
                            A = PS[0:4]
                            Bk = PS[4:6]
                            O = PS[6:8]
                            s4 = sm4.next()
                            if hi == lo:
                                m.op("pool", lambda e: e.memset(s4[:, :, 0:384], NEG), writes=[s4])
                            else:
                                if lo > 0:
                                    m.op("pool", lambda e: e.memset(s4[:, :, 0:lo], NEG), writes=[s4])
                                if hi < 384:
                                    m.op("pool", lambda e: e.memset(s4[:, :, hi:384], NEG), writes=[s4])
                            for hq in range(4):
                                hh = h0 + hq
                                c = hh // 2
                                pb = (hh % 2) * 64
                                if hi > lo:
                                    m.op("pe", lambda e: e.matmul(
                                        A[hq][:, lo:hi], lhsT=q[pb:pb + 64, c, blk * 128:(blk + 1) * 128],
                                        rhs=kk[pb:pb + 64, kv, blk * 128 + lo:blk * 128 + hi], start=True, stop=True),
                                        reads=[q, kk], writes=[A[hq]])
                                m.op("pe", lambda e: e.matmul(
                                    Bk[hq % 2][:, (hq // 2) * 256:(hq // 2) * 256 + 256],
                                    lhsT=q[pb:pb + 64, c, blk * 128:(blk + 1) * 128],
                                    rhs=kkc[pb:pb + 64, kv, :], start=True, stop=True),
                                    reads=[q, kkc], writes=[Bk[hq % 2]])
                            if hi > lo:
                                m.op("dve", lambda e: e.tensor_tensor(
                                    out=s4[:, :, lo:hi], in0=psall[:, 0:4, lo:hi],
                                    in1=msk[:, lo:hi].unsqueeze(1).to_broadcast([128, 4, hi - lo]), op=ALU.add),
                                    reads=A + [msk], writes=[s4])
                            m.op("act", lambda e: e.activation(
                                out=s4[:, :, 384:640].rearrange("p (h b) c -> p h b c", b=2),
                                in_=psall[:, 4:6, :].rearrange("p b (h c) -> p h b c", h=2),
                                func=AF.Copy), reads=Bk, writes=[s4])
                            mx = sc4[0].next()
                            m.op("dve", lambda e: e.tensor_reduce(out=mx[:], in_=s4[:], axis=AX.X, op=ALU.max),
                                 reads=[s4], writes=[mx])
                            ngm = sc4[1].next()
                            m.op("dve", lambda e: e.scalar_tensor_tensor(
                                out=ngm[:], in0=mx[:], scalar=-SCALE, in1=nsnk[:, h0:h0 + 4], op0=ALU.mult, op1=ALU.min),
                                reads=[mx, nsnk], writes=[ngm])
                            p4 = pe4.next()
                            for hq in range(4):
                                m.op("act", lambda e: e.activation(
                                    out=p4[:, hq, :], in_=s4[:, hq, :], func=AF.Exp, bias=ngm[:, hq:hq + 1], scale=SCALE),
                                    reads=[s4, ngm], writes=[p4])
                            tt = sc4[2].next()
                            m.op("dve", lambda e: e.tensor_tensor(out=tt[:], in0=snk[:, h0:h0 + 4], in1=ngm[:], op=ALU.add),
                                 reads=[snk, ngm], writes=[tt])
                            es_ = sc4[3].next()
                            m.op("act", lambda e: e.activation(out=es_[:], in_=tt[:], func=AF.Exp), reads=[tt], writes=[es_])
                            rsum = sc4[4].next()
                            m.op("dve", lambda e: e.tensor_reduce(out=rsum[:], in_=p4[:], axis=AX.X, op=ALU.add),
                                 reads=[p4], writes=[rsum])
                            den = sc4[5].next()
                            m.op("dve", lambda e: e.tensor_tensor(out=den[:], in0=rsum[:], in1=es_[:], op=ALU.add),
                                 reads=[rsum, es_], writes=[den])
                            inv = sc4[6].next()
                            m.op("dve", lambda e: e.reciprocal(out=inv[:], in_=den[:]), reads=[den], writes=[inv])
                            n4 = pn4.next()
                            m.op("dve", lambda e: e.tensor_tensor(
                                out=n4[:], in0=p4[:], in1=inv[:].unsqueeze(2).to_broadcast([128, 4, 640]), op=ALU.mult),
                                reads=[p4, inv], writes=[n4])
                            for hq in range(4):
                                for kb in kbs:
                                    m.op("pe", lambda e: e.transpose(
                                        A[hq][:, kb * 128:(kb + 1) * 128], n4[:, hq, kb * 128:(kb + 1) * 128], ident[:]),
                                        reads=[n4, ident], writes=[A[hq]])
                                for cb in range(2):
                                    o_ = (hq // 2) * 256 + cb * 128
                                    m.op("pe", lambda e: e.transpose(
                                        Bk[hq % 2][:, o_:o_ + 128], n4[:, hq, 384 + cb * 128:384 + (cb + 1) * 128], ident[:]),
                                        reads=[n4, ident], writes=[Bk[hq % 2]])
                            pt = pT4.next()
                            if kbs:
                                k0, k1 = kbs[0], kbs[-1] + 1
                                m.op("dve", lambda e: e.tensor_copy(
                                    out=pt[:, :, k0:k1, :],
                                    in_=psall[:, 0:4, k0 * 128:k1 * 128].rearrange("p h (k t) -> p h k t", t=128)),
                                    reads=A, writes=[pt])
                            for h2_ in range(2):
                                m.op("act", lambda e: e.activation(
                                    out=pt[:, 2 * h2_:2 * h2_ + 2, 3:5, :],
                                    in_=psall[:, 4:6, h2_ * 256:(h2_ + 1) * 256].rearrange("p b (k t) -> p b k t", k=2),
                                    func=AF.Copy), reads=Bk, writes=[pt])
                            for cp in range(2):
                                pO = O[cp]
                                first = True
                                for par in range(2):
                                    hq = 2 * cp + par
                                    seq = [(vx, blk + kb, kb) for kb in kbs] + [(vaxc, cb, 3 + cb) for cb in range(2)]
                                    for i_, (vb, vi, pi) in enumerate(seq):
                                        lastmm = (par == 1 and i_ == len(seq) - 1)
                                        m.op("pe", lambda e: e.matmul(
                                            pO[:, 0:128], lhsT=vb[:, vi, kv * 256 + par * 128:kv * 256 + (par + 1) * 128],
                                            rhs=pt[:, hq, pi, :], start=first, stop=lastmm),
                                            reads=[vb, pt], writes=[pO])
                                        first = False
                            m.op("act", lambda e: e.activation(
                                out=y[:, 4 + kv * 2:6 + kv * 2, blk * 128:(blk + 1) * 128], in_=psall[:, 6:8, 0:128],
                                func=AF.Copy), reads=O, writes=[y])
                    m.dma("act", fm(Yd, t0, ts), y[:, :, 0:ts], reads=[y], writes=[Yd], sembuf=y)
        barrier()

    def stage3a(l, last):
        with ExitStack() as es:
            wg = stage_sb(es, "s3wg", [128, KC, 3072], BF16)
            wbr = stage_sb(es, "s3wbr", [128, KC, D], BF16)
            wv = Wb_in[l].t.rearrange("(k p) n -> p k n", p=128)
            m.dma("sp", wg[:], wv[:, :, 2048:5120], reads=[Wb_in[l]], writes=[wg], sembuf=wg)
            m.dma("sp", wbr[:], Wb_br[l].t.rearrange("(k p) n -> p k n", p=128), reads=[Wb_br[l]], writes=[wbr], sembuf=wbr)
            rh = Ring("s3h", 2, [128, KC, 512], BF16, es)
            ry = Ring("s3y", 2, [128, KC, 512], BF16, es)
            rmix = Ring("s3mix", 2, [128, KC, 512], BF16, es)
            sg = Ring("s3sg", 3, [128, 512], F32, es)
            tmp = Ring("s3tmp", 3, [128, 512], F32, es)
            mixf = Ring("s3mixf", 2, [128, 512], F32, es)
            brk = ((0, 2), (2, 4), (4, 8))
            for (t0, ts, r) in tiles512():
                if last and r == R - 1:
                    continue
                h = rh.next()
                m.dma("sp", h[:, :, 0:ts], fm(H1, t0, ts), reads=[H1], writes=[h], sembuf=h)
                y = ry.next()
                m.dma("sp", y[:, :, 0:ts], fm(Yd, t0, ts), reads=[Yd], writes=[y], sembuf=y)
                mix = rmix.next()
                for j in range(KC):
                    mf = mixf.next()
                    for bi, (ka, kb) in enumerate(brk):
                        pg = nps()
                        for k in range(KC):
                            m.op("pe", lambda e, k=k, pg=pg, bi=bi, j=j: e.matmul(
                                pg[:, 0:ts], lhsT=wg[:, k, bi * 1024 + j * 128:bi * 1024 + (j + 1) * 128],
                                rhs=h[:, k, 0:ts], start=(k == 0), stop=(k == KC - 1)), reads=[wg, h], writes=[pg])
                        pp = nps()
                        for k in range(ka, kb):
                            m.op("pe", lambda e, k=k, pp=pp, j=j, ka=ka, kb=kb: e.matmul(
                                pp[:, 0:ts], lhsT=wbr[:, k, j * 128:(j + 1) * 128], rhs=y[:, k, 0:ts],
                                start=(k == ka), stop=(k == kb - 1)), reads=[wbr, y], writes=[pp])
                        s_ = sg.next()
                        m.op("act", lambda e, s_=s_, pg=pg: e.activation(out=s_[:, 0:ts], in_=pg[:, 0:ts],
                                                                         func=AF.Sigmoid), reads=[pg], writes=[s_])
                        if bi == 0:
                            m.op("dve", lambda e, mf=mf, pp=pp, s_=s_: e.tensor_tensor(
                                out=mf[:, 0:ts], in0=pp[:, 0:ts], in1=s_[:, 0:ts], op=ALU.mult),
                                reads=[pp, s_], writes=[mf])
                        else:
                            t_ = tmp.next()
                            m.op("dve", lambda e, t_=t_, pp=pp, s_=s_: e.tensor_tensor(
                                out=t_[:, 0:ts], in0=pp[:, 0:ts], in1=s_[:, 0:ts], op=ALU.mult),
                                reads=[pp, s_], writes=[t_])
                            if bi == 1:
                                m.op("pool", lambda e, mf=mf, t_=t_: e.tensor_tensor(
                                    out=mf[:, 0:ts], in0=mf[:, 0:ts], in1=t_[:, 0:ts], op=ALU.add),
                                    reads=[mf, t_], writes=[mf])
                            else:
                                m.op("pool", lambda e, mf=mf, t_=t_, j=j: e.tensor_tensor(
                                    out=mix[:, j, 0:ts], in0=mf[:, 0:ts], in1=t_[:, 0:ts], op=ALU.add),
                                    reads=[mf, t_], writes=[mix])
                m.dma("act", fm(MIXd, t0, ts), mix[:, :, 0:ts], reads=[mix], writes=[MIXd], sembuf=mix)
        barrier()

    def stage3b(l, last, xr, xw):
        moe = (l % 2 == 1)
        with ExitStack() as es:
            wo = stage_sb(es, "s3wo", [128, KC, D], BF16)
            m.dma("sp", wo[:], Wb_out[l].t.rearrange("(k p) n -> p k n", p=128), reads=[Wb_out[l]], writes=[wo], sembuf=wo)
            rtw = stage_sb(es, "s3rt", [128, KC, E], F32)
            if moe:
                m.dma("sp", rtw[:], moe_rt.t[l // 2].rearrange("(k p) e -> p k e", p=128),
                      reads=[moe_rt], writes=[rtw], sembuf=rtw)
            rmix = Ring("s3bmix", 2, [128, KC, 512], BF16, es)
            rx = Ring("s3bx", 2, [128, KC, 512], F32, es)
            sqb = stage_sb(es, "s3bsq", [128, KC, 512], BF16)
            rt = stage_sb(es, "s3brt", [128, 512], F32)
            rstd = stage_sb(es, "s3brstd", [128, 512], F32)
            tmp = stage_sb(es, "s3btmp", [128, KC, 512], F32)
            hf = stage_sb(es, "s3bhf", [128, KC, 512], F32)
            rh2 = Ring("s3bh2", 2, [128, KC, 512], BF16, es)
            rgt = Ring("s3bgt", 2, [E, 512], BF16, es)
            sm8 = [Ring("s3bs%d" % i, 2, [128, E], F32, es) for i in range(5)]
            sc1 = [Ring("s3bc%d" % i, 2, [128, 1], F32, es) for i in range(7)]
            for (t0, ts, r) in tiles512():
                if last and r == R - 1:
                    continue
                nb = ts // 128
                mix = rmix.next()
                m.dma("sp", mix[:, :, 0:ts], fm(MIXd, t0, ts), reads=[MIXd], writes=[mix], sembuf=mix)
                xt = rx.next()
                m.dma("sp", xt[:, :, 0:ts], fm(xr, t0, ts), reads=[xr], writes=[xt], sembuf=xt)
                for j in range(KC):
                    p = nps()
                    for k in range(KC):
                        m.op("pe", lambda e, k=k, p=p, j=j: e.matmul(
                            p[:, 0:ts], lhsT=wo[:, k, j * 128:(j + 1) * 128], rhs=mix[:, k, 0:ts],
                            start=(k == 0), stop=(k == KC - 1)), reads=[wo, mix], writes=[p])
                    m.op("dve", lambda e, p=p, j=j: e.scalar_tensor_tensor(
                        out=xt[:, j, 0:ts], in0=p[:, 0:ts], scalar=modT[:, r, 16 + j:17 + j], in1=xt[:, j, 0:ts],
                        op0=ALU.mult, op1=ALU.add), reads=[p, modT, xt], writes=[xt])
                m.dma("act", fm(xw, t0, ts), xt[:, :, 0:ts], reads=[xt], writes=[xw], sembuf=xt)
                h2 = rh2.next()
                norm_mod(xt, ts, lambda k: A2[:, r, k:k + 1], lambda k: modT[:, r, 24 + k:25 + k],
                         sqb, rt, rstd, tmp, h2, hf=hf if moe else None)
                m.dma("act", fm(H2d, t0, ts), h2[:, :, 0:ts], reads=[h2], writes=[H2d], sembuf=h2)
                if moe:
                    gtt = rgt.next()
                    for blk in range(nb):
                        p = nps()
                        for k in range(KC):
                            m.op("pe", lambda e, k=k, p=p, blk=blk: e.matmul(
                                p[:, 0:E], lhsT=hf[:, k, blk * 128:(blk + 1) * 128], rhs=rtw[:, k, :],
                                start=(k == 0), stop=(k == KC - 1)), reads=[hf, rtw], writes=[p])
                        lg = sm8[0].next()
                        m.op("act", lambda e, lg=lg, p=p: e.activation(out=lg[:], in_=p[:, 0:E], func=AF.Copy),
                             reads=[p], writes=[lg])
                        m1 = sc1[0].next()
                        m.op("dve", lambda e, m1=m1, lg=lg: e.tensor_reduce(out=m1[:], in_=lg[:], axis=AX.X, op=ALU.max),
                             reads=[lg], writes=[m1])
                        eq1 = sm8[1].next()
                        m.op("dve", lambda e, eq1=eq1, lg=lg, m1=m1: e.tensor_scalar(
                            out=eq1[:], in0=lg[:], scalar1=m1[:, 0:1], scalar2=None, op0=ALU.is_equal),
                            reads=[lg, m1], writes=[eq1])
                        msk2 = sm8[2].next()
                        m.op("dve", lambda e, msk2=msk2, eq1=eq1, lg=lg: e.scalar_tensor_tensor(
                            out=msk2[:], in0=eq1[:], scalar=NEG, in1=lg[:], op0=ALU.mult, op1=ALU.add),
                            reads=[eq1, lg], writes=[msk2])
                        m2 = sc1[1].next()
                        m.op("dve", lambda e, m2=m2, msk2=msk2: e.tensor_reduce(out=m2[:], in_=msk2[:], axis=AX.X, op=ALU.max),
                             reads=[msk2], writes=[m2])
                        eq2 = sm8[3].next()
                        m.op("dve", lambda e, eq2=eq2, msk2=msk2, m2=m2: e.tensor_scalar(
                            out=eq2[:], in0=msk2[:], scalar1=m2[:, 0:1], scalar2=None, op0=ALU.is_equal),
                            reads=[msk2, m2], writes=[eq2])
                        dd = sc1[2].next()
                        m.op("dve", lambda e, dd=dd, m2=m2, m1=m1: e.tensor_tensor(out=dd[:], in0=m2[:], in1=m1[:], op=ALU.subtract),
                             reads=[m1, m2], writes=[dd])
                        ee = sc1[3].next()
                        m.op("act", lambda e, ee=ee, dd=dd: e.activation(out=ee[:], in_=dd[:], func=AF.Exp),
                             reads=[dd], writes=[ee])
                        dn = sc1[4].next()
                        m.op("dve", lambda e, dn=dn, ee=ee: e.tensor_scalar(out=dn[:], in0=ee[:], scalar1=1.0, scalar2=None, op0=ALU.add),
                             reads=[ee], writes=[dn])
                        w1_ = sc1[5].next()
                        m.op("dve", lambda e, w1_=w1_, dn=dn: e.reciprocal(out=w1_[:], in_=dn[:]), reads=[dn], writes=[w1_])
                        w2_ = sc1[6].next()
                        m.op("dve", lambda e, w2_=w2_, w1_=w1_, ee=ee: e.tensor_tensor(out=w2_[:], in0=w1_[:], in1=ee[:], op=ALU.mult),
                             reads=[w1_, ee], writes=[w2_])
                        ga = sm8[4].next()
                        m.op("dve", lambda e, ga=ga, eq1=eq1, w1_=w1_: e.tensor_scalar(
                            out=ga[:], in0=eq1[:], scalar1=w1_[:, 0:1], scalar2=None, op0=ALU.mult),
                            reads=[eq1, w1_], writes=[ga])
                        m.op("dve", lambda e, ga=ga, eq2=eq2, w2_=w2_: e.scalar_tensor_tensor(
                            out=ga[:], in0=eq2[:], scalar=w2_[:, 0:1], in1=ga[:], op0=ALU.mult, op1=ALU.add),
                            reads=[eq2, w2_, ga], writes=[ga])
                        p2 = nps()
                        m.op("pe", lambda e, p2=p2, ga=ga: e.transpose(p2[0:E, 0:128], ga[:], ident[:]),
                             reads=[ga, ident], writes=[p2])
                        m.op("act", lambda e, p2=p2, gtt=gtt, blk=blk: e.activation(
                            out=gtt[:, blk * 128:(blk + 1) * 128], in_=p2[0:E, 0:128], func=AF.Copy),
                            reads=[p2], writes=[gtt])
                    m.dma("act", GTd.t[:, t0:t0 + ts], gtt[:, 0:ts], reads=[gtt], writes=[GTd], sembuf=gtt)
        barrier()

    def stage4(l, last, xr, xw):
        moe = (l % 2 == 1)
        NE = E if moe else 1
        F = FE if moe else FD
        GF = 4 if moe else 2
        GW = GF * 128
        NG = F // GW
        TT = 1024
        tl = []
        if not last:
            for i in range((CT + TT - 1) // TT):
                tl.append((i * TT, min(TT, CT - i * TT), R - 1))
        for b in range(NB):
            for i in range(S // TT):
                tl.append((CT + b * S + i * TT, TT, b))
        with ExitStack() as es:
            rh2 = Ring("s4h", 2, [128, KC, TT], BF16, es)
            yacc = stage_sb(es, "s4yacc", [128, KC, TT], F32)
            gbc = stage_sb(es, "s4gbc", [128, E, TT], BF16) if moe else None
            ract = Ring("s4act", 3, [128, GF, 512], BF16, es)
            rs = Ring("s4s", 3, [128, 512], BF16, es)
            rt_ = Ring("s4t", 3, [128, 512], BF16, es)
            rxc = Ring("s4xc", 2, [128, TT], F32, es)
            rwgu = Ring("s4wgu", 3, [128, KC, 2 * GW], BF16, es)
            rwd = Ring("s4wd", 3, [128, GF, D], BF16, es)
            guv = [Wb_gu[l].t[e].rearrange("(k p) n -> p k n", p=128) for e in range(NE)]
            dv = [Wb_d[l].t[e] for e in range(NE)]
            gring = [0]
            per_tile = (len(conv_q) + len(tl) - 1) // max(len(tl), 1)
            for (t0, ts, r) in tl:
                conv_pop(per_tile)
                h2 = rh2.next()
                m.dma("act", h2[:, :, 0:ts], fm(H2d, t0, ts), reads=[H2d], writes=[h2], sembuf=h2)
                if moe:
                    for e_ in range(E):
                        m.dma("act", gbc[:, e_, 0:ts], GTd.t[e_:e_ + 1, t0:t0 + ts].partition_broadcast(128),
                              reads=[GTd], writes=[gbc], sembuf=gbc)
                halves = [(o, min(512, ts - o)) for o in range(0, ts, 512)]
                pend = None
                first_unit = [True]

                def down(act_hs, wd, fu):
                    for (ho, hs), act in act_hs:
                        for j in range(KC):
                            py = PS[4 + (gring[0] % 4)]
                            gring[0] += 1
                            for f_ in range(GF):
                                m.op("pe", lambda e, py=py, f_=f_, j=j, act=act, hs=hs: e.matmul(
                                    py[:, 0:hs], lhsT=wd[:, f_, j * 128:(j + 1) * 128], rhs=act[:, f_, 0:hs],
                                    start=(f_ == 0), stop=(f_ == GF - 1)), reads=[wd, act], writes=[py])
                            if fu:
                                m.op("act", lambda e, py=py, j=j, ho=ho, hs=hs: e.activation(
                                    out=yacc[:, j, ho:ho + hs], in_=py[:, 0:hs], func=AF.Copy),
                                    reads=[py], writes=[yacc])
                            else:
                                m.op("dve", lambda e, py=py, j=j, ho=ho, hs=hs: e.tensor_tensor(
                                    out=yacc[:, j, ho:ho + hs], in0=py[:, 0:hs], in1=yacc[:, j, ho:ho + hs], op=ALU.add),
                                    reads=[py, yacc], writes=[yacc])

                ui = 0
                for e_ in range(NE):
                    for gi in range(NG):
                        wgu = rwgu.next()
                        m.dma("sp", wgu[:, :, 0:GW], guv[e_][:, :, gi * GW:(gi + 1) * GW],
                              reads=[Wb_gu[l]], writes=[wgu], sembuf=wgu)
                        m.dma("sp", wgu[:, :, GW:2 * GW], guv[e_][:, :, F + gi * GW:F + (gi + 1) * GW],
                              reads=[Wb_gu[l]], writes=[wgu], sembuf=wgu)
                        wd = rwd.next()
                        m.dma("sp", wd[:], dv[e_][gi * GW:(gi + 1) * GW, :].rearrange("(f p) d -> p f d", p=128),
                              reads=[Wb_d[l]], writes=[wd], sembuf=wd)
                        act_hs = []
                        for (ho, hs) in halves:
                            act = ract.next()
                            for f_ in range(GF):
                                pg = PS[(ui % 2)]
                                pu = PS[2 + (ui % 2)]
                                ui += 1
                                for k in range(KC):
                                    m.op("pe", lambda e, k=k, pg=pg, f_=f_, ho=ho, hs=hs: e.matmul(
                                        pg[:, 0:hs], lhsT=wgu[:, k, f_ * 128:(f_ + 1) * 128], rhs=h2[:, k, ho:ho + hs],
                                        start=(k == 0), stop=(k == KC - 1)), reads=[wgu, h2], writes=[pg])
                                for k in range(KC):
                                    m.op("pe", lambda e, k=k, pu=pu, f_=f_, ho=ho, hs=hs: e.matmul(
                                        pu[:, 0:hs], lhsT=wgu[:, k, GW + f_ * 128:GW + (f_ + 1) * 128],
                                        rhs=h2[:, k, ho:ho + hs], start=(k == 0), stop=(k == KC - 1)),
                                        reads=[wgu, h2], writes=[pu])
                                s_ = rs.next()
                                m.op("act", lambda e, s_=s_, pg=pg, hs=hs: e.activation(
                                    out=s_[:, 0:hs], in_=pg[:, 0:hs], func=AF.Silu), reads=[pg], writes=[s_])
                                if moe:
                                    t_ = rt_.next()
                                    m.op("dve", lambda e, t_=t_, pu=pu, s_=s_, hs=hs: e.tensor_tensor(
                                        out=t_[:, 0:hs], in0=pu[:, 0:hs], in1=s_[:, 0:hs], op=ALU.mult),
                                        reads=[pu, s_], writes=[t_])
                                    m.op("pool", lambda e, t_=t_, act=act, f_=f_, e_=e_, ho=ho, hs=hs: e.tensor_tensor(
                                        out=act[:, f_, 0:hs], in0=t_[:, 0:hs], in1=gbc[:, e_, ho:ho + hs], op=ALU.mult),
                                        reads=[t_, gbc], writes=[act])
                                else:
                                    m.op("dve", lambda e, act=act, pu=pu, s_=s_, f_=f_, hs=hs: e.tensor_tensor(
                                        out=act[:, f_, 0:hs], in0=pu[:, 0:hs], in1=s_[:, 0:hs], op=ALU.mult),
                                        reads=[pu, s_], writes=[act])
                            act_hs.append(((ho, hs), act))
                            if pend is not None and (ho, hs) == halves[0]:
                                down(*pend)
                                pend = None
                        pend = (act_hs, wd, first_unit[0])
                        first_unit[0] = False
                down(*pend)
                for j in range(KC):
                    xc = rxc.next()
                    m.dma("act", xc[:, 0:ts], xr.t[j, :, t0:t0 + ts], reads=[xr], writes=[xc], sembuf=xc)
                    m.op("dve", lambda e, xc=xc, j=j: e.scalar_tensor_tensor(
                        out=xc[:, 0:ts], in0=yacc[:, j, 0:ts], scalar=modT[:, r, 40 + j:41 + j], in1=xc[:, 0:ts],
                        op0=ALU.mult, op1=ALU.add), reads=[yacc, modT, xc], writes=[xc])
                    m.dma("act", xw.t[j, :, t0:t0 + ts], xc[:, 0:ts], reads=[xc], writes=[xw], sembuf=xc)
        barrier()

    def stage_final(xr):
        with ExitStack() as es:
            fn = stage_sb(es, "fn", [128, KC], F32)
            m.dma("sp", fn[:], fnT[:], reads=[fnT], writes=[fn], sembuf=fn)
            rx = Ring("sfx", 2, [128, KC, 512], F32, es)
            sqb = stage_sb(es, "sfsq", [128, KC, 512], BF16)
            rt = stage_sb(es, "sfrt", [128, 512], F32)
            rstd = stage_sb(es, "sfrstd", [128, 512], F32)
            yt = stage_sb(es, "sfy", [128, KC, 512], F32)
            ro = Ring("sfo", 2, [128, 4, D], F32, es)
            for i in range(TX // 512):
                t0 = CT + i * 512
                ts = 512
                xt = rx.next()
                m.dma("sp", xt[:], fm(xr, t0, ts), reads=[xr], writes=[xt], sembuf=xt)
                m.op("act", lambda e: e.activation(out=sqb[:], in_=xt[:], func=AF.Square), reads=[xt], writes=[sqb])
                p = nps()
                for k in range(KC):
                    m.op("pe", lambda e, k=k: e.matmul(p[:], lhsT=ones_bf[:], rhs=sqb[:, k, :], start=(k == 0),
                                                       stop=(k == KC - 1)), reads=[ones_bf, sqb], writes=[p])
                m.op("act", lambda e: e.activation(out=rt[:], in_=p[:], func=AF.Sqrt, bias=epsc[:], scale=1.0 / D),
                     reads=[p, epsc], writes=[rt])
                m.op("dve", lambda e: e.reciprocal(out=rstd[:], in_=rt[:]), reads=[rt], writes=[rstd])
                for k in range(KC):
                    m.op("dve", lambda e, k=k: e.scalar_tensor_tensor(
                        out=yt[:, k, :], in0=xt[:, k, :], scalar=fn[:, k:k + 1], in1=rstd[:],
                        op0=ALU.mult, op1=ALU.mult), reads=[xt, fn, rstd], writes=[yt])
                ot = ro.next()
                for blk in range(4):
                    for kh in range(2):
                        pp = nps()
                        for kk in range(4):
                            k = kh * 4 + kk
                            m.op("pe", lambda e, pp=pp, kk=kk, k=k, blk=blk: e.transpose(
                                pp[:, kk * 128:(kk + 1) * 128], yt[:, k, blk * 128:(blk + 1) * 128], ident[:]),
                                reads=[yt, ident], writes=[pp])
                        if (blk + kh) % 2 == 0:
                            m.op("act", lambda e, pp=pp, blk=blk, kh=kh: e.activation(
                                out=ot[:, blk, kh * 512:(kh + 1) * 512], in_=pp[:], func=AF.Copy),
                                reads=[pp], writes=[ot])
                        else:
                            m.op("dve", lambda e, pp=pp, blk=blk, kh=kh: e.tensor_copy(
                                out=ot[:, blk, kh * 512:(kh + 1) * 512], in_=pp[:]), reads=[pp], writes=[ot])
                m.dma("act", out.t[i * 512:(i + 1) * 512, :].rearrange("(b p) d -> p b d", p=128), ot[:],
                      reads=[ot], writes=[out], sembuf=ot)
        barrier()

    convert_layer(0)
    stage0()
    cur = 0
    stop = cfg.stop
    done = False
    for l in range(L):
        last = (l == L - 1)
        ada(l, l == 0)
        stage1(l, XR[cur])
        if stop == (l, 1): done = True; break
        stage2(l, last)
        if stop == (l, 2): done = True; break
        if l + 1 < L:
            convert_layer(l + 1, defer=True)
            conv_pop(4)
        stage3a(l, last)
        stage3b(l, last, XR[cur], XR[1 - cur])
        cur = 1 - cur
        if stop == (l, 3): done = True; break
        stage4(l, last, XR[cur], XR[1 - cur])
        conv_pop(len(conv_q))
        cur = 1 - cur
        if stop == (l, 4): done = True; break
    if not done:
        stage_final(XR[cur])
    nobar.clear()
    barrier()
    ges.close()
    return nc, m


def host_prep(cfg, core, inp):
    NB, S, L = cfg.NB, cfg.S, cfg.L
    f = lambda a: np.ascontiguousarray(a, dtype=np.float32)
    b0 = core * NB
    d = {}
    d["x_in"] = f(inp["x"][b0:b0 + NB].reshape(NB * S, D))
    d["c_in"] = f(inp["ctx"][b0:b0 + NB].reshape(NB * LC, D))
    cv = np.concatenate([inp["c"][b0:b0 + NB], inp["c_ctx"][None]], 0)
    d["cT"] = f(cv.reshape(cfg.R, KC, 128).transpose(2, 1, 0))
    return d


def host_shared(cfg, inp):
    L = cfg.L
    f = lambda a: np.ascontiguousarray(a, dtype=np.float32)
    d = {}
    d["w_mod"] = f(inp["w_mod"])
    d["bmodT"] = f(inp["b_mod"].reshape(L, 48, 128).transpose(0, 2, 1))
    d["n1T"] = f(inp["norm1_g"].reshape(L, KC, 128).transpose(0, 2, 1))
    d["n2T"] = f(inp["norm2_g"].reshape(L, KC, 128).transpose(0, 2, 1))
    d["fnT"] = f(inp["final_norm_g"].reshape(KC, 128).T)
    w_in = inp["w_in"]
    d["w_in"] = f(w_in)
    rs = rot_src()
    qcols = np.concatenate([1280 + hh * 64 + rs for hh in range(8)])
    kk = [np.concatenate([1792 + kv * 64 + np.arange(64)] * 2) for kv in range(2)]
    kkp = [np.concatenate([1792 + kv * 64 + rs] * 2) for kv in range(2)]
    cols = np.concatenate([qcols] + kk + kkp)
    d["w_ex"] = f(w_in[:, :, cols])
    d["convT"] = f(inp["conv_w"].transpose(0, 2, 1).reshape(L, 2, 128, 3).transpose(0, 2, 1, 3))
    d["wsT"] = f(inp["gmlp_ws"].transpose(0, 3, 1, 2))
    gbv = inp["gmlp_b"]
    gb = np.repeat(gbv[:, :, None, :], 64, axis=2)
    d["gb"] = f(gb.reshape(L, 2, 128, 128).transpose(0, 2, 1, 3))
    d["sinkbc"] = f(np.broadcast_to(inp["attn_sink"][:, None, :], (L, 128, NH)))
    d["w_br"] = f(np.concatenate([inp["w_br_conv"], inp["w_br_gmlp"], inp["w_br_attn"]], axis=1))
    d["w_out"] = f(inp["w_out"])
    d["ffn_gu"] = f(inp["ffn_w_gu"])
    d["ffn_d"] = f(inp["ffn_w_d"])
    d["moe_rt"] = f(inp["moe_router"])
    d["moe_gu"] = f(inp["moe_w_gu"])
    d["moe_d"] = f(inp["moe_w_d"])
    tc, ts_ = rope_tabs(cfg)
    d["tabC"], d["tabS"] = tc, ts_
    qi = np.arange(128)[:, None]
    jj = np.arange(384)[None, :]
    d["mask"] = np.where((jj >= qi) & (jj <= qi + 256), 0.0, NEG).astype(np.float32)
    return d


_CACHE = {}


def run(cfg, inp, ncores):
    key = (cfg.NB, cfg.S, cfg.L, cfg.dbg, cfg.stop)
    if key not in _CACHE:
        _CACHE[key] = build(cfg)
    nc, m = _CACHE[key]
    sh = host_shared(cfg, inp)
    in_maps = []
    for c in range(ncores):
        dd = dict(sh)
        dd.update(host_prep(cfg, c, inp))
        in_maps.append(dd)
    res = run_bass_kernel_spmd(nc, in_maps, core_ids=list(range(ncores)))
    return res


def kernel(**inputs):
    cfg = Cfg(NB=2, S=4096, L=4)
    inp = {k: np.asarray(v) for k, v in inputs.items()}
    res = run(cfg, inp, 8)
    outs = [r["out"].reshape(cfg.NB, cfg.S, D) for r in res.results]
    return np.ascontiguousarray(np.concatenate(outs, axis=0), dtype=np.float32)
```

```python
import numpy as np
import concourse.bass as bass
import concourse.mybir as mybir
from contextlib import ExitStack

F32 = mybir.dt.float32
BF16 = mybir.dt.bfloat16
AF = mybir.ActivationFunctionType
ALU = mybir.AluOpType
AX = mybir.AxisListType


class Buf:
    __slots__ = ("name", "t", "writers", "readers", "dsem")

    def __init__(self, name, t=None):
        self.name = name
        self.t = t
        self.writers = {}
        self.readers = {}
        self.dsem = None

    def __getitem__(self, k):
        return self.t[k]


class MK:
    ENG = ("pe", "act", "dve", "pool", "sp")

    def __init__(self, nc, es):
        self.nc = nc
        self.es = es
        self.h = {"pe": nc.tensor, "act": nc.scalar, "dve": nc.vector,
                  "pool": nc.gpsimd, "sp": nc.sync}
        self.sems = {}
        self.issued = {}
        self.seen = {e: {} for e in self.ENG}
        for e in self.ENG:
            self.sems[e] = nc.alloc_semaphore(name="s_" + e)
            self.issued[e] = 0
        self.ndsem = 0
        self.ninstr = 0
        self.stage_bufs = []
        self.free_dsems = []
        self.dkeys = set()

    def sb(self, name, shape, dt):
        t = self.es.enter_context(self.nc.sbuf_tensor(name, list(shape), dt))
        return Buf(name, t)

    def uname(self, name):
        self.uid = getattr(self, "uid", 0) + 1
        return "%s_u%d" % (name, self.uid)

    def track(self, b):
        self.stage_bufs.append(b)
        return b

    def end_stage(self):
        for b in self.stage_bufs:
            if b.dsem is not None:
                self.free_dsems.append(b.dsem)
                b.dsem = None
        self.stage_bufs = []

    def ps(self, name, shape, dt):
        t = self.es.enter_context(self.nc.psum_tensor(name, list(shape), dt))
        return Buf(name, t)

    def dram(self, name, shape, dt, kind="Internal"):
        t = self.nc.dram_tensor(name, list(shape), dt, kind=kind)
        return Buf(name, t.ap())

    def _dsem(self, b):
        if b.dsem is None:
            if self.free_dsems:
                b.dsem = self.free_dsems.pop()
                return b.dsem
            k = "q%d" % self.ndsem
            self.ndsem += 1
            self.sems[k] = self.nc.alloc_semaphore(name="s_" + k)
            self.issued[k] = 0
            self.dkeys.add(k)
            b.dsem = k
        return b.dsem

    def _need(self, eng, reads, writes):
        need = {}

        def add(k, c, kind):
            if k == eng:
                if eng == "pe":
                    return
                if kind == "war":
                    return
            if c > need.get(k, 0):
                need[k] = c

        for b in reads:
            for k, c in b.writers.items():
                add(k, c, "raw")
        for b in writes:
            for k, c in b.writers.items():
                add(k, c, "waw")
            for k, c in b.readers.items():
                add(k, c, "war")
        seen = self.seen[eng]
        hnd = self.h[eng]
        for k, c in need.items():
            if seen.get(k, 0) >= c:
                continue
            if k in self.dkeys:
                c = max(c, self.issued[k])
            hnd.wait_ge(self.sems[k], c)
            seen[k] = c

    def _mark(self, key, cnt, reads, writes):
        for b in writes:
            b.writers = {key: cnt}
            b.readers = {}
        for b in reads:
            if b not in writes:
                b.readers[key] = cnt

    def op(self, eng, fn, reads=(), writes=()):
        self._need(eng, reads, writes)
        ins = fn(self.h[eng])
        self.issued[eng] += 1
        ins.then_inc(self.sems[eng], 1)
        self._mark(eng, self.issued[eng], reads, writes)
        self.ninstr += 1
        return ins

    def dma(self, q, out, in_, reads=(), writes=(), sembuf=None, **kw):
        self._need(q, reads, writes)
        k = self._dsem(sembuf)
        ins = self.h[q].dma_start(out=out, in_=in_, **kw)
        self.issued[k] += 16
        ins.then_inc(self.sems[k], 16)
        self._mark(k, self.issued[k], reads, writes)
        self.ninstr += 1
        return ins

    def wait_all(self, eng, bufs):
        self._need(eng, bufs, ())

from concourse.bass_utils import run_bass_kernel_spmd

D = 1024
KC = 8
LC = 256
NH = 8
E = 8
FD = 2816
FE = 3584
EPS = 1e-6
SCALE = 0.125
NEG = -1e30


class Cfg:
    def __init__(self, NB=2, S=4096, L=4, dbg=False, stop=None):
        self.NB, self.S, self.L, self.dbg, self.stop = NB, S, L, dbg, stop
        self.R = NB + 1
        self.CT = NB * LC
        self.TX = NB * S
        self.TA = self.CT + self.TX


def rope_tabs(cfg):
    S = cfg.S
    rows = S // 64
    row = np.repeat(np.arange(rows), 64).astype(np.float32)
    col = np.tile(np.arange(64), rows).astype(np.float32)
    half = 32
    inv = (1.0 / (10000.0 ** (np.arange(0, half, 2, dtype=np.float32) / half))).astype(np.float32)
    ang_r = row[:, None] * inv[None, :]
    ang_c = col[:, None] * inv[None, :]
    ang = np.concatenate([ang_r, ang_r, ang_c, ang_c], axis=-1)
    cos = np.cos(ang).astype(np.float32).T
    sin = np.sin(ang).astype(np.float32).T
    sgn = np.where((np.arange(64) % 32) < 16, -1.0, 1.0).astype(np.float32)[:, None]
    sins = sin * sgn
    tc = np.ones((128, cfg.TA), np.float32)
    ts_ = np.zeros((128, cfg.TA), np.float32)
    for b in range(cfg.NB):
        o = cfg.CT + b * S
        tc[:, o:o + S] = np.concatenate([cos, cos], 0)
        ts_[:, o:o + S] = np.concatenate([sins, sins], 0)
    return tc, ts_


def rot_src():
    j = np.arange(64)
    return (j // 32) * 32 + ((j % 32) + 16) % 32


def build(cfg):
    NB, S, L, R, CT, TX, TA = cfg.NB, cfg.S, cfg.L, cfg.R, cfg.CT, cfg.TX, cfg.TA
    nc = bass.Bass("TRN2", target_bir_lowering=False)
    ges = ExitStack()
    m = MK(nc, ges)
    EI = "ExternalInput"

    x_in = m.dram("x_in", [TX, D], F32, EI)
    c_in = m.dram("c_in", [CT, D], F32, EI)
    cT_in = m.dram("cT", [128, KC, R], F32, EI)
    w_mod = m.dram("w_mod", [L, D, 6 * D], F32, EI)
    bmodT = m.dram("bmodT", [L, 128, 48], F32, EI)
    n1T = m.dram("n1T", [L, 128, KC], F32, EI)
    n2T = m.dram("n2T", [L, 128, KC], F32, EI)
    fnT = m.dram("fnT", [128, KC], F32, EI)
    w_in = m.dram("w_in", [L, D, 5120], F32, EI)
    w_ex = m.dram("w_ex", [L, D, 1024], F32, EI)
    convT = m.dram("convT", [L, 128, 2, 3], F32, EI)
    wsT_in = m.dram("wsT", [L, 128, 4, 128], F32, EI)
    gb_in = m.dram("gb", [L, 128, 2, 128], F32, EI)
    sink_in = m.dram("sinkbc", [L, 128, NH], F32, EI)
    w_br = m.dram("w_br", [L, D, D], F32, EI)
    w_out = m.dram("w_out", [L, D, D], F32, EI)
    ND = (L + 1) // 2
    NM = max(L // 2, 1)
    ffn_gu = m.dram("ffn_gu", [ND, D, 2 * FD], F32, EI)
    ffn_d = m.dram("ffn_d", [ND, FD, D], F32, EI)
    moe_rt = m.dram("moe_rt", [NM, D, E], F32, EI)
    moe_gu = m.dram("moe_gu", [NM, E, D, 2 * FE], F32, EI)
    moe_d = m.dram("moe_d", [NM, E, FE, D], F32, EI)
    tabC = m.dram("tabC", [128, TA], F32, EI)
    tabS = m.dram("tabS", [128, TA], F32, EI)
    mask_in = m.dram("mask", [128, 384], F32, EI)
    out = m.dram("out", [TX, D], F32, "ExternalOutput")

    OK = "ExternalOutput" if cfg.dbg else "Internal"
    XR = [m.dram("XR%d" % i, [KC, 128, TA], F32, OK) for i in range(2)]
    H1 = m.dram("H1", [KC, 128, TA], BF16, OK)
    BGd = m.dram("BGd", [2, 128, TA], BF16, OK)
    CHd = m.dram("CHd", [2, 128, TA], BF16, OK)
    UGd = m.dram("UGd", [2, 128, TA], BF16, OK)
    Qd = m.dram("Qd", [4, 128, TA], BF16, OK)
    KKd = m.dram("KKd", [2, 128, TA], BF16, OK)
    VNXd = m.dram("VNXd", [TA, 512], BF16, OK)
    VAXd = m.dram("VAXd", [TA, 512], BF16, OK)
    Yd = m.dram("Yd", [KC, 128, TA], BF16, OK)
    MIXd = m.dram("MIXd", [KC, 128, TA], BF16, OK)
    H2d = m.dram("H2d", [KC, 128, TA], BF16, OK)
    GTd = m.dram("GTd", [E, TA], BF16, OK)
    Wb_in = [m.dram("Wb_in%d" % l, [D, 6144], BF16) for l in range(L)]
    Wb_br = [m.dram("Wb_br%d" % l, [D, D], BF16) for l in range(L)]
    Wb_out = [m.dram("Wb_out%d" % l, [D, D], BF16) for l in range(L)]
    Wb_gu, Wb_d = [], []
    for l in range(L):
        if l % 2 == 0:
            Wb_gu.append(m.dram("Wb_gu%d" % l, [1, D, 2 * FD], BF16))
            Wb_d.append(m.dram("Wb_d%d" % l, [1, FD, D], BF16))
        else:
            Wb_gu.append(m.dram("Wb_gu%d" % l, [E, D, 2 * FE], BF16))
            Wb_d.append(m.dram("Wb_d%d" % l, [E, FE, D], BF16))

    ident = m.sb("ident", [128, 128], F32)
    ones_bf = m.sb("ones_bf", [128, 128], BF16)
    epsc = m.sb("epsc", [128, 1], F32)
    m.op("pool", lambda e: e.memset(ident[:], 0.0), writes=[ident])
    m.op("pool", lambda e: e.affine_select(out=ident[:], in_=ident[:], pattern=[[-1, 128]],
                                            compare_op=ALU.not_equal, fill=1.0, base=0,
                                            channel_multiplier=1), reads=[ident], writes=[ident])
    m.op("pool", lambda e: e.memset(ones_bf[:], 1.0), writes=[ones_bf])
    m.op("pool", lambda e: e.memset(epsc[:], EPS), writes=[epsc])
    psall_t = ges.enter_context(nc.psum_tensor("psall", [128, 8, 512], F32))
    psall = psall_t[:]
    PS = [Buf("ps%d" % i, psall[:, i, :]) for i in range(8)]
    modT = m.sb("modT", [128, R, 48], F32)
    A1 = m.sb("A1", [128, R, KC], F32)
    A2 = m.sb("A2", [128, R, KC], F32)
    csil = m.sb("csil", [128, KC, R], F32)
    cst = m.sb("cst", [128, KC, R], F32)
    bmod = m.sb("bmod", [128, 48], F32)
    n1 = m.sb("n1", [128, KC], F32)
    n2 = m.sb("n2", [128, KC], F32)

    state = {"psi": 0}

    def nps():
        p = PS[state["psi"] % 8]
        state["psi"] += 1
        return p

    class Ring:
        def __init__(self, name, n, shape, dt, es=None):
            self.slots = []
            for i in range(n):
                nm = m.uname("%s_%d" % (name, i))
                t = (es or ges).enter_context(nc.sbuf_tensor(nm, list(shape), dt))
                self.slots.append(m.track(Buf(nm, t)))
            self.i = 0

        def next(self):
            s = self.slots[self.i % len(self.slots)]
            self.i += 1
            return s

    def stage_sb(es, name, shape, dt):
        nm = m.uname(name)
        t = es.enter_context(nc.sbuf_tensor(nm, list(shape), dt))
        return m.track(Buf(nm, t))

    nobar = set()

    def barrier():
        for e in MK.ENG:
            for k in list(m.sems.keys()):
                if k == e or k in nobar:
                    continue
                c = m.issued[k]
                if c > m.seen[e].get(k, 0):
                    m.h[e].wait_ge(m.sems[k], c)
                    m.seen[e][k] = c
        m.end_stage()

    cvb = Buf("cvsem")

    def conv_w(dst, dst_ap, src, src_ap):
        m.dma("pool", dst_ap, src_ap, reads=[src], writes=[dst], sembuf=dst)
        nobar.add(dst.dsem)

    conv_w_impl = [conv_w]

    def conv_pop(n):
        for _ in range(n):
            if conv_q:
                conv_w(*conv_q.pop(0))

    def v2(ap, rows):
        return ap.rearrange("(p r) n -> p (r n)", p=128)

    conv_q = []

    def convert_layer(l, defer=False):
        if defer:
            jobs = []
            real = conv_w_impl[0]
            conv_w_impl[0] = lambda *a: jobs.append(a)
            convert_layer(l)
            conv_w_impl[0] = real
            conv_q.extend(jobs)
            return
        cw_ = lambda *a: conv_w_impl[0](*a)
        cw_(Wb_in[l], Wb_in[l][:, 0:5120], w_in, w_in[l])
        cw_(Wb_in[l], Wb_in[l][:, 5120:6144], w_ex, w_ex[l])
        cw_(Wb_br[l], v2(Wb_br[l][:], D), w_br, v2(w_br[l], D))
        cw_(Wb_out[l], v2(Wb_out[l][:], D), w_out, v2(w_out[l], D))
        if l % 2 == 0:
            cw_(Wb_gu[l], v2(Wb_gu[l][0], D), ffn_gu, v2(ffn_gu[l // 2], D))
            cw_(Wb_d[l], v2(Wb_d[l][0], FD), ffn_d, v2(ffn_d[l // 2], FD))
        else:
            for e in range(E):
                cw_(Wb_gu[l], v2(Wb_gu[l][e], D), moe_gu, v2(moe_gu[l // 2, e], D))
                cw_(Wb_d[l], v2(Wb_d[l][e], FE), moe_d, v2(moe_d[l // 2, e], FE))

    def tiles512():
        tl = [(0, CT, R - 1)] if CT <= 512 else [(i * 512, 512, R - 1) for i in range(CT // 512)]
        for b in range(NB):
            for i in range(S // 512):
                tl.append((CT + b * S + i * 512, 512, b))
        return tl

    def fm(dr, t0, ts, k0=0, k1=None):
        k1 = dr.t.shape[0] if k1 is None else k1
        return dr.t[k0:k1, :, t0:t0 + ts].rearrange("k p t -> p k t")

    def stage0():
        with ExitStack() as es:
            rin = Ring("s0in", 2, [128, 4, D], F32, es)
            rout = Ring("s0out", 2, [128, KC, 512], F32, es)
            srcs = [(c_in, i * 512, min(512, CT - i * 512), i * 512) for i in range((CT + 511) // 512)]
            srcs += [(x_in, i * 512, 512, CT + i * 512) for i in range(TX // 512)]
            for (src, r0, ts, t0) in srcs:
                nb = ts // 128
                it = rin.next()
                m.dma("sp", it[:, 0:nb, :], src.t[r0:r0 + ts, :].rearrange("(b p) d -> p b d", p=128),
                      reads=[src], writes=[it], sembuf=it)
                ot = rout.next()
                for blk in range(nb):
                    for kh in range(2):
                        p = nps()
                        for kk in range(4):
                            k = kh * 4 + kk
                            m.op("pe", lambda e, p=p, kk=kk, k=k, blk=blk: e.transpose(
                                p[:, kk * 128:(kk + 1) * 128], it[:, blk, k * 128:(k + 1) * 128], ident[:]),
                                reads=[it, ident], writes=[p])
                        eng = "act" if (blk + kh) % 2 == 0 else "dve"
                        src_v = p[:].rearrange("p (k t) -> p k t", k=4)
                        dst_v = ot[:, kh * 4:(kh + 1) * 4, blk * 128:(blk + 1) * 128]
                        if eng == "act":
                            m.op("act", lambda e, a=dst_v, b=src_v: e.activation(out=a, in_=b, func=AF.Copy),
                                 reads=[p], writes=[ot])
                        else:
                            m.op("dve", lambda e, a=dst_v, b=src_v: e.tensor_copy(out=a, in_=b),
                                 reads=[p], writes=[ot])
                m.dma("act", fm(XR[0], t0, ts), ot[:, :, 0:ts], reads=[ot], writes=[XR[0]], sembuf=ot)
        barrier()

    def ada(l, first):
        with ExitStack() as es:
            rw = Ring("adaw", 2, [128, KC, 512], F32, es)
            if first:
                m.dma("sp", cst[:], cT_in[:], reads=[cT_in], writes=[cst], sembuf=cst)
                m.op("act", lambda e: e.activation(out=csil[:], in_=cst[:], func=AF.Silu),
                     reads=[cst], writes=[csil])
            m.dma("sp", bmod[:], bmodT[l], reads=[bmodT], writes=[bmod], sembuf=bmod)
            m.dma("sp", n1[:], n1T[l], reads=[n1T], writes=[n1], sembuf=n1)
            m.dma("sp", n2[:], n2T[l], reads=[n2T], writes=[n2], sembuf=n2)
            for pc in range(12):
                wt = rw.next()
                m.dma("sp", wt[:], w_mod.t[l, :, pc * 512:(pc + 1) * 512].rearrange("(k p) n -> p k n", p=128),
                      reads=[w_mod], writes=[wt], sembuf=wt)
                p = nps()
                for jj in range(4):
                    for k in range(KC):
                        m.op("pe", lambda e, p=p, jj=jj, k=k: e.matmul(
                            p[:, jj * R:(jj + 1) * R], lhsT=wt[:, k, jj * 128:(jj + 1) * 128], rhs=csil[:, k, :],
                            start=(k == 0), stop=(k == KC - 1)), reads=[wt, csil], writes=[p])
                for jj in range(4):
                    ch = pc * 4 + jj
                    m.op("act", lambda e, p=p, jj=jj, ch=ch: e.activation(
                        out=modT[:, :, ch], in_=p[:, jj * R:(jj + 1) * R], func=AF.Identity,
                        bias=bmod[:, ch:ch + 1], scale=1.0), reads=[p, bmod], writes=[modT])
            for r in range(R):
                m.op("dve", lambda e, r=r: e.scalar_tensor_tensor(
                    out=A1[:, r, :], in0=modT[:, r, 8:16], scalar=1.0, in1=n1[:], op0=ALU.add, op1=ALU.mult),
                    reads=[modT, n1], writes=[A1])
                m.op("dve", lambda e, r=r: e.scalar_tensor_tensor(
                    out=A2[:, r, :], in0=modT[:, r, 32:40], scalar=1.0, in1=n2[:], op0=ALU.add, op1=ALU.mult),
                    reads=[modT, n2], writes=[A2])
        barrier()

    def norm_mod(xt, ts, Aap, Bap, sqb, rt, rstd, tmp, hdst, hf=None):
        m.op("act", lambda e: e.activation(out=sqb[:, :, 0:ts], in_=xt[:, :, 0:ts], func=AF.Square),
             reads=[xt], writes=[sqb])
        p = nps()
        for k in range(KC):
            m.op("pe", lambda e, k=k: e.matmul(p[:, 0:ts], lhsT=ones_bf[:], rhs=sqb[:, k, 0:ts],
                                               start=(k == 0), stop=(k == KC - 1)),
                 reads=[ones_bf, sqb], writes=[p])
        m.op("act", lambda e: e.activation(out=rt[:, 0:ts], in_=p[:, 0:ts], func=AF.Sqrt,
                                           bias=epsc[:], scale=1.0 / D), reads=[p, epsc], writes=[rt])
        m.op("dve", lambda e: e.reciprocal(out=rstd[:, 0:ts], in_=rt[:, 0:ts]), reads=[rt], writes=[rstd])
        for k in range(KC):
            m.op("dve", lambda e, k=k: e.scalar_tensor_tensor(
                out=tmp[:, k, 0:ts], in0=xt[:, k, 0:ts], scalar=Aap(k), in1=rstd[:, 0:ts],
                op0=ALU.mult, op1=ALU.mult), reads=[xt, rstd, A1, A2], writes=[tmp])
            if hf is None:
                m.op("act", lambda e, k=k: e.activation(out=hdst[:, k, 0:ts], in_=tmp[:, k, 0:ts],
                                                        func=AF.Identity, bias=Bap(k), scale=1.0),
                     reads=[tmp, modT], writes=[hdst])
            else:
                m.op("act", lambda e, k=k: e.activation(out=hf[:, k, 0:ts], in_=tmp[:, k, 0:ts],
                                                        func=AF.Identity, bias=Bap(k), scale=1.0),
                     reads=[tmp, modT], writes=[hf])
        if hf is not None:
            m.op("pool", lambda e: e.tensor_copy(out=hdst[:, :, 0:ts], in_=hf[:, :, 0:ts]),
                 reads=[hf], writes=[hdst])

    def stage1(l, xr):
        with ExitStack() as es:
            w1 = stage_sb(es, "w1", [128, KC, 3072], BF16)
            wv = Wb_in[l].t.rearrange("(k p) n -> p k n", p=128)
            m.dma("sp", w1[:, :, 0:2048], wv[:, :, 0:2048], reads=[Wb_in[l]], writes=[w1], sembuf=w1)
            m.dma("sp", w1[:, :, 2048:3072], wv[:, :, 5120:6144], reads=[Wb_in[l]], writes=[w1], sembuf=w1)
            rx = Ring("s1x", 2, [128, KC, 512], F32, es)
            rtab = Ring("s1tab", 2, [128, 2, 512], F32, es)
            sqb = stage_sb(es, "s1sq", [128, KC, 512], BF16)
            rt = stage_sb(es, "s1rt", [128, 512], F32)
            rstd = stage_sb(es, "s1rstd", [128, 512], F32)
            tmp = stage_sb(es, "s1tmp", [128, KC, 512], F32)
            rh = Ring("s1h", 2, [128, KC, 512], BF16, es)
            rfm = Ring("s1fm", 2, [128, 12, 512], BF16, es)
            cg = Ring("s1cg", 2, [128, 512], BF16, es)
            t1r = Ring("s1t1", 2, [128, 512], F32, es)
            t2r = Ring("s1t2", 2, [128, 512], F32, es)
            rvn = Ring("s1vn", 2, [128, 4, 512], BF16, es)
            rva = Ring("s1va", 2, [128, 4, 512], BF16, es)
            vg = Ring("s1vg", 2, [128, 256], F32, es)
            st6 = Ring("s1st", 2, [128, 6], F32, es)
            mv = Ring("s1mv", 2, [128, 2], F32, es)
            sd = Ring("s1sd", 2, [128, 1], F32, es)
            rs = Ring("s1rs", 2, [128, 1], F32, es)
            for s_ in rvn.slots + rva.slots:
                m.op("pool", lambda e, s_=s_: e.memset(s_[:], 0.0), writes=[s_])
            def front(t0, ts, r):
                xt = rx.next()
                m.dma("sp", xt[:, :, 0:ts], fm(xr, t0, ts), reads=[xr], writes=[xt], sembuf=xt)
                tb = rtab.next()
                m.dma("sp", tb[:, 0, 0:ts], tabC[:, t0:t0 + ts], reads=[tabC], writes=[tb], sembuf=tb)
                m.dma("sp", tb[:, 1, 0:ts], tabS[:, t0:t0 + ts], reads=[tabS], writes=[tb], sembuf=tb)
                h = rh.next()
                norm_mod(xt, ts, lambda k: A1[:, r, k:k + 1], lambda k: modT[:, r, k:k + 1],
                         sqb, rt, rstd, tmp, h)
                m.dma("act", fm(H1, t0, ts), h[:, :, 0:ts], reads=[h], writes=[H1], sembuf=h)
                return h, tb

            tls = tiles512()
            nxt = front(*tls[0])
            for ti, (t0, ts, r) in enumerate(tls):
                nb = ts // 128
                h, tb = nxt
                if ti + 1 < len(tls):
                    nxt = front(*tls[ti + 1])
                f = rfm.next()

                def proj(co):
                    p = nps()
                    for k in range(KC):
                        m.op("pe", lambda e, k=k: e.matmul(p[:, 0:ts], lhsT=w1[:, k, co:co + 128],
                                                           rhs=h[:, k, 0:ts], start=(k == 0), stop=(k == KC - 1)),
                             reads=[w1, h], writes=[p])
                    return p
                for j in range(2):
                    p = proj(0 + j * 128)
                    m.op("act", lambda e, p=p, j=j: e.activation(out=f[:, 0 + j, 0:ts], in_=p[:, 0:ts], func=AF.Copy),
                         reads=[p], writes=[f])
                for j in range(2):
                    p = proj(256 + j * 128)
                    c_ = cg.next()
                    m.op("act", lambda e, p=p, c_=c_: e.activation(out=c_[:, 0:ts], in_=p[:, 0:ts], func=AF.Copy),
                         reads=[p], writes=[c_])
                    p2 = proj(512 + j * 128)
                    m.op("dve", lambda e, p2=p2, c_=c_, j=j: e.tensor_tensor(
                        out=f[:, 2 + j, 0:ts], in0=p2[:, 0:ts], in1=c_[:, 0:ts], op=ALU.mult),
                        reads=[p2, c_], writes=[f])
                for j in range(2):
                    p = proj(768 + j * 128)
                    m.op("act", lambda e, p=p, j=j: e.activation(out=f[:, 4 + j, 0:ts], in_=p[:, 0:ts],
                                                                 func=AF.Gelu_apprx_tanh), reads=[p], writes=[f])
                for (co, cop, fo, n) in ((1280, 2048, 6, 4), (2560, 2816, 10, 2)):
                    for j in range(n):
                        p = proj(co + j * 128)
                        pp = proj(cop + j * 128)
                        a1 = t1r.next()
                        a2 = t2r.next()
                        m.op("dve", lambda e, p=p, a1=a1: e.tensor_tensor(
                            out=a1[:, 0:ts], in0=p[:, 0:ts], in1=tb[:, 0, 0:ts], op=ALU.mult),
                            reads=[p, tb], writes=[a1])
                        m.op("dve", lambda e, pp=pp, a2=a2: e.tensor_tensor(
                            out=a2[:, 0:ts], in0=pp[:, 0:ts], in1=tb[:, 1, 0:ts], op=ALU.mult),
                            reads=[pp, tb], writes=[a2])
                        m.op("pool", lambda e, a1=a1, a2=a2, fo=fo, j=j: e.tensor_tensor(
                            out=f[:, fo + j, 0:ts], in0=a1[:, 0:ts], in1=a2[:, 0:ts], op=ALU.add),
                            reads=[a1, a2], writes=[f])
                m.dma("act", fm(BGd, t0, ts), f[:, 0:2, 0:ts], reads=[f], writes=[BGd], sembuf=f)
                m.dma("act", fm(CHd, t0, ts), f[:, 2:4, 0:ts], reads=[f], writes=[CHd], sembuf=f)
                m.dma("act", fm(UGd, t0, ts), f[:, 4:6, 0:ts], reads=[f], writes=[UGd], sembuf=f)
                m.dma("act", fm(Qd, t0, ts), f[:, 6:10, 0:ts], reads=[f], writes=[Qd], sembuf=f)
                m.dma("act", fm(KKd, t0, ts), f[:, 10:12, 0:ts], reads=[f], writes=[KKd], sembuf=f)
                vn = rvn.next()
                va = rva.next()
                for blk in range(nb):
                    pv = nps()
                    for k in range(KC):
                        m.op("pe", lambda e, k=k, pv=pv, blk=blk: e.matmul(
                            pv[:, 0:256], lhsT=h[:, k, blk * 128:(blk + 1) * 128], rhs=w1[:, k, 1024:1280],
                            start=(k == 0), stop=(k == KC - 1)), reads=[w1, h], writes=[pv])
                    pa = nps()
                    for k in range(KC):
                        m.op("pe", lambda e, k=k, pa=pa, blk=blk: e.matmul(
                            pa[:, 0:128], lhsT=h[:, k, blk * 128:(blk + 1) * 128], rhs=w1[:, k, 1920:2048],
                            start=(k == 0), stop=(k == KC - 1)), reads=[w1, h], writes=[pa])
                    g_ = vg.next()
                    m.op("act", lambda e, pv=pv, g_=g_: e.activation(out=g_[:], in_=pv[:, 0:256],
                                                                     func=AF.Gelu_apprx_tanh),
                         reads=[pv], writes=[g_])
                    s6 = st6.next()
                    m.op("dve", lambda e, g_=g_, s6=s6: e.bn_stats(out=s6[:], in_=g_[:]), reads=[g_], writes=[s6])
                    mv_ = mv.next()
                    m.op("dve", lambda e, mv_=mv_, s6=s6: e.bn_aggr(out=mv_[:], in_=s6[:]), reads=[s6], writes=[mv_])
                    sd_ = sd.next()
                    m.op("act", lambda e, sd_=sd_, mv_=mv_: e.activation(out=sd_[:], in_=mv_[:, 1:2], func=AF.Sqrt,
                                                                         bias=epsc[:], scale=1.0),
                         reads=[mv_, epsc], writes=[sd_])
                    rs_ = rs.next()
                    m.op("dve", lambda e, sd_=sd_, rs_=rs_: e.reciprocal(out=rs_[:], in_=sd_[:]),
                         reads=[sd_], writes=[rs_])
                    for par in range(2):
                        src = g_[:].rearrange("p (g c) -> p g c", c=64)[:, par::2, :]
                        dst = vn[:, blk, :].rearrange("p (g c) -> p g c", c=128)[:, par::2, par * 64:(par + 1) * 64]
                        m.op("dve", lambda e, src=src, dst=dst, mv_=mv_, rs_=rs_: e.tensor_scalar(
                            out=dst, in0=src, scalar1=mv_[:, 0:1], scalar2=rs_[:, 0:1],
                            op0=ALU.subtract, op1=ALU.mult), reads=[g_, mv_, rs_], writes=[vn])
                        srca = pa[:, 0:128].rearrange("p (kv c) -> p kv c", c=64)
                        dsta = va[:, blk, :].rearrange("p (kv q c) -> p kv q c", kv=2, q=2)[:, :, par, par * 64:(par + 1) * 64]
                        m.op("act", lambda e, srca=srca, dsta=dsta: e.activation(out=dsta, in_=srca, func=AF.Copy),
                             reads=[pa], writes=[va])
                m.dma("act", VNXd.t[t0:t0 + ts, :].rearrange("(b p) c -> p b c", p=128), vn[:, 0:nb, :],
                      reads=[vn], writes=[VNXd], sembuf=vn)
                m.dma("act", VAXd.t[t0:t0 + ts, :].rearrange("(b p) c -> p b c", p=128), va[:, 0:nb, :],
                      reads=[va], writes=[VAXd], sembuf=va)
        barrier()

    def stage2(l, last):
        with ExitStack() as es:
            cw = stage_sb(es, "s2cw", [128, 2, 3], F32)
            wsf = stage_sb(es, "s2wsf", [128, 4, 128], F32)
            wsb = stage_sb(es, "s2wsb", [128, 4, 128], BF16)
            gbt = stage_sb(es, "s2gb", [128, 2, 128], F32)
            snk = stage_sb(es, "s2snk", [128, NH], F32)
            nsnk = stage_sb(es, "s2nsnk", [128, NH], F32)
            msk = stage_sb(es, "s2msk", [128, 384], F32)
            m.dma("sp", cw[:], convT[l], reads=[convT], writes=[cw], sembuf=cw)
            m.dma("sp", wsf[:], wsT_in[l], reads=[wsT_in], writes=[wsf], sembuf=wsf)
            m.dma("sp", gbt[:], gb_in[l], reads=[gb_in], writes=[gbt], sembuf=gbt)
            m.dma("sp", snk[:], sink_in[l], reads=[sink_in], writes=[snk], sembuf=snk)
            m.dma("sp", msk[:], mask_in[:], reads=[mask_in], writes=[msk], sembuf=msk)
            m.op("dve", lambda e: e.tensor_copy(out=wsb[:], in_=wsf[:]), reads=[wsf], writes=[wsb])
            m.op("dve", lambda e: e.tensor_scalar(out=nsnk[:], in0=snk[:], scalar1=-1.0, scalar2=None,
                                                  op0=ALU.mult), reads=[snk], writes=[nsnk])
            kkc = stage_sb(es, "s2kkc", [128, 2, LC], BF16)
            vaxc = stage_sb(es, "s2vaxc", [128, 2, 512], BF16)
            rch = Ring("s2ch", 2, [128, 2, 514], BF16, es)
            rbg = Ring("s2bg", 2, [128, 2, 512], BF16, es)
            rug = Ring("s2ug", 2, [128, 2, 512], BF16, es)
            rvn = Ring("s2vn", 2, [128, 4, 512], BF16, es)
            rq = Ring("s2q", 2, [128, 4, 512], BF16, es)
            rkk = Ring("s2kk", 2, [128, 2, 768], BF16, es)
            rvx = Ring("s2vx", 2, [128, 6, 512], BF16, es)
            ry = Ring("s2y", 2, [128, KC, 512], BF16, es)
            acc = Ring("s2acc", 2, [128, 512], F32, es)
            gt = Ring("s2gt", 2, [128, 2, 128], F32, es)
            sm4 = Ring("s2sm", 2, [128, 4, 640], F32, es)
            pe4 = Ring("s2pe", 2, [128, 4, 640], F32, es)
            pn4 = Ring("s2pn", 2, [128, 4, 640], F32, es)
            pT4 = Ring("s2pT", 2, [128, 4, 5, 128], BF16, es)
            sc4 = [Ring("s2sc%d" % i, 2, [128, 4], F32, es) for i in range(7)]
            for b in range(NB):
                m.dma("sp", kkc[:], fm(KKd, b * LC, LC), reads=[KKd], writes=[kkc], sembuf=kkc)
                m.dma("sp", vaxc[:], VAXd.t[b * LC:(b + 1) * LC, :].rearrange("(b p) c -> p b c", p=128),
                      reads=[VAXd], writes=[vaxc], sembuf=vaxc)
                tl = []
                if not last:
                    tl.append((b * LC, LC, 0, LC, True))
                for i in range(S // 512):
                    tl.append((CT + b * S + i * 512, 512, i * 512, S, False))
                for (t0, ts, s0, slen, isctx) in tl:
                    nb = ts // 128
                    hl = 1 if s0 > 0 else 0
                    hr = 1 if s0 + ts < slen else 0
                    ch = rch.next()
                    if not hl:
                        m.op("pool", lambda e, ch=ch: e.memset(ch[:, :, 0:1], 0.0), writes=[ch])
                    if not hr:
                        m.op("pool", lambda e, ch=ch: e.memset(ch[:, :, ts + 1:ts + 2], 0.0), writes=[ch])
                    m.dma("sp", ch[:, :, 1 - hl:ts + 1 + hr], fm(CHd, t0 - hl, ts + hl + hr),
                          reads=[CHd], writes=[ch], sembuf=ch)
                    bg = rbg.next()
                    m.dma("sp", bg[:, :, 0:ts], fm(BGd, t0, ts), reads=[BGd], writes=[bg], sembuf=bg)
                    ug = rug.next()
                    m.dma("sp", ug[:, :, 0:ts], fm(UGd, t0, ts), reads=[UGd], writes=[ug], sembuf=ug)
                    vn = rvn.next()
                    m.dma("sp", vn[:, 0:nb, :], VNXd.t[t0:t0 + ts, :].rearrange("(b p) c -> p b c", p=128),
                          reads=[VNXd], writes=[vn], sembuf=vn)
                    q = rq.next()
                    m.dma("sp", q[:, :, 0:ts], fm(Qd, t0, ts), reads=[Qd], writes=[q], sembuf=q)
                    kk = vx = None
                    if not isctx:
                        kl = 128 if s0 > 0 else 0
                        kr = 128 if s0 + ts < slen else 0
                        kk = rkk.next()
                        m.dma("sp", kk[:, :, 128 - kl:128 + ts + kr], fm(KKd, t0 - kl, ts + kl + kr),
                              reads=[KKd], writes=[kk], sembuf=kk)
                        vx = rvx.next()
                        nbl = (kl + ts + kr) // 128
                        b0 = 1 - kl // 128
                        m.dma("sp", vx[:, b0:b0 + nbl, :],
                              VAXd.t[t0 - kl:t0 + ts + kr, :].rearrange("(b p) c -> p b c", p=128),
                              reads=[VAXd], writes=[vx], sembuf=vx)
                    y = ry.next()
                    for j in range(2):
                        a = acc.next()
                        m.op("dve", lambda e, a=a, j=j: e.tensor_scalar(
                            out=a[:, 0:ts], in0=ch[:, j, 1:ts + 1], scalar1=cw[:, j, 1:2], scalar2=None,
                            op0=ALU.mult), reads=[ch, cw], writes=[a])
                        m.op("dve", lambda e, a=a, j=j: e.scalar_tensor_tensor(
                            out=a[:, 0:ts], in0=ch[:, j, 0:ts], scalar=cw[:, j, 0:1], in1=a[:, 0:ts],
                            op0=ALU.mult, op1=ALU.add), reads=[ch, cw, a], writes=[a])
                        m.op("dve", lambda e, a=a, j=j: e.scalar_tensor_tensor(
                            out=a[:, 0:ts], in0=ch[:, j, 2:ts + 2], scalar=cw[:, j, 2:3], in1=a[:, 0:ts],
                            op0=ALU.mult, op1=ALU.add), reads=[ch, cw, a], writes=[a])
                        m.op("pool", lambda e, a=a, j=j: e.tensor_tensor(
                            out=y[:, j, 0:ts], in0=a[:, 0:ts], in1=bg[:, j, 0:ts], op=ALU.mult),
                            reads=[a, bg], writes=[y])
                    for blk in range(nb):
                        p = nps()
                        for j in range(2):
                            for gg in range(2):
                                g = 2 * j + gg
                                m.op("pe", lambda e, p=p, j=j, g=g, gg=gg, blk=blk: e.matmul(
                                    p[:, j * 128:(j + 1) * 128], lhsT=vn[:, blk, g * 128:(g + 1) * 128],
                                    rhs=wsb[:, g, :], start=(gg == 0), stop=(gg == 1)),
                                    reads=[vn, wsb], writes=[p])
                        g_ = gt.next()
                        m.op("dve", lambda e, p=p, g_=g_: e.tensor_tensor(
                            out=g_[:], in0=p[:, 0:256].rearrange("p (j t) -> p j t", j=2), in1=gbt[:],
                            op=ALU.add), reads=[p, gbt], writes=[g_])
                        m.op("pool", lambda e, g_=g_, blk=blk: e.tensor_tensor(
                            out=y[:, 2:4, blk * 128:(blk + 1) * 128], in0=g_[:],
                            in1=ug[:, :, blk * 128:(blk + 1) * 128], op=ALU.mult),
                            reads=[g_, ug], writes=[y])
                    for blk in range(nb):
                        nbk = (s0 // 128) + blk
                        if isctx:
                            lo = hi = 384
                        else:
                            lo = 128 if nbk == 0 else 0
                            hi = 256 if nbk == slen // 128 - 1 else 384
                        kbs = list(range(lo // 128, hi // 128))
                        for kv in range(2):
                            h0 = kv * 4
                            A = PS[0:4]
                            Bk = PS[4:6]
                            O = PS[6:8]
                            s4 = sm4.next()
                            if hi == lo:
                                m.op("pool", lambda e: e.memset(s4[:, :, 0:384], NEG), writes=[s4])
                            else:
                                if lo > 0:
                                    m.op("pool", lambda e: e.memset(s4[:, :, 0:lo], NEG), writes=[s4])
                                if hi < 384:
                                    m.op("pool", lambda e: e.memset(s4[:, :, hi:384], NEG), writes=[s4])
                            for hq in range(4):
                                hh = h0 + hq
                                c = hh // 2
                                pb = (hh % 2) * 64
                                if hi > lo:
                                    m.op("pe", lambda e: e.matmul(
                                        A[hq][:, lo:hi], lhsT=q[pb:pb + 64, c, blk * 128:(blk + 1) * 128],
                                        rhs=kk[pb:pb + 64, kv, blk * 128 + lo:blk * 128 + hi], start=True, stop=True),
                                        reads=[q, kk], writes=[A[hq]])
                                m.op("pe", lambda e: e.matmul(
                                    Bk[hq % 2][:, (hq // 2) * 256:(hq // 2) * 256 + 256],
                                    lhsT=q[pb:pb + 64, c, blk * 128:(blk + 1) * 128],
                                    rhs=kkc[pb:pb + 64, kv, :], start=True, stop=True),
                                    reads=[q, kkc], writes=[Bk[hq % 2]])
                            if hi > lo:
                                m.op("dve", lambda e: e.tensor_tensor(
                                    out=s4[:, :, lo:hi], in0=psall[:, 0:4, lo:hi],
                                    in1=msk[:, lo:hi].unsqueeze(1).to_broadcast([128, 4, hi - lo]), op=ALU.add),
                                    reads=A + [msk], writes=[s4])
                            m.op("act", lambda e: e.activation(
                                out=s4[:, :, 384:640].rearrange("p (h b) c -> p h b c", b=2),
                                in_=psall[:, 4:6, :].rearrange("p b (h c) -> p h b c", h=2),
                                func=AF.Copy), reads=Bk, writes=[s4])
                            mx = sc4[0].next()
                            m.op("dve", lambda e: e.tensor_reduce(out=mx[:], in_=s4[:], axis=AX.X, op=ALU.max),
                                 reads=[s4], writes=[mx])
                            ngm = sc4[1].next()
                            m.op("dve", lambda e: e.scalar_tensor_tensor(
                                out=ngm[:], in0=mx[:], scalar=-SCALE, in1=nsnk[:, h0:h0 + 4], op0=ALU.mult, op1=ALU.min),
                                reads=[mx, nsnk], writes=[ngm])
                            p4 = pe4.next()
                            for hq in range(4):
                                m.op("act", lambda e: e.activation(
                                    out=p4[:, hq, :], in_=s4[:, hq, :], func=AF.Exp, bias=ngm[:, hq:hq + 1], scale=SCALE),
                                    reads=[s4, ngm], writes=[p4])
                            tt = sc4[2].next()
                            m.op("dve", lambda e: e.tensor_tensor(out=tt[:], in0=snk[:, h0:h0 + 4], in1=ngm[:], op=ALU.add),
                                 reads=[snk, ngm], writes=[tt])
                            es_ = sc4[3].next()
                            m.op("act", lambda e: e.activation(out=es_[:], in_=tt[:], func=AF.Exp), reads=[tt], writes=[es_])
                            rsum = sc4[4].next()
                            m.op("dve", lambda e: e.tensor_reduce(out=rsum[:], in_=p4[:], axis=AX.X, op=ALU.add),
                                 reads=[p4], writes=[rsum])
                            den = sc4[5].next()
                            m.op("dve", lambda e: e.tensor_tensor(out=den[:], in0=rsum[:], in1=es_[:], op=ALU.add),
                                 reads=[rsum, es_], writes=[den])
                            inv = sc4[6].next()
                            m.op("dve", lambda e: e.reciprocal(out=inv[:], in_=den[:]), reads=[den], writes=[inv])
                            n4 = pn4.next()
                            m.op("dve", lambda e: e.tensor_tensor(
                                out=n4[:], in0=p4[:], in1=inv[:].unsqueeze(2).to_broadcast([128, 4, 640]), op=ALU.mult),
                                reads=[p4, inv], writes=[n4])
                            for hq in range(4):
                                for kb in kbs:
                                    m.op("pe", lambda e: e.transpose(
                                        A[hq][:, kb * 128:(kb + 1) * 128], n4[:, hq, kb * 128:(kb + 1) * 128], ident[:]),
                                        reads=[n4, ident], writes=[A[hq]])
                                for cb in range(2):
                                    o_ = (hq // 2) * 256 + cb * 128
                                    m.op("pe", lambda e: e.transpose(
                                        Bk[hq % 2][:, o_:o_ + 128], n4[:, hq, 384 + cb * 128:384 + (cb + 1) * 128], ident[:]),
                                        reads=[n4, ident], writes=[Bk[hq % 2]])
                            pt = pT4.next()
                            if kbs:
                                k0, k1 = kbs[0], kbs[-1] + 1
                                m.op("dve", lambda e: e.tensor_copy(
                                    out=pt[:, :, k0:k1, :],
                                    in_=psall[:, 0:4, k0 * 128:k1 * 128].rearrange("p h (k t) -> p h k t", t=128)),
                                    reads=A, writes=[pt])
                            for h2_ in range(2):
                                m.op("act", lambda e: e.activation(
                                    out=pt[:, 2 * h2_:2 * h2_ + 2, 3:5, :],
                                    in_=psall[:, 4:6, h2_ * 256:(h2_ + 1) * 256].rearrange("p b (k t) -> p b k t", k=2),
                                    func=AF.Copy), reads=Bk, writes=[pt])
                            for cp in range(2):
                                pO = O[cp]
                                first = True
                                for par in range(2):
                                    hq = 2 * cp + par
                                    seq = [(vx, blk + kb, kb) for kb in kbs] + [(vaxc, cb, 3 + cb) for cb in range(2)]
                                    for i_, (vb, vi, pi) in enumerate(seq):
                                        lastmm = (par == 1 and i_ == len(seq) - 1)
                                        m.op("pe", lambda e: e.matmul(
                                            pO[:, 0:128], lhsT=vb[:, vi, kv * 256 + par * 128:kv * 256 + (par + 1) * 128],
                                            rhs=pt[:, hq, pi, :], start=first, stop=lastmm),
                                            reads=[vb, pt], writes=[pO])
                                        first = False
                            m.op("act", lambda e: e.activation(
                                out=y[:, 4 + kv * 2:6 + kv * 2, blk * 128:(blk + 1) * 128], in_=psall[:, 6:8, 0:128],
                                func=AF.Copy), reads=O, writes=[y])
                    m.dma("act", fm(Yd, t0, ts), y[:, :, 0:ts], reads=[y], writes=[Yd], sembuf=y)
        barrier()

    def stage3a(l, last):
        with ExitStack() as es:
            wg = stage_sb(es, "s3wg", [128, KC, 3072], BF16)
            wbr = stage_sb(es, "s3wbr", [128, KC, D], BF16)
            wv = Wb_in[l].t.rearrange("(k p) n -> p k n", p=128)
            m.dma("sp", wg[:], wv[:, :, 2048:5120], reads=[Wb_in[l]], writes=[wg], sembuf=wg)
            m.dma("sp", wbr[:], Wb_br[l].t.rearrange("(k p) n -> p k n", p=128), reads=[Wb_br[l]], writes=[wbr], sembuf=wbr)
            rh = Ring("s3h", 2, [128, KC, 512], BF16, es)
            ry = Ring("s3y", 2, [128, KC, 512], BF16, es)
            rmix = Ring("s3mix", 2, [128, KC, 512], BF16, es)
            sg = Ring("s3sg", 3, [128, 512], F32, es)
            tmp = Ring("s3tmp", 3, [128, 512], F32, es)
            mixf = Ring("s3mixf", 2, [128, 512], F32, es)
            brk = ((0, 2), (2, 4), (4, 8))
            for (t0, ts, r) in tiles512():
                if last and r == R - 1:
                    continue
                h = rh.next()
                m.dma("sp", h[:, :, 0:ts], fm(H1, t0, ts), reads=[H1], writes=[h], sembuf=h)
                y = ry.next()
                m.dma("sp", y[:, :, 0:ts], fm(Yd, t0, ts), reads=[Yd], writes=[y], sembuf=y)
                mix = rmix.next()
                for j in range(KC):
                    mf = mixf.next()
                    for bi, (ka, kb) in enumerate(brk):
                        pg = nps()
                        for k in range(KC):
                            m.op("pe", lambda e, k=k, pg=pg, bi=bi, j=j: e.matmul(
                                pg[:, 0:ts], lhsT=wg[:, k, bi * 1024 + j * 128:bi * 1024 + (j + 1) * 128],
                                rhs=h[:, k, 0:ts], start=(k == 0), stop=(k == KC - 1)), reads=[wg, h], writes=[pg])
                        pp = nps()
                        for k in range(ka, kb):
                            m.op("pe", lambda e, k=k, pp=pp, j=j, ka=ka, kb=kb: e.matmul(
                                pp[:, 0:ts], lhsT=wbr[:, k, j * 128:(j + 1) * 128], rhs=y[:, k, 0:ts],
                                start=(k == ka), stop=(k == kb - 1)), reads=[wbr, y], writes=[pp])
                        s_ = sg.next()
                        m.op("act", lambda e, s_=s_, pg=pg: e.activation(out=s_[:, 0:ts], in_=pg[:, 0:ts],
                                                                         func=AF.Sigmoid), reads=[pg], writes=[s_])
                        if bi == 0:
                            m.op("dve", lambda e, mf=mf, pp=pp, s_=s_: e.tensor_tensor(
                                out=mf[:, 0:ts], in0=pp[:, 0:ts], in1=s_[:, 0:ts], op=ALU.mult),
                                reads=[pp, s_], writes=[mf])
                        else:
                            t_ = tmp.next()
                            m.op("dve", lambda e, t_=t_, pp=pp, s_=s_: e.tensor_tensor(
                                out=t_[:, 0:ts], in0=pp[:, 0:ts], in1=s_[:, 0:ts], op=ALU.mult),
                                reads=[pp, s_], writes=[t_])
                            if bi == 1:
                                m.op("pool", lambda e, mf=mf, t_=t_: e.tensor_tensor(
                                    out=mf[:, 0:ts], in0=mf[:, 0:ts], in1=t_[:, 0:ts], op=ALU.add),
                                    reads=[mf, t_], writes=[mf])
                            else:
                                m.op("pool", lambda e, mf=mf, t_=t_, j=j: e.tensor_tensor(
                                    out=mix[:, j, 0:ts], in0=mf[:, 0:ts], in1=t_[:, 0:ts], op=ALU.add),
                                    reads=[mf, t_], writes=[mix])
                m.dma("act", fm(MIXd, t0, ts), mix[:, :, 0:ts], reads=[mix], writes=[MIXd], sembuf=mix)
        barrier()

    def stage3b(l, last, xr, xw):
        moe = (l % 2 == 1)
        with ExitStack() as es:
            wo = stage_sb(es, "s3wo", [128, KC, D], BF16)
            m.dma("sp", wo[:], Wb_out[l].t.rearrange("(k p) n -> p k n", p=128), reads=[Wb_out[l]], writes=[wo], sembuf=wo)
            rtw = stage_sb(es, "s3rt", [128, KC, E], F32)
            if moe:
                m.dma("sp", rtw[:], moe_rt.t[l // 2].rearrange("(k p) e -> p k e", p=128),
                      reads=[moe_rt], writes=[rtw], sembuf=rtw)
            rmix = Ring("s3bmix", 2, [128, KC, 512], BF16, es)
            rx = Ring("s3bx", 2, [128, KC, 512], F32, es)
            sqb = stage_sb(es, "s3bsq", [128, KC, 512], BF16)
            rt = stage_sb(es, "s3brt", [128, 512], F32)
            rstd = stage_sb(es, "s3brstd", [128, 512], F32)
            tmp = stage_sb(es, "s3btmp", [128, KC, 512], F32)
            hf = stage_sb(es, "s3bhf", [128, KC, 512], F32)
            rh2 = Ring("s3bh2", 2, [128, KC, 512], BF16, es)
            rgt = Ring("s3bgt", 2, [E, 512], BF16, es)
            sm8 = [Ring("s3bs%d" % i, 2, [128, E], F32, es) for i in range(5)]
            sc1 = [Ring("s3bc%d" % i, 2, [128, 1], F32, es) for i in range(7)]
            for (t0, ts, r) in tiles512():
                if last and r == R - 1:
                    continue
                nb = ts // 128
                mix = rmix.next()
                m.dma("sp", mix[:, :, 0:ts], fm(MIXd, t0, ts), reads=[MIXd], writes=[mix], sembuf=mix)
                xt = rx.next()
                m.dma("sp", xt[:, :, 0:ts], fm(xr, t0, ts), reads=[xr], writes=[xt], sembuf=xt)
                for j in range(KC):
                    p = nps()
                    for k in range(KC):
                        m.op("pe", lambda e, k=k, p=p, j=j: e.matmul(
                            p[:, 0:ts], lhsT=wo[:, k, j * 128:(j + 1) * 128], rhs=mix[:, k, 0:ts],
                            start=(k == 0), stop=(k == KC - 1)), reads=[wo, mix], writes=[p])
                    m.op("dve", lambda e, p=p, j=j: e.scalar_tensor_tensor(
                        out=xt[:, j, 0:ts], in0=p[:, 0:ts], scalar=modT[:, r, 16 + j:17 + j], in1=xt[:, j, 0:ts],
                        op0=ALU.mult, op1=ALU.add), reads=[p, modT, xt], writes=[xt])
                m.dma("act", fm(xw, t0, ts), xt[:, :, 0:ts], reads=[xt], writes=[xw], sembuf=xt)
                h2 = rh2.next()
                norm_mod(xt, ts, lambda k: A2[:, r, k:k + 1], lambda k: modT[:, r, 24 + k:25 + k],
                         sqb, rt, rstd, tmp, h2, hf=hf if moe else None)
                m.dma("act", fm(H2d, t0, ts), h2[:, :, 0:ts], reads=[h2], writes=[H2d], sembuf=h2)
                if moe:
                    gtt = rgt.next()
                    for blk in range(nb):
                        p = nps()
                        for k in range(KC):
                            m.op("pe", lambda e, k=k, p=p, blk=blk: e.matmul(
                                p[:, 0:E], lhsT=hf[:, k, blk * 128:(blk + 1) * 128], rhs=rtw[:, k, :],
                                start=(k == 0), stop=(k == KC - 1)), reads=[hf, rtw], writes=[p])
                        lg = sm8[0].next()
                        m.op("act", lambda e, lg=lg, p=p: e.activation(out=lg[:], in_=p[:, 0:E], func=AF.Copy),
                             reads=[p], writes=[lg])
                        m1 = sc1[0].next()
                        m.op("dve", lambda e, m1=m1, lg=lg: e.tensor_reduce(out=m1[:], in_=lg[:], axis=AX.X, op=ALU.max),
                             reads=[lg], writes=[m1])
                        eq1 = sm8[1].next()
                        m.op("dve", lambda e, eq1=eq1, lg=lg, m1=m1: e.tensor_scalar(
                            out=eq1[:], in0=lg[:], scalar1=m1[:, 0:1], scalar2=None, op0=ALU.is_equal),
                            reads=[lg, m1], writes=[eq1])
                        msk2 = sm8[2].next()
                        m.op("dve", lambda e, msk2=msk2, eq1=eq1, lg=lg: e.scalar_tensor_tensor(
                            out=msk2[:], in0=eq1[:], scalar=NEG, in1=lg[:], op0=ALU.mult, op1=ALU.add),
                            reads=[eq1, lg], writes=[msk2])
                        m2 = sc1[1].next()
                        m.op("dve", lambda e, m2=m2, msk2=msk2: e.tensor_reduce(out=m2[:], in_=msk2[:], axis=AX.X, op=ALU.max),
                             reads=[msk2], writes=[m2])
                        eq2 = sm8[3].next()
                        m.op("dve", lambda e, eq2=eq2, msk2=msk2, m2=m2: e.tensor_scalar(
                            out=eq2[:], in0=msk2[:], scalar1=m2[:, 0:1], scalar2=None, op0=ALU.is_equal),
                            reads=[msk2, m2], writes=[eq2])
                        dd = sc1[2].next()
                        m.op("dve", lambda e, dd=dd, m2=m2, m1=m1: e.tensor_tensor(out=dd[:], in0=m2[:], in1=m1[:], op=ALU.subtract),
                             reads=[m1, m2], writes=[dd])
                        ee = sc1[3].next()
                        m.op("act", lambda e, ee=ee, dd=dd: e.activation(out=ee[:], in_=dd[:], func=AF.Exp),
                             reads=[dd], writes=[ee])
                        dn = sc1[4].next()
                        m.op("dve", lambda e, dn=dn, ee=ee: e.tensor_scalar(out=dn[:], in0=ee[:], scalar1=1.0, scalar2=None, op0=ALU.add),
                             reads=[ee], writes=[dn])
                        w1_ = sc1[5].next()
                        m.op("dve", lambda e, w1_=w1_, dn=dn: e.reciprocal(out=w1_[:], in_=dn[:]), reads=[dn], writes=[w1_])
                        w2_ = sc1[6].next()
                        m.op("dve", lambda e, w2_=w2_, w1_=w1_, ee=ee: e.tensor_tensor(out=w2_[:], in0=w1_[:], in1=ee[:], op=ALU.mult),
                             reads=[w1_, ee], writes=[w2_])
                        ga = sm8[4].next()
                        m.op("dve", lambda e, ga=ga, eq1=eq1, w1_=w1_: e.tensor_scalar(
                            out=ga[:], in0=eq1[:], scalar1=w1_[:, 0:1], scalar2=None, op0=ALU.mult),
                            reads=[eq1, w1_], writes=[ga])
                        m.op("dve", lambda e, ga=ga, eq2=eq2, w2_=w2_: e.scalar_tensor_tensor(
                            out=ga[:], in0=eq2[:], scalar=w2_[:, 0:1], in1=ga[:], op0=ALU.mult, op1=ALU.add),
                            reads=[eq2, w2_, ga], writes=[ga])
                        p2 = nps()
                        m.op("pe", lambda e, p2=p2, ga=ga: e.transpose(p2[0:E, 0:128], ga[:], ident[:]),
                             reads=[ga, ident], writes=[p2])
                        m.op("act", lambda e, p2=p2, gtt=gtt, blk=blk: e.activation(
                            out=gtt[:, blk * 128:(blk + 1) * 128], in_=p2[0:E, 0:128], func=AF.Copy),
                            reads=[p2], writes=[gtt])
                    m.dma("act", GTd.t[:, t0:t0 + ts], gtt[:, 0:ts], reads=[gtt], writes=[GTd], sembuf=gtt)
        barrier()

    def stage4(l, last, xr, xw):
        moe = (l % 2 == 1)
        NE = E if moe else 1
        F = FE if moe else FD
        GF = 4
        GW = GF * 128
        groups = []
        c0_ = 0
        while c0_ < F // 128:
            groups.append((c0_, min(GF, F // 128 - c0_)))
            c0_ += groups[-1][1]
        TT = 1024
        tl = []
        if not last:
            for i in range((CT + TT - 1) // TT):
                tl.append((i * TT, min(TT, CT - i * TT), R - 1))
        for b in range(NB):
            for i in range(S // TT):
                tl.append((CT + b * S + i * TT, TT, b))
        with ExitStack() as es:
            rh2 = Ring("s4h", 2, [128, KC, TT], BF16, es)
            yacc = stage_sb(es, "s4yacc", [128, KC, TT], F32)
            rgbc = Ring("s4gbc", 2, [128, E, TT], BF16, es) if moe else None
            ract = Ring("s4act", 3, [128, GF, 512], BF16, es)
            rs = Ring("s4s", 3, [128, 512], BF16, es)
            rt_ = Ring("s4t", 3, [128, 512], BF16, es)
            rxc = Ring("s4xc", 2, [128, TT], F32, es)
            rwgu = Ring("s4wgu", 3, [128, KC, 2 * GW], BF16, es)
            rwd = Ring("s4wd", 3, [128, GF, D], BF16, es)
            guv = [Wb_gu[l].t[e].rearrange("(k p) n -> p k n", p=128) for e in range(NE)]
            dv = [Wb_d[l].t[e] for e in range(NE)]
            gring = [0]
            per_tile = (len(conv_q) + len(tl) - 1) // max(len(tl), 1)
            def load_tile(ti_):
                t0_, ts_, _r = tl[ti_]
                h2_ = rh2.next()
                m.dma("act", h2_[:, :, 0:ts_], fm(H2d, t0_, ts_), reads=[H2d], writes=[h2_], sembuf=h2_)
                g_ = None
                if moe:
                    g_ = rgbc.next()
                    for e_ in range(E):
                        m.dma("act", g_[:, e_, 0:ts_], GTd.t[e_:e_ + 1, t0_:t0_ + ts_].partition_broadcast(128),
                              reads=[GTd], writes=[g_], sembuf=g_)
                return h2_, g_

            nxt = load_tile(0)
            for ti, (t0, ts, r) in enumerate(tl):
                conv_pop(per_tile)
                h2, gbc = nxt
                if ti + 1 < len(tl):
                    nxt = load_tile(ti + 1)
                halves = [(o, min(512, ts - o)) for o in range(0, ts, 512)]
                pend = None
                first_unit = [True]

                def down(act_hs, wd, fu, gf):
                    for (ho, hs), act in act_hs:
                        for j in range(KC):
                            py = PS[4 + (gring[0] % 4)]
                            gring[0] += 1
                            for f_ in range(gf):
                                m.op("pe", lambda e, py=py, f_=f_, j=j, act=act, hs=hs: e.matmul(
                                    py[:, 0:hs], lhsT=wd[:, f_, j * 128:(j + 1) * 128], rhs=act[:, f_, 0:hs],
                                    start=(f_ == 0), stop=(f_ == gf - 1)), reads=[wd, act], writes=[py])
                            if fu:
                                m.op("act", lambda e, py=py, j=j, ho=ho, hs=hs: e.activation(
                                    out=yacc[:, j, ho:ho + hs], in_=py[:, 0:hs], func=AF.Copy),
                                    reads=[py], writes=[yacc])
                            else:
                                m.op("dve", lambda e, py=py, j=j, ho=ho, hs=hs: e.tensor_tensor(
                                    out=yacc[:, j, ho:ho + hs], in0=py[:, 0:hs], in1=yacc[:, j, ho:ho + hs], op=ALU.add),
                                    reads=[py, yacc], writes=[yacc])

                ui = 0
                for e_ in range(NE):
                    for (c0, gf) in groups:
                        gw = gf * 128
                        wgu = rwgu.next()
                        m.dma("sp", wgu[:, :, 0:gw], guv[e_][:, :, c0 * 128:c0 * 128 + gw],
                              reads=[Wb_gu[l]], writes=[wgu], sembuf=wgu)
                        m.dma("sp", wgu[:, :, GW:GW + gw], guv[e_][:, :, F + c0 * 128:F + c0 * 128 + gw],
                              reads=[Wb_gu[l]], writes=[wgu], sembuf=wgu)
                        wd = rwd.next()
                        m.dma("sp", wd[:, 0:gf, :], dv[e_][c0 * 128:c0 * 128 + gw, :].rearrange("(f p) d -> p f d", p=128),
                              reads=[Wb_d[l]], writes=[wd], sembuf=wd)
                        act_hs = []
                        for (ho, hs) in halves:
                            act = ract.next()
                            for f_ in range(gf):
                                pg = PS[(ui % 2)]
                                pu = PS[2 + (ui % 2)]
                                ui += 1
                                for k in range(KC):
                                    m.op("pe", lambda e, k=k, pg=pg, f_=f_, ho=ho, hs=hs: e.matmul(
                                        pg[:, 0:hs], lhsT=wgu[:, k, f_ * 128:(f_ + 1) * 128], rhs=h2[:, k, ho:ho + hs],
                                        start=(k == 0), stop=(k == KC - 1)), reads=[wgu, h2], writes=[pg])
                                for k in range(KC):
                                    m.op("pe", lambda e, k=k, pu=pu, f_=f_, ho=ho, hs=hs: e.matmul(
                                        pu[:, 0:hs], lhsT=wgu[:, k, GW + f_ * 128:GW + (f_ + 1) * 128],
                                        rhs=h2[:, k, ho:ho + hs], start=(k == 0), stop=(k == KC - 1)),
                                        reads=[wgu, h2], writes=[pu])
                                s_ = rs.next()
                                m.op("act", lambda e, s_=s_, pg=pg, hs=hs: e.activation(
                                    out=s_[:, 0:hs], in_=pg[:, 0:hs], func=AF.Silu), reads=[pg], writes=[s_])
                                if moe:
                                    t_ = rt_.next()
                                    m.op("dve", lambda e, t_=t_, pu=pu, s_=s_, hs=hs: e.tensor_tensor(
                                        out=t_[:, 0:hs], in0=pu[:, 0:hs], in1=s_[:, 0:hs], op=ALU.mult),
                                        reads=[pu, s_], writes=[t_])
                                    m.op("pool", lambda e, t_=t_, act=act, f_=f_, e_=e_, ho=ho, hs=hs: e.tensor_tensor(
                                        out=act[:, f_, 0:hs], in0=t_[:, 0:hs], in1=gbc[:, e_, ho:ho + hs], op=ALU.mult),
                                        reads=[t_, gbc], writes=[act])
                                else:
                                    m.op("dve", lambda e, act=act, pu=pu, s_=s_, f_=f_, hs=hs: e.tensor_tensor(
                                        out=act[:, f_, 0:hs], in0=pu[:, 0:hs], in1=s_[:, 0:hs], op=ALU.mult),
                                        reads=[pu, s_], writes=[act])
                            act_hs.append(((ho, hs), act))
                            if pend is not None and (ho, hs) == halves[0]:
                                down(*pend)
                                pend = None
                        pend = (act_hs, wd, first_unit[0], gf)
                        first_unit[0] = False
                down(*pend)
                for j in range(KC):
                    xc = rxc.next()
                    m.dma("act", xc[:, 0:ts], xr.t[j, :, t0:t0 + ts], reads=[xr], writes=[xc], sembuf=xc)
                    m.op("dve", lambda e, xc=xc, j=j: e.scalar_tensor_tensor(
                        out=xc[:, 0:ts], in0=yacc[:, j, 0:ts], scalar=modT[:, r, 40 + j:41 + j], in1=xc[:, 0:ts],
                        op0=ALU.mult, op1=ALU.add), reads=[yacc, modT, xc], writes=[xc])
                    m.dma("act", xw.t[j, :, t0:t0 + ts], xc[:, 0:ts], reads=[xc], writes=[xw], sembuf=xc)
        barrier()

    def stage_final(xr):
        with ExitStack() as es:
            fn = stage_sb(es, "fn", [128, KC], F32)
            m.dma("sp", fn[:], fnT[:], reads=[fnT], writes=[fn], sembuf=fn)
            rx = Ring("sfx", 2, [128, KC, 512], F32, es)
            sqb = stage_sb(es, "sfsq", [128, KC, 512], BF16)
            rt = stage_sb(es, "sfrt", [128, 512], F32)
            rstd = stage_sb(es, "sfrstd", [128, 512], F32)
            yt = stage_sb(es, "sfy", [128, KC, 512], F32)
            ro = Ring("sfo", 2, [128, 4, D], F32, es)
            for i in range(TX // 512):
                t0 = CT + i * 512
                ts = 512
                xt = rx.next()
                m.dma("sp", xt[:], fm(xr, t0, ts), reads=[xr], writes=[xt], sembuf=xt)
                m.op("act", lambda e: e.activation(out=sqb[:], in_=xt[:], func=AF.Square), reads=[xt], writes=[sqb])
                p = nps()
                for k in range(KC):
                    m.op("pe", lambda e, k=k: e.matmul(p[:], lhsT=ones_bf[:], rhs=sqb[:, k, :], start=(k == 0),
                                                       stop=(k == KC - 1)), reads=[ones_bf, sqb], writes=[p])
                m.op("act", lambda e: e.activation(out=rt[:], in_=p[:], func=AF.Sqrt, bias=epsc[:], scale=1.0 / D),
                     reads=[p, epsc], writes=[rt])
                m.op("dve", lambda e: e.reciprocal(out=rstd[:], in_=rt[:]), reads=[rt], writes=[rstd])
                for k in range(KC):
                    m.op("dve", lambda e, k=k: e.scalar_tensor_tensor(
                        out=yt[:, k, :], in0=xt[:, k, :], scalar=fn[:, k:k + 1], in1=rstd[:],
                        op0=ALU.mult, op1=ALU.mult), reads=[xt, fn, rstd], writes=[yt])
                ot = ro.next()
                for blk in range(4):
                    for kh in range(2):
                        pp = nps()
                        for kk in range(4):
                            k = kh * 4 + kk
                            m.op("pe", lambda e, pp=pp, kk=kk, k=k, blk=blk: e.transpose(
                                pp[:, kk * 128:(kk + 1) * 128], yt[:, k, blk * 128:(blk + 1) * 128], ident[:]),
                                reads=[yt, ident], writes=[pp])
                        if (blk + kh) % 2 == 0:
                            m.op("act", lambda e, pp=pp, blk=blk, kh=kh: e.activation(
                                out=ot[:, blk, kh * 512:(kh + 1) * 512], in_=pp[:], func=AF.Copy),
                                reads=[pp], writes=[ot])
                        else:
                            m.op("dve", lambda e, pp=pp, blk=blk, kh=kh: e.tensor_copy(
                                out=ot[:, blk, kh * 512:(kh + 1) * 512], in_=pp[:]), reads=[pp], writes=[ot])
                m.dma("act", out.t[i * 512:(i + 1) * 512, :].rearrange("(b p) d -> p b d", p=128), ot[:],
                      reads=[ot], writes=[out], sembuf=ot)
        barrier()

    convert_layer(0)
    stage0()
    cur = 0
    stop = cfg.stop
    done = False
    for l in range(L):
        last = (l == L - 1)
        ada(l, l == 0)
        stage1(l, XR[cur])
        if stop == (l, 1): done = True; break
        stage2(l, last)
        if stop == (l, 2): done = True; break
        if l + 1 < L:
            convert_layer(l + 1, defer=True)
            conv_pop(4)
        stage3a(l, last)
        stage3b(l, last, XR[cur], XR[1 - cur])
        cur = 1 - cur
        if stop == (l, 3): done = True; break
        stage4(l, last, XR[cur], XR[1 - cur])
        conv_pop(len(conv_q))
        cur = 1 - cur
        if stop == (l, 4): done = True; break
    if not done:
        stage_final(XR[cur])
    nobar.clear()
    barrier()
    ges.close()
    return nc, m


def host_prep(cfg, core, inp):
    NB, S, L = cfg.NB, cfg.S, cfg.L
    f = lambda a: np.ascontiguousarray(a, dtype=np.float32)
    b0 = core * NB
    d = {}
    d["x_in"] = f(inp["x"][b0:b0 + NB].reshape(NB * S, D))
    d["c_in"] = f(inp["ctx"][b0:b0 + NB].reshape(NB * LC, D))
    cv = np.concatenate([inp["c"][b0:b0 + NB], inp["c_ctx"][None]], 0)
    d["cT"] = f(cv.reshape(cfg.R, KC, 128).transpose(2, 1, 0))
    return d


def host_shared(cfg, inp):
    L = cfg.L
    f = lambda a: np.ascontiguousarray(a, dtype=np.float32)
    d = {}
    d["w_mod"] = f(inp["w_mod"])
    d["bmodT"] = f(inp["b_mod"].reshape(L, 48, 128).transpose(0, 2, 1))
    d["n1T"] = f(inp["norm1_g"].reshape(L, KC, 128).transpose(0, 2, 1))
    d["n2T"] = f(inp["norm2_g"].reshape(L, KC, 128).transpose(0, 2, 1))
    d["fnT"] = f(inp["final_norm_g"].reshape(KC, 128).T)
    w_in = inp["w_in"]
    d["w_in"] = f(w_in)
    rs = rot_src()
    qcols = np.concatenate([1280 + hh * 64 + rs for hh in range(8)])
    kk = [np.concatenate([1792 + kv * 64 + np.arange(64)] * 2) for kv in range(2)]
    kkp = [np.concatenate([1792 + kv * 64 + rs] * 2) for kv in range(2)]
    cols = np.concatenate([qcols] + kk + kkp)
    d["w_ex"] = f(w_in[:, :, cols])
    d["convT"] = f(inp["conv_w"].transpose(0, 2, 1).reshape(L, 2, 128, 3).transpose(0, 2, 1, 3))
    d["wsT"] = f(inp["gmlp_ws"].transpose(0, 3, 1, 2))
    gbv = inp["gmlp_b"]
    gb = np.repeat(gbv[:, :, None, :], 64, axis=2)
    d["gb"] = f(gb.reshape(L, 2, 128, 128).transpose(0, 2, 1, 3))
    d["sinkbc"] = f(np.broadcast_to(inp["attn_sink"][:, None, :], (L, 128, NH)))
    d["w_br"] = f(np.concatenate([inp["w_br_conv"], inp["w_br_gmlp"], inp["w_br_attn"]], axis=1))
    d["w_out"] = f(inp["w_out"])
    d["ffn_gu"] = f(inp["ffn_w_gu"])
    d["ffn_d"] = f(inp["ffn_w_d"])
    d["moe_rt"] = f(inp["moe_router"])
    d["moe_gu"] = f(inp["moe_w_gu"])
    d["moe_d"] = f(inp["moe_w_d"])
    tc, ts_ = rope_tabs(cfg)
    d["tabC"], d["tabS"] = tc, ts_
    qi = np.arange(128)[:, None]
    jj = np.arange(384)[None, :]
    d["mask"] = np.where((jj >= qi) & (jj <= qi + 256), 0.0, NEG).astype(np.float32)
    return d


_CACHE = {}


def run(cfg, inp, ncores):
    key = (cfg.NB, cfg.S, cfg.L, cfg.dbg, cfg.stop)
    if key not in _CACHE:
        _CACHE[key] = build(cfg)
    nc, m = _CACHE[key]
    sh = host_shared(cfg, inp)
    in_maps = []
    for c in range(ncores):
        dd = dict(sh)
        dd.update(host_prep(cfg, c, inp))
        in_maps.append(dd)
    res = run_bass_kernel_spmd(nc, in_maps, core_ids=list(range(ncores)))
    return res


def kernel(**inputs):
    cfg = Cfg(NB=2, S=4096, L=4)
    inp = {k: np.asarray(v) for k, v in inputs.items()}
    res = run(cfg, inp, 8)
    outs = [r["out"].reshape(cfg.NB, cfg.S, D) for r in res.results]
    return np.ascontiguousarray(np.concatenate(outs, axis=0), dtype=np.float32)
```

```python
import numpy as np
import concourse.bass as bass
import concourse.mybir as mybir
from contextlib import ExitStack

F32 = mybir.dt.float32
BF16 = mybir.dt.bfloat16
AF = mybir.ActivationFunctionType
ALU = mybir.AluOpType
AX = mybir.AxisListType


class Buf:
    __slots__ = ("name", "t", "writers", "readers", "dsem")

    def __init__(self, name, t=None):
        self.name = name
        self.t = t
        self.writers = {}
        self.readers = {}
        self.dsem = None

    def __getitem__(self, k):
        return self.t[k]


class MK:
    ENG = ("pe", "act", "dve", "pool", "sp")

    def __init__(self, nc, es):
        self.nc = nc
        self.es = es
        self.h = {"pe": nc.tensor, "act": nc.scalar, "dve": nc.vector,
                  "pool": nc.gpsimd, "sp": nc.sync}
        self.sems = {}
        self.issued = {}
        self.seen = {e: {} for e in self.ENG}
        for e in self.ENG:
            self.sems[e] = nc.alloc_semaphore(name="s_" + e)
            self.issued[e] = 0
        self.ndsem = 0
        self.ninstr = 0
        self.stage_bufs = []
        self.free_dsems = []
        self.dkeys = set()

    def sb(self, name, shape, dt):
        t = self.es.enter_context(self.nc.sbuf_tensor(name, list(shape), dt))
        return Buf(name, t)

    def uname(self, name):
        self.uid = getattr(self, "uid", 0) + 1
        return "%s_u%d" % (name, self.uid)

    def track(self, b):
        self.stage_bufs.append(b)
        return b

    def end_stage(self):
        for b in self.stage_bufs:
            if b.dsem is not None:
                self.free_dsems.append(b.dsem)
                b.dsem = None
        self.stage_bufs = []

    def ps(self, name, shape, dt):
        t = self.es.enter_context(self.nc.psum_tensor(name, list(shape), dt))
        return Buf(name, t)

    def dram(self, name, shape, dt, kind="Internal"):
        t = self.nc.dram_tensor(name, list(shape), dt, kind=kind)
        return Buf(name, t.ap())

    def _dsem(self, b):
        if b.dsem is None:
            if self.free_dsems:
                b.dsem = self.free_dsems.pop()
                return b.dsem
            k = "q%d" % self.ndsem
            self.ndsem += 1
            self.sems[k] = self.nc.alloc_semaphore(name="s_" + k)
            self.issued[k] = 0
            self.dkeys.add(k)
            b.dsem = k
        return b.dsem

    def _need(self, eng, reads, writes):
        need = {}

        def add(k, c, kind):
            if k == eng:
                if eng == "pe":
                    return
                if kind == "war":
                    return
            if c > need.get(k, 0):
                need[k] = c

        for b in reads:
            for k, c in b.writers.items():
                add(k, c, "raw")
        for b in writes:
            for k, c in b.writers.items():
                add(k, c, "waw")
            for k, c in b.readers.items():
                add(k, c, "war")
        seen = self.seen[eng]
        hnd = self.h[eng]
        for k, c in need.items():
            if seen.get(k, 0) >= c:
                continue
            if k in self.dkeys:
                c = max(c, self.issued[k])
            hnd.wait_ge(self.sems[k], c)
            seen[k] = c

    def _mark(self, key, cnt, reads, writes):
        for b in writes:
            b.writers = {key: cnt}
            b.readers = {}
        for b in reads:
            if b not in writes:
                b.readers[key] = cnt

    def op(self, eng, fn, reads=(), writes=()):
        self._need(eng, reads, writes)
        ins = fn(self.h[eng])
        self.issued[eng] += 1
        ins.then_inc(self.sems[eng], 1)
        self._mark(eng, self.issued[eng], reads, writes)
        self.ninstr += 1
        return ins

    def dma(self, q, out, in_, reads=(), writes=(), sembuf=None, **kw):
        self._need(q, reads, writes)
        k = self._dsem(sembuf)
        ins = self.h[q].dma_start(out=out, in_=in_, **kw)
        self.issued[k] += 16
        ins.then_inc(self.sems[k], 16)
        self._mark(k, self.issued[k], reads, writes)
        self.ninstr += 1
        return ins

    def wait_all(self, eng, bufs):
        self._need(eng, bufs, ())

from concourse.bass_utils import run_bass_kernel_spmd

D = 1024
KC = 8
LC = 256
NH = 8
E = 8
FD = 2816
FE = 3584
EPS = 1e-6
SCALE = 0.125
NEG = -1e30


class Cfg:
    def __init__(self, NB=2, S=4096, L=4, dbg=False, stop=None):
        self.NB, self.S, self.L, self.dbg, self.stop = NB, S, L, dbg, stop
        self.R = NB + 1
        self.CT = NB * LC
        self.TX = NB * S
        self.TA = self.CT + self.TX


def rope_tabs(cfg):
    S = cfg.S
    rows = S // 64
    row = np.repeat(np.arange(rows), 64).astype(np.float32)
    col = np.tile(np.arange(64), rows).astype(np.float32)
    half = 32
    inv = (1.0 / (10000.0 ** (np.arange(0, half, 2, dtype=np.float32) / half))).astype(np.float32)
    ang_r = row[:, None] * inv[None, :]
    ang_c = col[:, None] * inv[None, :]
    ang = np.concatenate([ang_r, ang_r, ang_c, ang_c], axis=-1)
    cos = np.cos(ang).astype(np.float32).T
    sin = np.sin(ang).astype(np.float32).T
    sgn = np.where((np.arange(64) % 32) < 16, -1.0, 1.0).astype(np.float32)[:, None]
    sins = sin * sgn
    tc = np.ones((128, cfg.TA), np.float32)
    ts_ = np.zeros((128, cfg.TA), np.float32)
    for b in range(cfg.NB):
        o = cfg.CT + b * S
        tc[:, o:o + S] = np.concatenate([cos, cos], 0)
        ts_[:, o:o + S] = np.concatenate([sins, sins], 0)
    return tc, ts_


def rot_src():
    j = np.arange(64)
    return (j // 32) * 32 + ((j % 32) + 16) % 32


def build(cfg):
    NB, S, L, R, CT, TX, TA = cfg.NB, cfg.S, cfg.L, cfg.R, cfg.CT, cfg.TX, cfg.TA
    nc = bass.Bass("TRN2", target_bir_lowering=False)
    ges = ExitStack()
    m = MK(nc, ges)
    EI = "ExternalInput"

    x_in = m.dram("x_in", [TX, D], F32, EI)
    c_in = m.dram("c_in", [CT, D], F32, EI)
    cT_in = m.dram("cT", [128, KC, R], F32, EI)
    w_mod = m.dram("w_mod", [L, D, 6 * D], F32, EI)
    bmodT = m.dram("bmodT", [L, 128, 48], F32, EI)
    n1T = m.dram("n1T", [L, 128, KC], F32, EI)
    n2T = m.dram("n2T", [L, 128, KC], F32, EI)
    fnT = m.dram("fnT", [128, KC], F32, EI)
    w_in = m.dram("w_in", [L, D, 5120], F32, EI)
    w_ex = m.dram("w_ex", [L, D, 1024], F32, EI)
    convT = m.dram("convT", [L, 128, 2, 3], F32, EI)
    wsT_in = m.dram("wsT", [L, 128, 4, 128], F32, EI)
    gb_in = m.dram("gb", [L, 128, 2, 128], F32, EI)
    sink_in = m.dram("sinkbc", [L, 128, NH], F32, EI)
    w_br = m.dram("w_br", [L, D, D], F32, EI)
    w_out = m.dram("w_out", [L, D, D], F32, EI)
    ND = (L + 1) // 2
    NM = max(L // 2, 1)
    ffn_gu = m.dram("ffn_gu", [ND, D, 2 * FD], F32, EI)
    ffn_d = m.dram("ffn_d", [ND, FD, D], F32, EI)
    moe_rt = m.dram("moe_rt", [NM, D, E], F32, EI)
    moe_gu = m.dram("moe_gu", [NM, E, D, 2 * FE], F32, EI)
    moe_d = m.dram("moe_d", [NM, E, FE, D], F32, EI)
    tabC = m.dram("tabC", [128, TA], F32, EI)
    tabS = m.dram("tabS", [128, TA], F32, EI)
    mask_in = m.dram("mask", [128, 384], F32, EI)
    out = m.dram("out", [TX, D], F32, "ExternalOutput")

    OK = "ExternalOutput" if cfg.dbg else "Internal"
    XR = [m.dram("XR%d" % i, [KC, 128, TA], F32, OK) for i in range(2)]
    H1 = m.dram("H1", [KC, 128, TA], BF16, OK)
    BGd = m.dram("BGd", [2, 128, TA], BF16, OK)
    CHd = m.dram("CHd", [2, 128, TA], BF16, OK)
    UGd = m.dram("UGd", [2, 128, TA], BF16, OK)
    Qd = m.dram("Qd", [4, 128, TA], BF16, OK)
    KKd = m.dram("KKd", [2, 128, TA], BF16, OK)
    VNXd = m.dram("VNXd", [TA, 512], BF16, OK)
    VAXd = m.dram("VAXd", [TA, 512], BF16, OK)
    Yd = m.dram("Yd", [KC, 128, TA], BF16, OK)
    MIXd = m.dram("MIXd", [KC, 128, TA], BF16, OK)
    H2d = m.dram("H2d", [KC, 128, TA], BF16, OK)
    GTd = m.dram("GTd", [E, TA], BF16, OK)
    Wb_in = [m.dram("Wb_in%d" % l, [D, 6144], BF16) for l in range(L)]
    Wb_br = [m.dram("Wb_br%d" % l, [D, D], BF16) for l in range(L)]
    Wb_out = [m.dram("Wb_out%d" % l, [D, D], BF16) for l in range(L)]
    Wb_gu, Wb_d = [], []
    for l in range(L):
        if l % 2 == 0:
            Wb_gu.append(m.dram("Wb_gu%d" % l, [1, D, 2 * FD], BF16))
            Wb_d.append(m.dram("Wb_d%d" % l, [1, FD, D], BF16))
        else:
            Wb_gu.append(m.dram("Wb_gu%d" % l, [E, D, 2 * FE], BF16))
            Wb_d.append(m.dram("Wb_d%d" % l, [E, FE, D], BF16))

    ident = m.sb("ident", [128, 128], F32)
    ones_bf = m.sb("ones_bf", [128, 128], BF16)
    epsc = m.sb("epsc", [128, 1], F32)
    m.op("pool", lambda e: e.memset(ident[:], 0.0), writes=[ident])
    m.op("pool", lambda e: e.affine_select(out=ident[:], in_=ident[:], pattern=[[-1, 128]],
                                            compare_op=ALU.not_equal, fill=1.0, base=0,
                                            channel_multiplier=1), reads=[ident], writes=[ident])
    m.op("pool", lambda e: e.memset(ones_bf[:], 1.0), writes=[ones_bf])
    m.op("pool", lambda e: e.memset(epsc[:], EPS), writes=[epsc])
    psall_t = ges.enter_context(nc.psum_tensor("psall", [128, 8, 512], F32))
    psall = psall_t[:]
    PS = [Buf("ps%d" % i, psall[:, i, :]) for i in range(8)]
    modT = m.sb("modT", [128, R, 48], F32)
    A1 = m.sb("A1", [128, R, KC], F32)
    A2 = m.sb("A2", [128, R, KC], F32)
    csil = m.sb("csil", [128, KC, R], F32)
    cst = m.sb("cst", [128, KC, R], F32)
    bmod = m.sb("bmod", [128, 48], F32)
    n1 = m.sb("n1", [128, KC], F32)
    n2 = m.sb("n2", [128, KC], F32)

    state = {"psi": 0}

    def nps():
        p = PS[state["psi"] % 8]
        state["psi"] += 1
        return p

    class Ring:
        def __init__(self, name, n, shape, dt, es=None):
            self.slots = []
            for i in range(n):
                nm = m.uname("%s_%d" % (name, i))
                t = (es or ges).enter_context(nc.sbuf_tensor(nm, list(shape), dt))
                self.slots.append(m.track(Buf(nm, t)))
            self.i = 0

        def next(self):
            s = self.slots[self.i % len(self.slots)]
            self.i += 1
            return s

    def stage_sb(es, name, shape, dt):
        nm = m.uname(name)
        t = es.enter_context(nc.sbuf_tensor(nm, list(shape), dt))
        return m.track(Buf(nm, t))

    nobar = set()

    def barrier():
        for e in MK.ENG:
            for k in list(m.sems.keys()):
                if k == e or k in nobar:
                    continue
                c = m.issued[k]
                if c > m.seen[e].get(k, 0):
                    m.h[e].wait_ge(m.sems[k], c)
                    m.seen[e][k] = c
        m.end_stage()

    cvb = Buf("cvsem")

    def conv_w(dst, dst_ap, src, src_ap):
        m.dma("pool", dst_ap, src_ap, reads=[src], writes=[dst], sembuf=dst)
        nobar.add(dst.dsem)

    conv_w_impl = [conv_w]

    def conv_pop(n):
        for _ in range(n):
            if conv_q:
                conv_w(*conv_q.pop(0))

    def v2(ap, rows):
        return ap.rearrange("(p r) n -> p (r n)", p=128)

    conv_q = []

    def convert_layer(l, defer=False):
        if defer:
            jobs = []
            real = conv_w_impl[0]
            conv_w_impl[0] = lambda *a: jobs.append(a)
            convert_layer(l)
            conv_w_impl[0] = real
            conv_q.extend(jobs)
            return
        cw_ = lambda *a: conv_w_impl[0](*a)
        cw_(Wb_in[l], Wb_in[l][:, 0:5120], w_in, w_in[l])
        cw_(Wb_in[l], Wb_in[l][:, 5120:6144], w_ex, w_ex[l])
        cw_(Wb_br[l], v2(Wb_br[l][:], D), w_br, v2(w_br[l], D))
        cw_(Wb_out[l], v2(Wb_out[l][:], D), w_out, v2(w_out[l], D))
        if l % 2 == 0:
            cw_(Wb_gu[l], v2(Wb_gu[l][0], D), ffn_gu, v2(ffn_gu[l // 2], D))
            cw_(Wb_d[l], v2(Wb_d[l][0], FD), ffn_d, v2(ffn_d[l // 2], FD))
        else:
            for e in range(E):
                cw_(Wb_gu[l], v2(Wb_gu[l][e], D), moe_gu, v2(moe_gu[l // 2, e], D))
                cw_(Wb_d[l], v2(Wb_d[l][e], FE), moe_d, v2(moe_d[l // 2, e], FE))

    def tiles512():
        tl = [(0, CT, R - 1)] if CT <= 512 else [(i * 512, 512, R - 1) for i in range(CT // 512)]
        for b in range(NB):
            for i in range(S // 512):
                tl.append((CT + b * S + i * 512, 512, b))
        return tl

    def fm(dr, t0, ts, k0=0, k1=None):
        k1 = dr.t.shape[0] if k1 is None else k1
        return dr.t[k0:k1, :, t0:t0 + ts].rearrange("k p t -> p k t")

    def stage0():
        with ExitStack() as es:
            rin = Ring("s0in", 2, [128, 4, D], F32, es)
            rout = Ring("s0out", 2, [128, KC, 512], F32, es)
            srcs = [(c_in, i * 512, min(512, CT - i * 512), i * 512) for i in range((CT + 511) // 512)]
            srcs += [(x_in, i * 512, 512, CT + i * 512) for i in range(TX // 512)]
            for (src, r0, ts, t0) in srcs:
                nb = ts // 128
                it = rin.next()
                m.dma("sp", it[:, 0:nb, :], src.t[r0:r0 + ts, :].rearrange("(b p) d -> p b d", p=128),
                      reads=[src], writes=[it], sembuf=it)
                ot = rout.next()
                for blk in range(nb):
                    for kh in range(2):
                        p = nps()
                        for kk in range(4):
                            k = kh * 4 + kk
                            m.op("pe", lambda e, p=p, kk=kk, k=k, blk=blk: e.transpose(
                                p[:, kk * 128:(kk + 1) * 128], it[:, blk, k * 128:(k + 1) * 128], ident[:]),
                                reads=[it, ident], writes=[p])
                        eng = "act" if (blk + kh) % 2 == 0 else "dve"
                        src_v = p[:].rearrange("p (k t) -> p k t", k=4)
                        dst_v = ot[:, kh * 4:(kh + 1) * 4, blk * 128:(blk + 1) * 128]
                        if eng == "act":
                            m.op("act", lambda e, a=dst_v, b=src_v: e.activation(out=a, in_=b, func=AF.Copy),
                                 reads=[p], writes=[ot])
                        else:
                            m.op("dve", lambda e, a=dst_v, b=src_v: e.tensor_copy(out=a, in_=b),
                                 reads=[p], writes=[ot])
                m.dma("act", fm(XR[0], t0, ts), ot[:, :, 0:ts], reads=[ot], writes=[XR[0]], sembuf=ot)
        barrier()

    def ada(l, first):
        with ExitStack() as es:
            rw = Ring("adaw", 2, [128, KC, 512], F32, es)
            if first:
                m.dma("sp", cst[:], cT_in[:], reads=[cT_in], writes=[cst], sembuf=cst)
                m.op("act", lambda e: e.activation(out=csil[:], in_=cst[:], func=AF.Silu),
                     reads=[cst], writes=[csil])
            m.dma("sp", bmod[:], bmodT[l], reads=[bmodT], writes=[bmod], sembuf=bmod)
            m.dma("sp", n1[:], n1T[l], reads=[n1T], writes=[n1], sembuf=n1)
            m.dma("sp", n2[:], n2T[l], reads=[n2T], writes=[n2], sembuf=n2)
            for pc in range(12):
                wt = rw.next()
                m.dma("sp", wt[:], w_mod.t[l, :, pc * 512:(pc + 1) * 512].rearrange("(k p) n -> p k n", p=128),
                      reads=[w_mod], writes=[wt], sembuf=wt)
                p = nps()
                for jj in range(4):
                    for k in range(KC):
                        m.op("pe", lambda e, p=p, jj=jj, k=k: e.matmul(
                            p[:, jj * R:(jj + 1) * R], lhsT=wt[:, k, jj * 128:(jj + 1) * 128], rhs=csil[:, k, :],
                            start=(k == 0), stop=(k == KC - 1)), reads=[wt, csil], writes=[p])
                for jj in range(4):
                    ch = pc * 4 + jj
                    m.op("act", lambda e, p=p, jj=jj, ch=ch: e.activation(
                        out=modT[:, :, ch], in_=p[:, jj * R:(jj + 1) * R], func=AF.Identity,
                        bias=bmod[:, ch:ch + 1], scale=1.0), reads=[p, bmod], writes=[modT])
            for r in range(R):
                m.op("dve", lambda e, r=r: e.scalar_tensor_tensor(
                    out=A1[:, r, :], in0=modT[:, r, 8:16], scalar=1.0, in1=n1[:], op0=ALU.add, op1=ALU.mult),
                    reads=[modT, n1], writes=[A1])
                m.op("dve", lambda e, r=r: e.scalar_tensor_tensor(
                    out=A2[:, r, :], in0=modT[:, r, 32:40], scalar=1.0, in1=n2[:], op0=ALU.add, op1=ALU.mult),
                    reads=[modT, n2], writes=[A2])
        barrier()

    def norm_mod(xt, ts, Aap, Bap, sqb, rt, rstd, tmp, hdst, hf=None):
        m.op("act", lambda e: e.activation(out=sqb[:, :, 0:ts], in_=xt[:, :, 0:ts], func=AF.Square),
             reads=[xt], writes=[sqb])
        p = nps()
        for k in range(KC):
            m.op("pe", lambda e, k=k: e.matmul(p[:, 0:ts], lhsT=ones_bf[:], rhs=sqb[:, k, 0:ts],
                                               start=(k == 0), stop=(k == KC - 1)),
                 reads=[ones_bf, sqb], writes=[p])
        m.op("act", lambda e: e.activation(out=rt[:, 0:ts], in_=p[:, 0:ts], func=AF.Sqrt,
                                           bias=epsc[:], scale=1.0 / D), reads=[p, epsc], writes=[rt])
        m.op("dve", lambda e: e.reciprocal(out=rstd[:, 0:ts], in_=rt[:, 0:ts]), reads=[rt], writes=[rstd])
        for k in range(KC):
            m.op("dve", lambda e, k=k: e.scalar_tensor_tensor(
                out=tmp[:, k, 0:ts], in0=xt[:, k, 0:ts], scalar=Aap(k), in1=rstd[:, 0:ts],
                op0=ALU.mult, op1=ALU.mult), reads=[xt, rstd, A1, A2], writes=[tmp])
            if hf is None:
                m.op("act", lambda e, k=k: e.activation(out=hdst[:, k, 0:ts], in_=tmp[:, k, 0:ts],
                                                        func=AF.Identity, bias=Bap(k), scale=1.0),
                     reads=[tmp, modT], writes=[hdst])
            else:
                m.op("act", lambda e, k=k: e.activation(out=hf[:, k, 0:ts], in_=tmp[:, k, 0:ts],
                                                        func=AF.Identity, bias=Bap(k), scale=1.0),
                     reads=[tmp, modT], writes=[hf])
        if hf is not None:
            m.op("pool", lambda e: e.tensor_copy(out=hdst[:, :, 0:ts], in_=hf[:, :, 0:ts]),
                 reads=[hf], writes=[hdst])

    def stage1(l, xr):
        with ExitStack() as es:
            w1 = stage_sb(es, "w1", [128, KC, 3072], BF16)
            wv = Wb_in[l].t.rearrange("(k p) n -> p k n", p=128)
            m.dma("sp", w1[:, :, 0:2048], wv[:, :, 0:2048], reads=[Wb_in[l]], writes=[w1], sembuf=w1)
            m.dma("sp", w1[:, :, 2048:3072], wv[:, :, 5120:6144], reads=[Wb_in[l]], writes=[w1], sembuf=w1)
            rx = Ring("s1x", 2, [128, KC, 512], F32, es)
            rtab = Ring("s1tab", 2, [128, 2, 512], F32, es)
            sqb = stage_sb(es, "s1sq", [128, KC, 512], BF16)
            rt = stage_sb(es, "s1rt", [128, 512], F32)
            rstd = stage_sb(es, "s1rstd", [128, 512], F32)
            tmp = stage_sb(es, "s1tmp", [128, KC, 512], F32)
            rh = Ring("s1h", 2, [128, KC, 512], BF16, es)
            rfm = Ring("s1fm", 2, [128, 12, 512], BF16, es)
            cg = Ring("s1cg", 2, [128, 512], BF16, es)
            t1r = Ring("s1t1", 2, [128, 512], F32, es)
            t2r = Ring("s1t2", 2, [128, 512], F32, es)
            rvn = Ring("s1vn", 2, [128, 4, 512], BF16, es)
            rva = Ring("s1va", 2, [128, 4, 512], BF16, es)
            vg = Ring("s1vg", 2, [128, 256], F32, es)
            st6 = Ring("s1st", 2, [128, 6], F32, es)
            mv = Ring("s1mv", 2, [128, 2], F32, es)
            sd = Ring("s1sd", 2, [128, 1], F32, es)
            rs = Ring("s1rs", 2, [128, 1], F32, es)
            for s_ in rvn.slots + rva.slots:
                m.op("pool", lambda e, s_=s_: e.memset(s_[:], 0.0), writes=[s_])
            def front(t0, ts, r):
                xt = rx.next()
                m.dma("sp", xt[:, :, 0:ts], fm(xr, t0, ts), reads=[xr], writes=[xt], sembuf=xt)
                tb = rtab.next()
                m.dma("sp", tb[:, 0, 0:ts], tabC[:, t0:t0 + ts], reads=[tabC], writes=[tb], sembuf=tb)
                m.dma("sp", tb[:, 1, 0:ts], tabS[:, t0:t0 + ts], reads=[tabS], writes=[tb], sembuf=tb)
                h = rh.next()
                norm_mod(xt, ts, lambda k: A1[:, r, k:k + 1], lambda k: modT[:, r, k:k + 1],
                         sqb, rt, rstd, tmp, h)
                m.dma("act", fm(H1, t0, ts), h[:, :, 0:ts], reads=[h], writes=[H1], sembuf=h)
                return h, tb

            tls = tiles512()
            nxt = front(*tls[0])
            for ti, (t0, ts, r) in enumerate(tls):
                nb = ts // 128
                h, tb = nxt
                if ti + 1 < len(tls):
                    nxt = front(*tls[ti + 1])
                f = rfm.next()

                def proj(co):
                    p = nps()
                    for k in range(KC):
                        m.op("pe", lambda e, k=k: e.matmul(p[:, 0:ts], lhsT=w1[:, k, co:co + 128],
                                                           rhs=h[:, k, 0:ts], start=(k == 0), stop=(k == KC - 1)),
                             reads=[w1, h], writes=[p])
                    return p
                for j in range(2):
                    p = proj(0 + j * 128)
                    m.op("act", lambda e, p=p, j=j: e.activation(out=f[:, 0 + j, 0:ts], in_=p[:, 0:ts], func=AF.Copy),
                         reads=[p], writes=[f])
                for j in range(2):
                    p = proj(256 + j * 128)
                    c_ = cg.next()
                    m.op("act", lambda e, p=p, c_=c_: e.activation(out=c_[:, 0:ts], in_=p[:, 0:ts], func=AF.Copy),
                         reads=[p], writes=[c_])
                    p2 = proj(512 + j * 128)
                    m.op("dve", lambda e, p2=p2, c_=c_, j=j: e.tensor_tensor(
                        out=f[:, 2 + j, 0:ts], in0=p2[:, 0:ts], in1=c_[:, 0:ts], op=ALU.mult),
                        reads=[p2, c_], writes=[f])
                for j in range(2):
                    p = proj(768 + j * 128)
                    m.op("act", lambda e, p=p, j=j: e.activation(out=f[:, 4 + j, 0:ts], in_=p[:, 0:ts],
                                                                 func=AF.Gelu_apprx_tanh), reads=[p], writes=[f])
                for (co, cop, fo, n) in ((1280, 2048, 6, 4), (2560, 2816, 10, 2)):
                    for j in range(n):
                        p = proj(co + j * 128)
                        pp = proj(cop + j * 128)
                        a1 = t1r.next()
                        a2 = t2r.next()
                        m.op("dve", lambda e, p=p, a1=a1: e.tensor_tensor(
                            out=a1[:, 0:ts], in0=p[:, 0:ts], in1=tb[:, 0, 0:ts], op=ALU.mult),
                            reads=[p, tb], writes=[a1])
                        m.op("dve", lambda e, pp=pp, a2=a2: e.tensor_tensor(
                            out=a2[:, 0:ts], in0=pp[:, 0:ts], in1=tb[:, 1, 0:ts], op=ALU.mult),
                            reads=[pp, tb], writes=[a2])
                        m.op("pool", lambda e, a1=a1, a2=a2, fo=fo, j=j: e.tensor_tensor(
                            out=f[:, fo + j, 0:ts], in0=a1[:, 0:ts], in1=a2[:, 0:ts], op=ALU.add),
                            reads=[a1, a2], writes=[f])
                m.dma("act", fm(BGd, t0, ts), f[:, 0:2, 0:ts], reads=[f], writes=[BGd], sembuf=f)
                m.dma("act", fm(CHd, t0, ts), f[:, 2:4, 0:ts], reads=[f], writes=[CHd], sembuf=f)
                m.dma("act", fm(UGd, t0, ts), f[:, 4:6, 0:ts], reads=[f], writes=[UGd], sembuf=f)
                m.dma("act", fm(Qd, t0, ts), f[:, 6:10, 0:ts], reads=[f], writes=[Qd], sembuf=f)
                m.dma("act", fm(KKd, t0, ts), f[:, 10:12, 0:ts], reads=[f], writes=[KKd], sembuf=f)
                vn = rvn.next()
                va = rva.next()
                for blk in range(nb):
                    pv = nps()
                    for k in range(KC):
                        m.op("pe", lambda e, k=k, pv=pv, blk=blk: e.matmul(
                            pv[:, 0:256], lhsT=h[:, k, blk * 128:(blk + 1) * 128], rhs=w1[:, k, 1024:1280],
                            start=(k == 0), stop=(k == KC - 1)), reads=[w1, h], writes=[pv])
                    pa = nps()
                    for k in range(KC):
                        m.op("pe", lambda e, k=k, pa=pa, blk=blk: e.matmul(
                            pa[:, 0:128], lhsT=h[:, k, blk * 128:(blk + 1) * 128], rhs=w1[:, k, 1920:2048],
                            start=(k == 0), stop=(k == KC - 1)), reads=[w1, h], writes=[pa])
                    g_ = vg.next()
                    m.op("act", lambda e, pv=pv, g_=g_: e.activation(out=g_[:], in_=pv[:, 0:256],
                                                                     func=AF.Gelu_apprx_tanh),
                         reads=[pv], writes=[g_])
                    s6 = st6.next()
                    m.op("dve", lambda e, g_=g_, s6=s6: e.bn_stats(out=s6[:], in_=g_[:]), reads=[g_], writes=[s6])
                    mv_ = mv.next()
                    m.op("dve", lambda e, mv_=mv_, s6=s6: e.bn_aggr(out=mv_[:], in_=s6[:]), reads=[s6], writes=[mv_])
                    sd_ = sd.next()
                    m.op("act", lambda e, sd_=sd_, mv_=mv_: e.activation(out=sd_[:], in_=mv_[:, 1:2], func=AF.Sqrt,
                                                                         bias=epsc[:], scale=1.0),
                         reads=[mv_, epsc], writes=[sd_])
                    rs_ = rs.next()
                    m.op("dve", lambda e, sd_=sd_, rs_=rs_: e.reciprocal(out=rs_[:], in_=sd_[:]),
                         reads=[sd_], writes=[rs_])
                    for par in range(2):
                        src = g_[:].rearrange("p (g c) -> p g c", c=64)[:, par::2, :]
                        dst = vn[:, blk, :].rearrange("p (g c) -> p g c", c=128)[:, par::2, par * 64:(par + 1) * 64]
                        m.op("dve", lambda e, src=src, dst=dst, mv_=mv_, rs_=rs_: e.tensor_scalar(
                            out=dst, in0=src, scalar1=mv_[:, 0:1], scalar2=rs_[:, 0:1],
                            op0=ALU.subtract, op1=ALU.mult), reads=[g_, mv_, rs_], writes=[vn])
                        srca = pa[:, 0:128].rearrange("p (kv c) -> p kv c", c=64)
                        dsta = va[:, blk, :].rearrange("p (kv q c) -> p kv q c", kv=2, q=2)[:, :, par, par * 64:(par + 1) * 64]
                        m.op("act", lambda e, srca=srca, dsta=dsta: e.activation(out=dsta, in_=srca, func=AF.Copy),
                             reads=[pa], writes=[va])
                m.dma("act", VNXd.t[t0:t0 + ts, :].rearrange("(b p) c -> p b c", p=128), vn[:, 0:nb, :],
                      reads=[vn], writes=[VNXd], sembuf=vn)
                m.dma("act", VAXd.t[t0:t0 + ts, :].rearrange("(b p) c -> p b c", p=128), va[:, 0:nb, :],
                      reads=[va], writes=[VAXd], sembuf=va)
        barrier()

    def stage2(l, last):
        with ExitStack() as es:
            cw = stage_sb(es, "s2cw", [128, 2, 3], F32)
            wsf = stage_sb(es, "s2wsf", [128, 4, 128], F32)
            wsb = stage_sb(es, "s2wsb", [128, 4, 128], BF16)
            gbt = stage_sb(es, "s2gb", [128, 2, 128], F32)
            snk = stage_sb(es, "s2snk", [128, NH], F32)
            nsnk = stage_sb(es, "s2nsnk", [128, NH], F32)
            msk = stage_sb(es, "s2msk", [128, 384], F32)
            m.dma("sp", cw[:], convT[l], reads=[convT], writes=[cw], sembuf=cw)
            m.dma("sp", wsf[:], wsT_in[l], reads=[wsT_in], writes=[wsf], sembuf=wsf)
            m.dma("sp", gbt[:], gb_in[l], reads=[gb_in], writes=[gbt], sembuf=gbt)
            m.dma("sp", snk[:], sink_in[l], reads=[sink_in], writes=[snk], sembuf=snk)
            m.dma("sp", msk[:], mask_in[:], reads=[mask_in], writes=[msk], sembuf=msk)
            m.op("dve", lambda e: e.tensor_copy(out=wsb[:], in_=wsf[:]), reads=[wsf], writes=[wsb])
            m.op("dve", lambda e: e.tensor_scalar(out=nsnk[:], in0=snk[:], scalar1=-1.0, scalar2=None,
                                                  op0=ALU.mult), reads=[snk], writes=[nsnk])
            kkc = stage_sb(es, "s2kkc", [128, 2, LC], BF16)
            vaxc = stage_sb(es, "s2vaxc", [128, 2, 512], BF16)
            rch = Ring("s2ch", 2, [128, 2, 514], BF16, es)
            rbg = Ring("s2bg", 2, [128, 2, 512], BF16, es)
            rug = Ring("s2ug", 2, [128, 2, 512], BF16, es)
            rvn = Ring("s2vn", 2, [128, 4, 512], BF16, es)
            rq = Ring("s2q", 2, [128, 4, 512], BF16, es)
            rkk = Ring("s2kk", 2, [128, 2, 768], BF16, es)
            rvx = Ring("s2vx", 2, [128, 6, 512], BF16, es)
            ry = Ring("s2y", 2, [128, KC, 512], BF16, es)
            acc = Ring("s2acc", 2, [128, 512], F32, es)
            gt = Ring("s2gt", 2, [128, 2, 128], F32, es)
            sm4 = Ring("s2sm", 2, [128, 4, 640], F32, es)
            pe4 = Ring("s2pe", 2, [128, 4, 640], F32, es)
            pn4 = Ring("s2pn", 2, [128, 4, 640], F32, es)
            pT4 = Ring("s2pT", 2, [128, 4, 5, 128], BF16, es)
            sc4 = [Ring("s2sc%d" % i, 2, [128, 4], F32, es) for i in range(7)]
            for b in range(NB):
                m.dma("sp", kkc[:], fm(KKd, b * LC, LC), reads=[KKd], writes=[kkc], sembuf=kkc)
                m.dma("sp", vaxc[:], VAXd.t[b * LC:(b + 1) * LC, :].rearrange("(b p) c -> p b c", p=128),
                      reads=[VAXd], writes=[vaxc], sembuf=vaxc)
                tl = []
                if not last:
                    tl.append((b * LC, LC, 0, LC, True))
                for i in range(S // 512):
                    tl.append((CT + b * S + i * 512, 512, i * 512, S, False))
                for (t0, ts, s0, slen, isctx) in tl:
                    nb = ts // 128
                    hl = 1 if s0 > 0 else 0
                    hr = 1 if s0 + ts < slen else 0
                    ch = rch.next()
                    if not hl:
                        m.op("pool", lambda e, ch=ch: e.memset(ch[:, :, 0:1], 0.0), writes=[ch])
                    if not hr:
                        m.op("pool", lambda e, ch=ch: e.memset(ch[:, :, ts + 1:ts + 2], 0.0), writes=[ch])
                    m.dma("sp", ch[:, :, 1 - hl:ts + 1 + hr], fm(CHd, t0 - hl, ts + hl + hr),
                          reads=[CHd], writes=[ch], sembuf=ch)
                    bg = rbg.next()
                    m.dma("sp", bg[:, :, 0:ts], fm(BGd, t0, ts), reads=[BGd], writes=[bg], sembuf=bg)
                    ug = rug.next()
                    m.dma("sp", ug[:, :, 0:ts], fm(UGd, t0, ts), reads=[UGd], writes=[ug], sembuf=ug)
                    vn = rvn.next()
                    m.dma("sp", vn[:, 0:nb, :], VNXd.t[t0:t0 + ts, :].rearrange("(b p) c -> p b c", p=128),
                          reads=[VNXd], writes=[vn], sembuf=vn)
                    q = rq.next()
                    m.dma("sp", q[:, :, 0:ts], fm(Qd, t0, ts), reads=[Qd], writes=[q], sembuf=q)
                    kk = vx = None
                    if not isctx:
                        kl = 128 if s0 > 0 else 0
                        kr = 128 if s0 + ts < slen else 0
                        kk = rkk.next()
                        m.dma("sp", kk[:, :, 128 - kl:128 + ts + kr], fm(KKd, t0 - kl, ts + kl + kr),
                              reads=[KKd], writes=[kk], sembuf=kk)
                        vx = rvx.next()
                        nbl = (kl + ts + kr) // 128
                        b0 = 1 - kl // 128
                        m.dma("sp", vx[:, b0:b0 + nbl, :],
                              VAXd.t[t0 - kl:t0 + ts + kr, :].rearrange("(b p) c -> p b c", p=128),
                              reads=[VAXd], writes=[vx], sembuf=vx)
                    y = ry.next()
                    for j in range(2):
                        a = acc.next()
                        m.op("dve", lambda e, a=a, j=j: e.tensor_scalar(
                            out=a[:, 0:ts], in0=ch[:, j, 1:ts + 1], scalar1=cw[:, j, 1:2], scalar2=None,
                            op0=ALU.mult), reads=[ch, cw], writes=[a])
                        m.op("dve", lambda e, a=a, j=j: e.scalar_tensor_tensor(
                            out=a[:, 0:ts], in0=ch[:, j, 0:ts], scalar=cw[:, j, 0:1], in1=a[:, 0:ts],
                            op0=ALU.mult, op1=ALU.add), reads=[ch, cw, a], writes=[a])
                        m.op("dve", lambda e, a=a, j=j: e.scalar_tensor_tensor(
                            out=a[:, 0:ts], in0=ch[:, j, 2:ts + 2], scalar=cw[:, j, 2:3], in1=a[:, 0:ts],
                            op0=ALU.mult, op1=ALU.add), reads=[ch, cw, a], writes=[a])
                        m.op("pool", lambda e, a=a, j=j: e.tensor_tensor(
                            out=y[:, j, 0:ts], in0=a[:, 0:ts], in1=bg[:, j, 0:ts], op=ALU.mult),
                            reads=[a, bg], writes=[y])
                    for blk in range(nb):
                        p = nps()
                        for j in range(2):
                            for gg in range(2):
                                g = 2 * j + gg
                                m.op("pe", lambda e, p=p, j=j, g=g, gg=gg, blk=blk: e.matmul(
                                    p[:, j * 128:(j + 1) * 128], lhsT=vn[:, blk, g * 128:(g + 1) * 128],
                                    rhs=wsb[:, g, :], start=(gg == 0), stop=(gg == 1)),
                                    reads=[vn, wsb], writes=[p])
                        g_ = gt.next()
                        m.op("dve", lambda e, p=p, g_=g_: e.tensor_tensor(
                            out=g_[:], in0=p[:, 0:256].rearrange("p (j t) -> p j t", j=2), in1=gbt[:],
                            op=ALU.add), reads=[p, gbt], writes=[g_])
                        m.op("pool", lambda e, g_=g_, blk=blk: e.tensor_tensor(
                            out=y[:, 2:4, blk * 128:(blk + 1) * 128], in0=g_[:],
                            in1=ug[:, :, blk * 128:(blk + 1) * 128], op=ALU.mult),
                            reads=[g_, ug], writes=[y])
                    for blk in range(nb):
                        nbk = (s0 // 128) + blk
                        if isctx:
                            lo = hi = 384
                        else:
                            lo = 128 if nbk == 0 else 0
                            hi = 256 if nbk == slen // 128 - 1 else 384
                        kbs = list(range(lo // 128, hi // 128))
                        for kv in range(2):
                            h0 = kv * 4
                            A = PS[0:4]
                            Bk = PS[4:6]
                            O = PS[6:8]
                            s4 = sm4.next()
                            if hi == lo:
                                m.op("pool", lambda e: e.memset(s4[:, :, 0:384], NEG), writes=[s4])
                            else:
                                if lo > 0:
                                    m.op("pool", lambda e: e.memset(s4[:, :, 0:lo], NEG), writes=[s4])
                                if hi < 384:
                                    m.op("pool", lambda e: e.memset(s4[:, :, hi:384], NEG), writes=[s4])
                            for hq in range(4):
                                hh = h0 + hq
                                c = hh // 2
                                pb = (hh % 2) * 64
                                if hi > lo:
                                    m.op("pe", lambda e: e.matmul(
                                        A[hq][:, lo:hi], lhsT=q[pb:pb + 64, c, blk * 128:(blk + 1) * 128],
                                        rhs=kk[pb:pb + 64, kv, blk * 128 + lo:blk * 128 + hi], start=True, stop=True),
                                        reads=[q, kk], writes=[A[hq]])
                                m.op("pe", lambda e: e.matmul(
                                    Bk[hq % 2][:, (hq // 2) * 256:(hq // 2) * 256 + 256],
                                    lhsT=q[pb:pb + 64, c, blk * 128:(blk + 1) * 128],
                                    rhs=kkc[pb:pb + 64, kv, :], start=True, stop=True),
                                    reads=[q, kkc], writes=[Bk[hq % 2]])
                            if hi > lo:
                                m.op("dve", lambda e: e.tensor_tensor(
                                    out=s4[:, :, lo:hi], in0=psall[:, 0:4, lo:hi],
                                    in1=msk[:, lo:hi].unsqueeze(1).to_broadcast([128, 4, hi - lo]), op=ALU.add),
                                    reads=A + [msk], writes=[s4])
                            m.op("act", lambda e: e.activation(
                                out=s4[:, :, 384:640].rearrange("p (h b) c -> p h b c", b=2),
                                in_=psall[:, 4:6, :].rearrange("p b (h c) -> p h b c", h=2),
                                func=AF.Copy), reads=Bk, writes=[s4])
                            mx = sc4[0].next()
                            m.op("dve", lambda e: e.tensor_reduce(out=mx[:], in_=s4[:], axis=AX.X, op=ALU.max),
                                 reads=[s4], writes=[mx])
                            ngm = sc4[1].next()
                            m.op("dve", lambda e: e.scalar_tensor_tensor(
                                out=ngm[:], in0=mx[:], scalar=-SCALE, in1=nsnk[:, h0:h0 + 4], op0=ALU.mult, op1=ALU.min),
                                reads=[mx, nsnk], writes=[ngm])
                            p4 = pe4.next()
                            for hq in range(4):
                                m.op("act", lambda e: e.activation(
                                    out=p4[:, hq, :], in_=s4[:, hq, :], func=AF.Exp, bias=ngm[:, hq:hq + 1], scale=SCALE),
                                    reads=[s4, ngm], writes=[p4])
                            tt = sc4[2].next()
                            m.op("dve", lambda e: e.tensor_tensor(out=tt[:], in0=snk[:, h0:h0 + 4], in1=ngm[:], op=ALU.add),
                                 reads=[snk, ngm], writes=[tt])
                            es_ = sc4[3].next()
                            m.op("act", lambda e: e.activation(out=es_[:], in_=tt[:], func=AF.Exp), reads=[tt], writes=[es_])
                            rsum = sc4[4].next()
                            m.op("dve", lambda e: e.tensor_reduce(out=rsum[:], in_=p4[:], axis=AX.X, op=ALU.add),
                                 reads=[p4], writes=[rsum])
                            den = sc4[5].next()
                            m.op("dve", lambda e: e.tensor_tensor(out=den[:], in0=rsum[:], in1=es_[:], op=ALU.add),
                                 reads=[rsum, es_], writes=[den])
                            inv = sc4[6].next()
                            m.op("dve", lambda e: e.reciprocal(out=inv[:], in_=den[:]), reads=[den], writes=[inv])
                            n4 = pn4.next()
                            m.op("dve", lambda e: e.tensor_tensor(
                                out=n4[:], in0=p4[:], in1=inv[:].unsqueeze(2).to_broadcast([128, 4, 640]), op=ALU.mult),
                                reads=[p4, inv], writes=[n4])
                            for hq in range(4):
                                for kb in kbs:
                                    m.op("pe", lambda e: e.transpose(
                                        A[hq][:, kb * 128:(kb + 1) * 128], n4[:, hq, kb * 128:(kb + 1) * 128], ident[:]),
                                        reads=[n4, ident], writes=[A[hq]])
                                for cb in range(2):
                                    o_ = (hq // 2) * 256 + cb * 128
                                    m.op("pe", lambda e: e.transpose(
                                        Bk[hq % 2][:, o_:o_ + 128], n4[:, hq, 384 + cb * 128:384 + (cb + 1) * 128], ident[:]),
                                        reads=[n4, ident], writes=[Bk[hq % 2]])
                            pt = pT4.next()
                            if kbs:
                                k0, k1 = kbs[0], kbs[-1] + 1
                                m.op("dve", lambda e: e.tensor_copy(
                                    out=pt[:, :, k0:k1, :],
                                    in_=psall[:, 0:4, k0 * 128:k1 * 128].rearrange("p h (k t) -> p h k t", t=128)),
                                    reads=A, writes=[pt])
                            for h2_ in range(2):
                                m.op("act", lambda e: e.activation(
                                    out=pt[:, 2 * h2_:2 * h2_ + 2, 3:5, :],
                                    in_=psall[:, 4:6, h2_ * 256:(h2_ + 1) * 256].rearrange("p b (k t) -> p b k t", k=2),
                                    func=AF.Copy), reads=Bk, writes=[pt])
                            for cp in range(2):
                                pO = O[cp]
                                first = True
                                for par in range(2):
                                    hq = 2 * cp + par
                                    seq = [(vx, blk + kb, kb) for kb in kbs] + [(vaxc, cb, 3 + cb) for cb in range(2)]
                                    for i_, (vb, vi, pi) in enumerate(seq):
                                        lastmm = (par == 1 and i_ == len(seq) - 1)
                                        m.op("pe", lambda e: e.matmul(
                                            pO[:, 0:128], lhsT=vb[:, vi, kv * 256 + par * 128:kv * 256 + (par + 1) * 128],
                                            rhs=pt[:, hq, pi, :], start=first, stop=lastmm),
                                            reads=[vb, pt], writes=[pO])
                                        first = False
                            m.op("act", lambda e: e.activation(
                                out=y[:, 4 + kv * 2:6 + kv * 2, blk * 128:(blk + 1) * 128], in_=psall[:, 6:8, 0:128],
                                func=AF.Copy), reads=O, writes=[y])
                    m.dma("act", fm(Yd, t0, ts), y[:, :, 0:ts], reads=[y], writes=[Yd], sembuf=y)
        barrier()

    def stage3a(l, last):
        with ExitStack() as es:
            wg = stage_sb(es, "s3wg", [128, KC, 3072], BF16)
            wbr = stage_sb(es, "s3wbr", [128, KC, D], BF16)
            wv = Wb_in[l].t.rearrange("(k p) n -> p k n", p=128)
            m.dma("sp", wg[:], wv[:, :, 2048:5120], reads=[Wb_in[l]], writes=[wg], sembuf=wg)
            m.dma("sp", wbr[:], Wb_br[l].t.rearrange("(k p) n -> p k n", p=128), reads=[Wb_br[l]], writes=[wbr], sembuf=wbr)
            rh = Ring("s3h", 2, [128, KC, 512], BF16, es)
            ry = Ring("s3y", 2, [128, KC, 512], BF16, es)
            rmix = Ring("s3mix", 2, [128, KC, 512], BF16, es)
            sg = Ring("s3sg", 3, [128, 512], F32, es)
            tmp = Ring("s3tmp", 3, [128, 512], F32, es)
            mixf = Ring("s3mixf", 2, [128, 512], F32, es)
            brk = ((0, 2), (2, 4), (4, 8))
            for (t0, ts, r) in tiles512():
                if last and r == R - 1:
                    continue
                h = rh.next()
                m.dma("sp", h[:, :, 0:ts], fm(H1, t0, ts), reads=[H1], writes=[h], sembuf=h)
                y = ry.next()
                m.dma("sp", y[:, :, 0:ts], fm(Yd, t0, ts), reads=[Yd], writes=[y], sembuf=y)
                mix = rmix.next()
                for j in range(KC):
                    mf = mixf.next()
                    for bi, (ka, kb) in enumerate(brk):
                        pg = nps()
                        for k in range(KC):
                            m.op("pe", lambda e, k=k, pg=pg, bi=bi, j=j: e.matmul(
                                pg[:, 0:ts], lhsT=wg[:, k, bi * 1024 + j * 128:bi * 1024 + (j + 1) * 128],
                                rhs=h[:, k, 0:ts], start=(k == 0), stop=(k == KC - 1)), reads=[wg, h], writes=[pg])
                        pp = nps()
                        for k in range(ka, kb):
                            m.op("pe", lambda e, k=k, pp=pp, j=j, ka=ka, kb=kb: e.matmul(
                                pp[:, 0:ts], lhsT=wbr[:, k, j * 128:(j + 1) * 128], rhs=y[:, k, 0:ts],
                                start=(k == ka), stop=(k == kb - 1)), reads=[wbr, y], writes=[pp])
                        s_ = sg.next()
                        m.op("act", lambda e, s_=s_, pg=pg: e.activation(out=s_[:, 0:ts], in_=pg[:, 0:ts],
                                                                         func=AF.Sigmoid), reads=[pg], writes=[s_])
                        if bi == 0:
                            m.op("dve", lambda e, mf=mf, pp=pp, s_=s_: e.tensor_tensor(
                                out=mf[:, 0:ts], in0=pp[:, 0:ts], in1=s_[:, 0:ts], op=ALU.mult),
                                reads=[pp, s_], writes=[mf])
                        else:
                            t_ = tmp.next()
                            m.op("dve", lambda e, t_=t_, pp=pp, s_=s_: e.tensor_tensor(
                                out=t_[:, 0:ts], in0=pp[:, 0:ts], in1=s_[:, 0:ts], op=ALU.mult),
                                reads=[pp, s_], writes=[t_])
                            if bi == 1:
                                m.op("pool", lambda e, mf=mf, t_=t_: e.tensor_tensor(
                                    out=mf[:, 0:ts], in0=mf[:, 0:ts], in1=t_[:, 0:ts], op=ALU.add),
                                    reads=[mf, t_], writes=[mf])
                            else:
                                m.op("pool", lambda e, mf=mf, t_=t_, j=j: e.tensor_tensor(
                                    out=mix[:, j, 0:ts], in0=mf[:, 0:ts], in1=t_[:, 0:ts], op=ALU.add),
                                    reads=[mf, t_], writes=[mix])
                m.dma("act", fm(MIXd, t0, ts), mix[:, :, 0:ts], reads=[mix], writes=[MIXd], sembuf=mix)
        barrier()

    def stage3b(l, last, xr, xw):
        moe = (l % 2 == 1)
        with ExitStack() as es:
            wo = stage_sb(es, "s3wo", [128, KC, D], BF16)
            m.dma("sp", wo[:], Wb_out[l].t.rearrange("(k p) n -> p k n", p=128), reads=[Wb_out[l]], writes=[wo], sembuf=wo)
            rtw = stage_sb(es, "s3rt", [128, KC, E], F32)
            if moe:
                m.dma("sp", rtw[:], moe_rt.t[l // 2].rearrange("(k p) e -> p k e", p=128),
                      reads=[moe_rt], writes=[rtw], sembuf=rtw)
            rmix = Ring("s3bmix", 2, [128, KC, 512], BF16, es)
            rx = Ring("s3bx", 2, [128, KC, 512], F32, es)
            sqb = stage_sb(es, "s3bsq", [128, KC, 512], BF16)
            rt = stage_sb(es, "s3brt", [128, 512], F32)
            rstd = stage_sb(es, "s3brstd", [128, 512], F32)
            tmp = stage_sb(es, "s3btmp", [128, KC, 512], F32)
            hf = stage_sb(es, "s3bhf", [128, KC, 512], F32)
            rh2 = Ring("s3bh2", 2, [128, KC, 512], BF16, es)
            rgt = Ring("s3bgt", 2, [E, 512], BF16, es)
            sm8 = [Ring("s3bs%d" % i, 2, [128, E], F32, es) for i in range(5)]
            sc1 = [Ring("s3bc%d" % i, 2, [128, 1], F32, es) for i in range(7)]
            for (t0, ts, r) in tiles512():
                if last and r == R - 1:
                    continue
                nb = ts // 128
                mix = rmix.next()
                m.dma("sp", mix[:, :, 0:ts], fm(MIXd, t0, ts), reads=[MIXd], writes=[mix], sembuf=mix)
                xt = rx.next()
                m.dma("sp", xt[:, :, 0:ts], fm(xr, t0, ts), reads=[xr], writes=[xt], sembuf=xt)
                for j in range(KC):
                    p = nps()
                    for k in range(KC):
                        m.op("pe", lambda e, k=k, p=p, j=j: e.matmul(
                            p[:, 0:ts], lhsT=wo[:, k, j * 128:(j + 1) * 128], rhs=mix[:, k, 0:ts],
                            start=(k == 0), stop=(k == KC - 1)), reads=[wo, mix], writes=[p])
                    m.op("dve", lambda e, p=p, j=j: e.scalar_tensor_tensor(
                        out=xt[:, j, 0:ts], in0=p[:, 0:ts], scalar=modT[:, r, 16 + j:17 + j], in1=xt[:, j, 0:ts],
                        op0=ALU.mult, op1=ALU.add), reads=[p, modT, xt], writes=[xt])
                m.dma("act", fm(xw, t0, ts), xt[:, :, 0:ts], reads=[xt], writes=[xw], sembuf=xt)
                h2 = rh2.next()
                norm_mod(xt, ts, lambda k: A2[:, r, k:k + 1], lambda k: modT[:, r, 24 + k:25 + k],
                         sqb, rt, rstd, tmp, h2, hf=hf if moe else None)
                m.dma("act", fm(H2d, t0, ts), h2[:, :, 0:ts], reads=[h2], writes=[H2d], sembuf=h2)
                if moe:
                    gtt = rgt.next()
                    for blk in range(nb):
                        p = nps()
                        for k in range(KC):
                            m.op("pe", lambda e, k=k, p=p, blk=blk: e.matmul(
                                p[:, 0:E], lhsT=hf[:, k, blk * 128:(blk + 1) * 128], rhs=rtw[:, k, :],
                                start=(k == 0), stop=(k == KC - 1)), reads=[hf, rtw], writes=[p])
                        lg = sm8[0].next()
                        m.op("act", lambda e, lg=lg, p=p: e.activation(out=lg[:], in_=p[:, 0:E], func=AF.Copy),
                             reads=[p], writes=[lg])
                        m1 = sc1[0].next()
                        m.op("dve", lambda e, m1=m1, lg=lg: e.tensor_reduce(out=m1[:], in_=lg[:], axis=AX.X, op=ALU.max),
                             reads=[lg], writes=[m1])
                        eq1 = sm8[1].next()
                        m.op("dve", lambda e, eq1=eq1, lg=lg, m1=m1: e.tensor_scalar(
                            out=eq1[:], in0=lg[:], scalar1=m1[:, 0:1], scalar2=None, op0=ALU.is_equal),
                            reads=[lg, m1], writes=[eq1])
                        msk2 = sm8[2].next()
                        m.op("dve", lambda e, msk2=msk2, eq1=eq1, lg=lg: e.scalar_tensor_tensor(
                            out=msk2[:], in0=eq1[:], scalar=NEG, in1=lg[:], op0=ALU.mult, op1=ALU.add),
                            reads=[eq1, lg], writes=[msk2])
                        m2 = sc1[1].next()
                        m.op("dve", lambda e, m2=m2, msk2=msk2: e.tensor_reduce(out=m2[:], in_=msk2[:], axis=AX.X, op=ALU.max),
                             reads=[msk2], writes=[m2])
                        eq2 = sm8[3].next()
                        m.op("dve", lambda e, eq2=eq2, msk2=msk2, m2=m2: e.tensor_scalar(
                            out=eq2[:], in0=msk2[:], scalar1=m2[:, 0:1], scalar2=None, op0=ALU.is_equal),
                            reads=[msk2, m2], writes=[eq2])
                        dd = sc1[2].next()
                        m.op("dve", lambda e, dd=dd, m2=m2, m1=m1: e.tensor_tensor(out=dd[:], in0=m2[:], in1=m1[:], op=ALU.subtract),
                             reads=[m1, m2], writes=[dd])
                        ee = sc1[3].next()
                        m.op("act", lambda e, ee=ee, dd=dd: e.activation(out=ee[:], in_=dd[:], func=AF.Exp),
                             reads=[dd], writes=[ee])
                        dn = sc1[4].next()
                        m.op("dve", lambda e, dn=dn, ee=ee: e.tensor_scalar(out=dn[:], in0=ee[:], scalar1=1.0, scalar2=None, op0=ALU.add),
                             reads=[ee], writes=[dn])
                        w1_ = sc1[5].next()
                        m.op("dve", lambda e, w1_=w1_, dn=dn: e.reciprocal(out=w1_[:], in_=dn[:]), reads=[dn], writes=[w1_])
                        w2_ = sc1[6].next()
                        m.op("dve", lambda e, w2_=w2_, w1_=w1_, ee=ee: e.tensor_tensor(out=w2_[:], in0=w1_[:], in1=ee[:], op=ALU.mult),
                             reads=[w1_, ee], writes=[w2_])
                        ga = sm8[4].next()
                        m.op("dve", lambda e, ga=ga, eq1=eq1, w1_=w1_: e.tensor_scalar(
                            out=ga[:], in0=eq1[:], scalar1=w1_[:, 0:1], scalar2=None, op0=ALU.mult),
                            reads=[eq1, w1_], writes=[ga])
                        m.op("dve", lambda e, ga=ga, eq2=eq2, w2_=w2_: e.scalar_tensor_tensor(
                            out=ga[:], in0=eq2[:], scalar=w2_[:, 0:1], in1=ga[:], op0=ALU.mult, op1=ALU.add),
                            reads=[eq2, w2_, ga], writes=[ga])
                        p2 = nps()
                        m.op("pe", lambda e, p2=p2, ga=ga: e.transpose(p2[0:E, 0:128], ga[:], ident[:]),
                             reads=[ga, ident], writes=[p2])
                        m.op("act", lambda e, p2=p2, gtt=gtt, blk=blk: e.activation(
                            out=gtt[:, blk * 128:(blk + 1) * 128], in_=p2[0:E, 0:128], func=AF.Copy),
                            reads=[p2], writes=[gtt])
                    m.dma("act", GTd.t[:, t0:t0 + ts], gtt[:, 0:ts], reads=[gtt], writes=[GTd], sembuf=gtt)
        barrier()

    def stage4(l, last, xr, xw):
        moe = (l % 2 == 1)
        NE = E if moe else 1
        F = FE if moe else FD
        GF = 4
        GW = GF * 128
        groups = []
        c0_ = 0
        while c0_ < F // 128:
            groups.append((c0_, min(GF, F // 128 - c0_)))
            c0_ += groups[-1][1]
        TT = 1024
        tl = []
        if not last:
            for i in range((CT + TT - 1) // TT):
                tl.append((i * TT, min(TT, CT - i * TT), R - 1))
        for b in range(NB):
            for i in range(S // TT):
                tl.append((CT + b * S + i * TT, TT, b))
        with ExitStack() as es:
            rh2 = Ring("s4h", 2, [128, KC, TT], BF16, es)
            yacc = stage_sb(es, "s4yacc", [128, KC, TT], F32)
            rgbc = Ring("s4gbc", 2, [128, E, TT], BF16, es) if moe else None
            ract = Ring("s4act", 3, [128, GF, 512], BF16, es)
            rs = Ring("s4s", 3, [128, 512], BF16, es)
            rt_ = Ring("s4t", 3, [128, 512], BF16, es)
            rxc = Ring("s4xc", 2, [128, TT], F32, es)
            rwgu = Ring("s4wgu", 3, [128, KC, 2 * GW], BF16, es)
            rwd = Ring("s4wd", 3, [128, GF, D], BF16, es)
            guv = [Wb_gu[l].t[e].rearrange("(k p) n -> p k n", p=128) for e in range(NE)]
            dv = [Wb_d[l].t[e] for e in range(NE)]
            gring = [0]
            per_tile = (len(conv_q) + len(tl) - 1) // max(len(tl), 1)
            def load_tile(ti_):
                t0_, ts_, _r = tl[ti_]
                h2_ = rh2.next()
                m.dma("act", h2_[:, :, 0:ts_], fm(H2d, t0_, ts_), reads=[H2d], writes=[h2_], sembuf=h2_)
                g_ = None
                if moe:
                    g_ = rgbc.next()
                    for e_ in range(E):
                        m.dma("act", g_[:, e_, 0:ts_], GTd.t[e_:e_ + 1, t0_:t0_ + ts_].partition_broadcast(128),
                              reads=[GTd], writes=[g_], sembuf=g_)
                return h2_, g_

            nxt = load_tile(0)
            for ti, (t0, ts, r) in enumerate(tl):
                conv_pop(per_tile)
                h2, gbc = nxt
                if ti + 1 < len(tl):
                    nxt = load_tile(ti + 1)
                halves = [(o, min(512, ts - o)) for o in range(0, ts, 512)]
                pend = None
                first_unit = [True]

                def down(act_hs, wd, fu, gf):
                    for (ho, hs), act in act_hs:
                        for j in range(KC):
                            py = PS[4 + (gring[0] % 4)]
                            gring[0] += 1
                            for f_ in range(gf):
                                m.op("pe", lambda e, py=py, f_=f_, j=j, act=act, hs=hs: e.matmul(
                                    py[:, 0:hs], lhsT=wd[:, f_, j * 128:(j + 1) * 128], rhs=act[:, f_, 0:hs],
                                    start=(f_ == 0), stop=(f_ == gf - 1)), reads=[wd, act], writes=[py])
                            if fu:
                                m.op("act", lambda e, py=py, j=j, ho=ho, hs=hs: e.activation(
                                    out=yacc[:, j, ho:ho + hs], in_=py[:, 0:hs], func=AF.Copy),
                                    reads=[py], writes=[yacc])
                            else:
                                m.op("dve", lambda e, py=py, j=j, ho=ho, hs=hs: e.tensor_tensor(
                                    out=yacc[:, j, ho:ho + hs], in0=py[:, 0:hs], in1=yacc[:, j, ho:ho + hs], op=ALU.add),
                                    reads=[py, yacc], writes=[yacc])

                ui = 0
                for e_ in range(NE):
                    for (c0, gf) in groups:
                        gw = gf * 128
                        wgu = rwgu.next()
                        m.dma("sp", wgu[:, :, 0:gw], guv[e_][:, :, c0 * 128:c0 * 128 + gw],
                              reads=[Wb_gu[l]], writes=[wgu], sembuf=wgu)
                        m.dma("sp", wgu[:, :, GW:GW + gw], guv[e_][:, :, F + c0 * 128:F + c0 * 128 + gw],
                              reads=[Wb_gu[l]], writes=[wgu], sembuf=wgu)
                        wd = rwd.next()
                        m.dma("sp", wd[:, 0:gf, :], dv[e_][c0 * 128:c0 * 128 + gw, :].rearrange("(f p) d -> p f d", p=128),
                              reads=[Wb_d[l]], writes=[wd], sembuf=wd)
                        act_hs = []
                        for (ho, hs) in halves:
                            act = ract.next()
                            for f_ in range(gf):
                                pg = PS[(ui % 2)]
                                pu = PS[2 + (ui % 2)]
                                ui += 1
                                for k in range(KC):
                                    m.op("pe", lambda e, k=k, pg=pg, f_=f_, ho=ho, hs=hs: e.matmul(
                                        pg[:, 0:hs], lhsT=wgu[:, k, f_ * 128:(f_ + 1) * 128], rhs=h2[:, k, ho:ho + hs],
                                        start=(k == 0), stop=(k == KC - 1)), reads=[wgu, h2], writes=[pg])
                                for k in range(KC):
                                    m.op("pe", lambda e, k=k, pu=pu, f_=f_, ho=ho, hs=hs: e.matmul(
                                        pu[:, 0:hs], lhsT=wgu[:, k, GW + f_ * 128:GW + (f_ + 1) * 128],
                                        rhs=h2[:, k, ho:ho + hs], start=(k == 0), stop=(k == KC - 1)),
                                        reads=[wgu, h2], writes=[pu])
                                s_ = rs.next()
                                m.op("act", lambda e, s_=s_, pg=pg, hs=hs: e.activation(
                                    out=s_[:, 0:hs], in_=pg[:, 0:hs], func=AF.Silu), reads=[pg], writes=[s_])
                                if moe:
                                    t_ = rt_.next()
                                    m.op("dve", lambda e, t_=t_, pu=pu, s_=s_, hs=hs: e.tensor_tensor(
                                        out=t_[:, 0:hs], in0=pu[:, 0:hs], in1=s_[:, 0:hs], op=ALU.mult),
                                        reads=[pu, s_], writes=[t_])
                                    m.op("pool", lambda e, t_=t_, act=act, f_=f_, e_=e_, ho=ho, hs=hs: e.tensor_tensor(
                                        out=act[:, f_, 0:hs], in0=t_[:, 0:hs], in1=gbc[:, e_, ho:ho + hs], op=ALU.mult),
                                        reads=[t_, gbc], writes=[act])
                                else:
                                    m.op("dve", lambda e, act=act, pu=pu, s_=s_, f_=f_, hs=hs: e.tensor_tensor(
                                        out=act[:, f_, 0:hs], in0=pu[:, 0:hs], in1=s_[:, 0:hs], op=ALU.mult),
                                        reads=[pu, s_], writes=[act])
                            act_hs.append(((ho, hs), act))
                            if pend is not None and (ho, hs) == halves[0]:
                                down(*pend)
                                pend = None
                        pend = (act_hs, wd, first_unit[0], gf)
                        first_unit[0] = False
                down(*pend)
                for j in range(KC):
                    xc = rxc.next()
                    m.dma("act", xc[:, 0:ts], xr.t[j, :, t0:t0 + ts], reads=[xr], writes=[xc], sembuf=xc)
                    m.op("dve", lambda e, xc=xc, j=j: e.scalar_tensor_tensor(
                        out=xc[:, 0:ts], in0=yacc[:, j, 0:ts], scalar=modT[:, r, 40 + j:41 + j], in1=xc[:, 0:ts],
                        op0=ALU.mult, op1=ALU.add), reads=[yacc, modT, xc], writes=[xc])
                    m.dma("act", xw.t[j, :, t0:t0 + ts], xc[:, 0:ts], reads=[xc], writes=[xw], sembuf=xc)
        barrier()

    def stage_final(xr):
        with ExitStack() as es:
            fn = stage_sb(es, "fn", [128, KC], F32)
            m.dma("sp", fn[:], fnT[:], reads=[fnT], writes=[fn], sembuf=fn)
            rx = Ring("sfx", 2, [128, KC, 512], F32, es)
            sqb = stage_sb(es, "sfsq", [128, KC, 512], BF16)
            rt = stage_sb(es, "sfrt", [128, 512], F32)
            rstd = stage_sb(es, "sfrstd", [128, 512], F32)
            yt = stage_sb(es, "sfy", [128, KC, 512], F32)
            ro = Ring("sfo", 2, [128, 4, D], F32, es)
            for i in range(TX // 512):
                t0 = CT + i * 512
                ts = 512
                xt = rx.next()
                m.dma("sp", xt[:], fm(xr, t0, ts), reads=[xr], writes=[xt], sembuf=xt)
                m.op("act", lambda e: e.activation(out=sqb[:], in_=xt[:], func=AF.Square), reads=[xt], writes=[sqb])
                p = nps()
                for k in range(KC):
                    m.op("pe", lambda e, k=k: e.matmul(p[:], lhsT=ones_bf[:], rhs=sqb[:, k, :], start=(k == 0),
                                                       stop=(k == KC - 1)), reads=[ones_bf, sqb], writes=[p])
                m.op("act", lambda e: e.activation(out=rt[:], in_=p[:], func=AF.Sqrt, bias=epsc[:], scale=1.0 / D),
                     reads=[p, epsc], writes=[rt])
                m.op("dve", lambda e: e.reciprocal(out=rstd[:], in_=rt[:]), reads=[rt], writes=[rstd])
                for k in range(KC):
                    m.op("dve", lambda e, k=k: e.scalar_tensor_tensor(
                        out=yt[:, k, :], in0=xt[:, k, :], scalar=fn[:, k:k + 1], in1=rstd[:],
                        op0=ALU.mult, op1=ALU.mult), reads=[xt, fn, rstd], writes=[yt])
                ot = ro.next()
                for blk in range(4):
                    for kh in range(2):
                        pp = nps()
                        for kk in range(4):
                            k = kh * 4 + kk
                            m.op("pe", lambda e, pp=pp, kk=kk, k=k, blk=blk: e.transpose(
                                pp[:, kk * 128:(kk + 1) * 128], yt[:, k, blk * 128:(blk + 1) * 128], ident[:]),
                                reads=[yt, ident], writes=[pp])
                        if (blk + kh) % 2 == 0:
                            m.op("act", lambda e, pp=pp, blk=blk, kh=kh: e.activation(
                                out=ot[:, blk, kh * 512:(kh + 1) * 512], in_=pp[:], func=AF.Copy),
                                reads=[pp], writes=[ot])
                        else:
                            m.op("dve", lambda e, pp=pp, blk=blk, kh=kh: e.tensor_copy(
                                out=ot[:, blk, kh * 512:(kh + 1) * 512], in_=pp[:]), reads=[pp], writes=[ot])
                m.dma("act", out.t[i * 512:(i + 1) * 512, :].rearrange("(b p) d -> p b d", p=128), ot[:],
                      reads=[ot], writes=[out], sembuf=ot)
        barrier()

    convert_layer(0)
    stage0()
    cur = 0
    stop = cfg.stop
    done = False
    for l in range(L):
        last = (l == L - 1)
        ada(l, l == 0)
        stage1(l, XR[cur])
        if stop == (l, 1): done = True; break
        stage2(l, last)
        if stop == (l, 2): done = True; break
        if l + 1 < L:
            convert_layer(l + 1, defer=True)
        stage3a(l, last)
        stage3b(l, last, XR[cur], XR[1 - cur])
        cur = 1 - cur
        if stop == (l, 3): done = True; break
        stage4(l, last, XR[cur], XR[1 - cur])
        conv_pop(len(conv_q))
        cur = 1 - cur
        if stop == (l, 4): done = True; break
    if not done:
        stage_final(XR[cur])
    nobar.clear()
    barrier()
    ges.close()
    return nc, m


def host_prep(cfg, core, inp):
    NB, S, L = cfg.NB, cfg.S, cfg.L
    f = lambda a: np.ascontiguousarray(a, dtype=np.float32)
    b0 = core * NB
    d = {}
    d["x_in"] = f(inp["x"][b0:b0 + NB].reshape(NB * S, D))
    d["c_in"] = f(inp["ctx"][b0:b0 + NB].reshape(NB * LC, D))
    cv = np.concatenate([inp["c"][b0:b0 + NB], inp["c_ctx"][None]], 0)
    d["cT"] = f(cv.reshape(cfg.R, KC, 128).transpose(2, 1, 0))
    return d


def host_shared(cfg, inp):
    L = cfg.L
    f = lambda a: np.ascontiguousarray(a, dtype=np.float32)
    d = {}
    d["w_mod"] = f(inp["w_mod"])
    d["bmodT"] = f(inp["b_mod"].reshape(L, 48, 128).transpose(0, 2, 1))
    d["n1T"] = f(inp["norm1_g"].reshape(L, KC, 128).transpose(0, 2, 1))
    d["n2T"] = f(inp["norm2_g"].reshape(L, KC, 128).transpose(0, 2, 1))
    d["fnT"] = f(inp["final_norm_g"].reshape(KC, 128).T)
    w_in = inp["w_in"]
    d["w_in"] = f(w_in)
    rs = rot_src()
    qcols = np.concatenate([1280 + hh * 64 + rs for hh in range(8)])
    kk = [np.concatenate([1792 + kv * 64 + np.arange(64)] * 2) for kv in range(2)]
    kkp = [np.concatenate([1792 + kv * 64 + rs] * 2) for kv in range(2)]
    cols = np.concatenate([qcols] + kk + kkp)
    d["w_ex"] = f(w_in[:, :, cols])
    d["convT"] = f(inp["conv_w"].transpose(0, 2, 1).reshape(L, 2, 128, 3).transpose(0, 2, 1, 3))
    d["wsT"] = f(inp["gmlp_ws"].transpose(0, 3, 1, 2))
    gbv = inp["gmlp_b"]
    gb = np.repeat(gbv[:, :, None, :], 64, axis=2)
    d["gb"] = f(gb.reshape(L, 2, 128, 128).transpose(0, 2, 1, 3))
    d["sinkbc"] = f(np.broadcast_to(inp["attn_sink"][:, None, :], (L, 128, NH)))
    d["w_br"] = f(np.concatenate([inp["w_br_conv"], inp["w_br_gmlp"], inp["w_br_attn"]], axis=1))
    d["w_out"] = f(inp["w_out"])
    d["ffn_gu"] = f(inp["ffn_w_gu"])
    d["ffn_d"] = f(inp["ffn_w_d"])
    d["moe_rt"] = f(inp["moe_router"])
    d["moe_gu"] = f(inp["moe_w_gu"])
    d["moe_d"] = f(inp["moe_w_d"])
    tc, ts_ = rope_tabs(cfg)
    d["tabC"], d["tabS"] = tc, ts_
    qi = np.arange(128)[:, None]
    jj = np.arange(384)[None, :]
    d["mask"] = np.where((jj >= qi) & (jj <= qi + 256), 0.0, NEG).astype(np.float32)
    return d


_CACHE = {}


def run(cfg, inp, ncores):
    key = (cfg.NB, cfg.S, cfg.L, cfg.dbg, cfg.stop)
    if key not in _CACHE:
        _CACHE[key] = build(cfg)
    nc, m = _CACHE[key]
    sh = host_shared(cfg, inp)
    in_maps = []
    for c in range(ncores):
        dd = dict(sh)
        dd.update(host_prep(cfg, c, inp))
        in_maps.append(dd)
    res = run_bass_kernel_spmd(nc, in_maps, core_ids=list(range(ncores)))
    return res


def kernel(**inputs):
    cfg = Cfg(NB=2, S=4096, L=4)
    inp = {k: np.asarray(v) for k, v in inputs.items()}
    res = run(cfg, inp, 8)
    outs = [r["out"].reshape(cfg.NB, cfg.S, D) for r in res.results]
    return np.ascontiguousarray(np.concatenate(outs, axis=0), dtype=np.float32)
```
